# Optimizing a Trainium2 kernel written in Bass

```python
import math
import jax
import jax.numpy as jnp
from jax import lax
import numpy as np

D_MODEL = 1024
BATCH = 8
SEQ = 4096
DEPTH = 1

GRID_W = 64
CTX_LEN = 256
NH_M = 4
DQK_M = D_MODEL // 8
DV_M = D_MODEL // 4
MLSTM_CHUNK = 128
NH_D = 8
DH_D = D_MODEL // 16
Q_BLOCK = 128
ROPE_BASE = 10000.0
N_EXPERTS = 32
TOP_K = 4
D_FF = D_MODEL
SWIGLU_LIMIT = 7.0
SWIGLU_ALPHA = 1.702
EXPERT_BLOCK = 128
NORM_EPS = 1e-6
SPLIT_SIZES = (NH_M * DQK_M, NH_M * DQK_M, NH_M * DV_M, 4 * NH_M, NH_M * DV_M,
               NH_D * 2 * DH_D, NH_D * 2 * DH_D, NH_D * 2 * DH_D, D_MODEL, D_MODEL)
IN_COLS = sum(SPLIT_SIZES)

kernel_name = 'hybrid_mlstm_diffattn_moe_dit'


def rmsnorm(x, g):
    xf = x.astype(jnp.float32)
    y = xf * lax.rsqrt(jnp.mean(xf * xf, axis=-1, keepdims=True) + NORM_EPS)
    return (y * g.astype(jnp.float32)).astype(x.dtype)


def modulate(u, shift, scale):
    return u * (1 + scale) + shift


def split_proj(u, w_in):
    return jnp.split(u @ w_in, np.cumsum(SPLIT_SIZES)[:-1].tolist(), axis=-1)


def axial_rope_tables(n_tok):
    rows = n_tok // GRID_W
    row = jnp.repeat(jnp.arange(rows, dtype=jnp.float32), GRID_W)
    col = jnp.tile(jnp.arange(GRID_W, dtype=jnp.float32), rows)
    n_freq = DH_D // 4
    inv_freq = ROPE_BASE ** (-jnp.arange(n_freq, dtype=jnp.float32) / n_freq)
    ang_r = (row[:, None] * inv_freq)[:, None, None, :]
    ang_c = (col[:, None] * inv_freq)[:, None, None, :]
    return (jnp.cos(ang_r), jnp.sin(ang_r), jnp.cos(ang_c), jnp.sin(ang_c))


def rotate_pairs(x, cos, sin):
    x1, x2 = jnp.split(x, 2, axis=-1)
    cos = cos.astype(x.dtype)
    sin = sin.astype(x.dtype)
    return jnp.concatenate([x1 * cos - x2 * sin, x2 * cos + x1 * sin], axis=-1)


def apply_axial_rope(x, tables):
    cos_r, sin_r, cos_c, sin_c = tables
    half = DH_D // 2
    return jnp.concatenate([rotate_pairs(x[..., :half], cos_r, sin_r),
                            rotate_pairs(x[..., half:], cos_c, sin_c)], axis=-1)


def mlstm_chunkwise(q, k, v, i_pre, logf, state):
    b_, h_, t_, _ = q.shape
    L = MLSTM_CHUNK
    nc = t_ // L

    def chunks(a):
        return jnp.moveaxis(a.reshape(a.shape[:2] + (nc, L) + a.shape[3:]), 2, 0)

    causal = jnp.tril(jnp.ones((L, L), dtype=bool))

    def step(carry, xs):
        C, n, m = carry
        qc, kc, vc, ic, fc = xs
        bcum = jnp.cumsum(fc, axis=-1)
        log_d = jnp.where(causal, bcum[..., :, None] - bcum[..., None, :] + ic[..., None, :], -jnp.inf)
        inter = bcum + m[..., None]
        m_t = jnp.maximum(inter, jnp.max(log_d, axis=-1))
        s = jnp.einsum('bhtk,bhsk->bhts', qc, kc) * jnp.exp(log_d - m_t[..., None])
        w_inter = jnp.exp(inter - m_t)
        num = w_inter[..., None] * jnp.einsum('bhtk,bhkv->bhtv', qc, C) + jnp.einsum('bhts,bhsv->bhtv', s, vc)
        den = w_inter * jnp.einsum('bhtk,bhk->bht', qc, n) + jnp.sum(s, axis=-1)
        h = num / jnp.maximum(jnp.abs(den), jnp.exp(-m_t))[..., None]
        b_last = bcum[..., -1]
        log_w = b_last[..., None] - bcum + ic
        m_new = jnp.maximum(b_last + m, jnp.max(log_w, axis=-1))
        w = jnp.exp(log_w - m_new[..., None])
        decay = jnp.exp(b_last + m - m_new)
        C_new = decay[..., None, None] * C + jnp.einsum('bhsk,bhsv->bhkv', kc * w[..., None], vc)
        n_new = decay[..., None] * n + jnp.einsum('bhs,bhsk->bhk', w, kc)
        return (C_new, n_new, m_new), h

    state, h = lax.scan(step, state, (chunks(q), chunks(k), chunks(v), chunks(i_pre), chunks(logf)))
    return jnp.moveaxis(h, 0, 2).reshape(b_, h_, t_, v.shape[-1]), state


def mlstm_zero_state(batch):
    return (jnp.zeros((batch, NH_M, DQK_M, DV_M), jnp.float32),
            jnp.zeros((batch, NH_M, DQK_M), jnp.float32),
            jnp.zeros((batch, NH_M), jnp.float32))


def mlstm_prepare(p, gate_b):
    b, t = p[0].shape[:2]

    def heads(a, d):
        return a.reshape(b, t, NH_M, d).transpose(0, 2, 1, 3).astype(jnp.float32)

    q = heads(p[0], DQK_M) * DQK_M ** -0.5
    k = heads(p[1], DQK_M)
    v = heads(p[2], DV_M)
    g = (p[3] + gate_b).astype(jnp.float32).reshape(b, t, 4, NH_M).transpose(2, 0, 3, 1)
    return q, k, v, g


def mlstm_direction(q, k, v, i_pre, f_pre, state, reverse):
    logf = jax.nn.log_sigmoid(f_pre)
    if reverse:
        q, k, v, i_pre, logf = (jnp.flip(a, axis=2) for a in (q, k, v, i_pre, logf))
    h, state = mlstm_chunkwise(q, k, v, i_pre, logf, state)
    if reverse:
        h = jnp.flip(h, axis=2)
    return h, state


def mlstm_output(h, o_raw, g):
    b, t = o_raw.shape[:2]
    hn = rmsnorm(h.transpose(0, 2, 1, 3), g)
    o = jax.nn.sigmoid(o_raw.astype(jnp.float32)).reshape(b, t, NH_M, DV_M)
    return (hn * o).reshape(b, t, NH_M * DV_M).astype(o_raw.dtype)


def diff_prepare(p, tables):
    b, t = p[5].shape[:2]
    q = p[5].reshape(b, t, NH_D, 2, DH_D)
    k = p[6].reshape(b, t, NH_D, 2, DH_D)
    if tables is not None:
        q = apply_axial_rope(q, tables)
        k = apply_axial_rope(k, tables)
    v = p[7].reshape(b, t, NH_D, 2 * DH_D)
    return q.transpose(0, 2, 3, 1, 4), k.transpose(0, 2, 3, 1, 4), v.transpose(0, 2, 1, 3)


def diff_attend(q, k, v, lam):
    s = jnp.einsum('bhmqd,bhmkd->bhmqk', q, k).astype(jnp.float32) * DH_D ** -0.5
    p = jax.nn.softmax(s, axis=-1)
    a = p[:, :, 0] - lam * p[:, :, 1]
    return jnp.einsum('bhqk,bhkd->bhqd', a.astype(v.dtype), v)


def diff_output(o, g, lam_init):
    b, h, t, d = o.shape
    on = rmsnorm(o.transpose(0, 2, 1, 3), g) * (1.0 - lam_init)
    return on.reshape(b, t, h * d)


def merge_branches(p, a_out, b_out, w_a, w_b, w_o):
    y = jax.nn.sigmoid(p[8]) * (a_out @ w_a) + jax.nn.sigmoid(p[9]) * (b_out @ w_b)
    return y @ w_o


def token_mixer(u, uc, tables, w_in, gate_b, m_norm_g, lam_p, d_norm_g, w_a, w_b, w_o, lam_init, need_ctx):
    pl = split_proj(u, w_in)
    pc = split_proj(uc, w_in)
    ql, kl, vl, gl = mlstm_prepare(pl, gate_b)
    qc, kc, vc, gc = mlstm_prepare(pc, gate_b)
    st0 = mlstm_zero_state(uc.shape[0])
    h_cf, st_f = mlstm_direction(qc, kc, vc, gc[0], gc[1], st0, False)
    h_cb, st_b = mlstm_direction(qc, kc, vc, gc[2], gc[3], st0, True)
    h_lf, _ = mlstm_direction(ql, kl, vl, gl[0], gl[1], st_f, False)
    h_lb, _ = mlstm_direction(ql, kl, vl, gl[2], gl[3], st_b, True)
    a_lat = mlstm_output(h_lf + h_lb, pl[4], m_norm_g)
    lp = lam_p.astype(jnp.float32)
    lam = jnp.exp(jnp.sum(lp[0] * lp[1])) - jnp.exp(jnp.sum(lp[2] * lp[3])) + lam_init
    qdl, kdl, vdl = diff_prepare(pl, tables)
    qdc, kdc, vdc = diff_prepare(pc, None)
    k_all = jnp.concatenate([kdc, kdl], axis=3)
    v_all = jnp.concatenate([vdc, vdl], axis=2)
    b, h, _, t, dh = qdl.shape
    n_blk = t // Q_BLOCK
    q_blocks = jnp.moveaxis(qdl.reshape(b, h, 2, n_blk, Q_BLOCK, dh), 3, 0)
    o_blocks = lax.map(lambda qb: diff_attend(qb, k_all, v_all, lam), q_blocks)
    o_lat = jnp.moveaxis(o_blocks, 0, 2).reshape(b, h, t, 2 * dh)
    b_lat = diff_output(o_lat, d_norm_g, lam_init)
    y = merge_branches(pl, a_lat, b_lat, w_a, w_b, w_o)
    if not need_ctx:
        return y, None
    a_ctx = mlstm_output(h_cf + h_cb, pc[4], m_norm_g)
    b_ctx = diff_output(diff_attend(qdc, kdc, vdc, lam), d_norm_g, lam_init)
    return y, merge_branches(pc, a_ctx, b_ctx, w_a, w_b, w_o)


def moe_ffn(u, router_w, router_b, w1, b1, w2, b2):
    b, t, d = u.shape
    n_tok = b * t
    xf = u.reshape(n_tok, d)
    logits = (xf @ router_w + router_b).astype(jnp.float32)
    top_val, top_idx = lax.top_k(logits, TOP_K)
    gate = jax.nn.softmax(top_val, axis=-1).astype(u.dtype)
    n_asg = n_tok * TOP_K
    flat_e = top_idx.reshape(n_asg)
    flat_tok = jnp.arange(n_asg, dtype=jnp.int32) // TOP_K
    order = jnp.argsort(flat_e)
    sorted_e = flat_e[order]
    counts = jnp.bincount(flat_e, length=N_EXPERTS)
    padded = (counts + EXPERT_BLOCK - 1) // EXPERT_BLOCK * EXPERT_BLOCK
    pad_end = jnp.cumsum(padded)
    pad_start = pad_end - padded
    sorted_start = jnp.cumsum(counts) - counts
    dest = pad_start[sorted_e] + jnp.arange(n_asg) - sorted_start[sorted_e]
    n_buf = -(-n_asg // EXPERT_BLOCK) * EXPERT_BLOCK + N_EXPERTS * EXPERT_BLOCK
    buf_tok = jnp.zeros((n_buf,), jnp.int32).at[dest].set(flat_tok[order])
    buf_w = jnp.zeros((n_buf,), u.dtype).at[dest].set(gate.reshape(n_asg)[order])
    n_blk = n_buf // EXPERT_BLOCK
    blk_e = jnp.minimum(jnp.searchsorted(pad_end, jnp.arange(n_blk) * EXPERT_BLOCK, side='right'), N_EXPERTS - 1)

    def expert_block(args):
        tok, e = args
        hcat = xf[tok] @ w1[e] + b1[e]
        h_glu = jnp.minimum(hcat[:, :D_FF], SWIGLU_LIMIT)
        h_lin = jnp.clip(hcat[:, D_FF:], -SWIGLU_LIMIT, SWIGLU_LIMIT)
        act = h_glu * jax.nn.sigmoid(SWIGLU_ALPHA * h_glu) * (h_lin + 1)
        return act @ w2[e] + b2[e]

    y_buf = lax.map(expert_block, (buf_tok.reshape(n_blk, EXPERT_BLOCK), blk_e))
    y = jax.ops.segment_sum(y_buf.reshape(n_buf, d) * buf_w[:, None], buf_tok, num_segments=n_tok)
    return y.reshape(b, t, d)


def setup_inputs(seed: int = 0) -> dict:
    key = jax.random.key(seed)
    ks = jax.random.split(key, 24)

    def nrm(k, shape, scale):
        return jax.random.normal(k, shape, jnp.float32) * scale

    L = DEPTH
    gate_base = jnp.array([0.0, 3.0, 0.0, 3.0], jnp.float32)[None, :, None]
    return {
        'x': nrm(ks[0], (BATCH, SEQ, D_MODEL), 1.0),
        'c': nrm(ks[1], (BATCH, D_MODEL), 1.0),
        'ctx': nrm(ks[2], (BATCH, CTX_LEN, D_MODEL), 1.0),
        'c_ctx': nrm(ks[3], (D_MODEL,), 1.0),
        'ada_w': nrm(ks[4], (L, D_MODEL, 6 * D_MODEL), 0.5 * D_MODEL ** -0.5),
        'ada_b': nrm(ks[5], (L, 6 * D_MODEL), 0.02),
        'norm1_g': 1.0 + nrm(ks[6], (L, D_MODEL), 0.02),
        'norm2_g': 1.0 + nrm(ks[7], (L, D_MODEL), 0.02),
        'w_in': nrm(ks[8], (L, D_MODEL, IN_COLS), D_MODEL ** -0.5),
        'mlstm_gate_b': (gate_base + nrm(ks[9], (L, 4, NH_M), 0.1)).reshape(L, 4 * NH_M),
        'mlstm_norm_g': 1.0 + nrm(ks[10], (L, NH_M, DV_M), 0.02),
        'diff_lambda': nrm(ks[11], (L, 4, DH_D), 0.1),
        'diff_norm_g': 1.0 + nrm(ks[12], (L, NH_D, 2 * DH_D), 0.02),
        'w_branch_a': nrm(ks[13], (L, NH_M * DV_M, D_MODEL), (NH_M * DV_M) ** -0.5),
        'w_branch_b': nrm(ks[14], (L, NH_D * 2 * DH_D, D_MODEL), (NH_D * 2 * DH_D) ** -0.5),
        'w_out': nrm(ks[15], (L, D_MODEL, D_MODEL), D_MODEL ** -0.5),
        'router_w': nrm(ks[16], (L, D_MODEL, N_EXPERTS), D_MODEL ** -0.5),
        'router_b': nrm(ks[17], (L, N_EXPERTS), 0.01),
        'exp_w1': nrm(ks[18], (L, N_EXPERTS, D_MODEL, 2 * D_FF), D_MODEL ** -0.5),
        'exp_b1': nrm(ks[19], (L, N_EXPERTS, 2 * D_FF), 0.01),
        'exp_w2': nrm(ks[20], (L, N_EXPERTS, D_FF, D_MODEL), D_FF ** -0.5),
        'exp_b2': nrm(ks[21], (L, N_EXPERTS, D_MODEL), 0.01),
        'final_norm_g': 1.0 + nrm(ks[22], (D_MODEL,), 0.02),
    }


def reference(x, c, ctx, c_ctx, ada_w, ada_b, norm1_g, norm2_g, w_in, mlstm_gate_b, mlstm_norm_g,
              diff_lambda, diff_norm_g, w_branch_a, w_branch_b, w_out, router_w, router_b,
              exp_w1, exp_b1, exp_w2, exp_b2, final_norm_g):
    tables = axial_rope_tables(x.shape[1])
    xc = ctx
    for l in range(DEPTH):
        need_ctx = l < DEPTH - 1
        lam_init = 0.8 - 0.6 * math.exp(-0.3 * l)
        mod = jax.nn.silu(c) @ ada_w[l] + ada_b[l]
        sh1, sc1, g1, sh2, sc2, g2 = jnp.split(mod[:, None, :], 6, axis=-1)
        modc = jax.nn.silu(c_ctx) @ ada_w[l] + ada_b[l]
        sh1c, sc1c, g1c, sh2c, sc2c, g2c = jnp.split(modc, 6, axis=-1)
        u = modulate(rmsnorm(x, norm1_g[l]), sh1, sc1)
        uc = modulate(rmsnorm(xc, norm1_g[l]), sh1c, sc1c)
        y, yc = token_mixer(u, uc, tables, w_in[l], mlstm_gate_b[l], mlstm_norm_g[l], diff_lambda[l],
                            diff_norm_g[l], w_branch_a[l], w_branch_b[l], w_out[l], lam_init, need_ctx)
        x = x + g1 * y
        x = x + g2 * moe_ffn(modulate(rmsnorm(x, norm2_g[l]), sh2, sc2), router_w[l], router_b[l],
                             exp_w1[l], exp_b1[l], exp_w2[l], exp_b2[l])
        if need_ctx:
            xc = xc + g1c * yc
            xc = xc + g2c * moe_ffn(modulate(rmsnorm(xc, norm2_g[l]), sh2c, sc2c), router_w[l], router_b[l],
                                    exp_w1[l], exp_b1[l], exp_w2[l], exp_b2[l])
    return rmsnorm(x, final_norm_g)
```

```python
import math
import os
from contextlib import ExitStack

import ml_dtypes
import numpy as np

import concourse.bass as bass
import concourse.mybir as mybir
from concourse.bass_utils import run_bass_kernel_spmd

F32 = mybir.dt.float32
BF16 = mybir.dt.bfloat16
ALU = mybir.AluOpType
AF = mybir.ActivationFunctionType
AX = mybir.AxisListType

D = 1024
T = 4096
TC = 256
TT = T + TC
NT = TT // 128
NE = 32
BLK = 256
NBLK = 96
NBUF = NBLK * BLK
EPS = 1e-6
LAM_INIT = 0.8 - 0.6 * math.exp(0.0)
O_MQ, O_MK, O_MV, O_MG, O_MO, O_DQ, O_DK, O_DV, O_GA, O_GB = 0, 512, 1024, 2048, 2064, 3088, 4112, 5136, 6160, 7184
INC = 8208

ENG_ATTR = {'pe': 'tensor', 'act': 'scalar', 'dve': 'vector', 'pool': 'gpsimd', 'sp': 'sync'}
NDMA_SEMS = 8
NPOOL_SEMS = 4


class Sched:
    def __init__(self, nc, stack):
        self.nc = nc
        self.prog = {e: [] for e in ENG_ATTR}
        self.sem = {}
        for e in ENG_ATTR:
            self.sem[e] = stack.enter_context(nc.semaphore('s_' + e))
        self.dq = {}
        for q in ('sp', 'pool'):
            for i in range(NDMA_SEMS):
                k = ('dma', q, i)
                self.sem[k] = stack.enter_context(nc.semaphore('d_%s%d' % (q, i)))
            self.dq[q] = 0
        self.count = {k: 0 for k in self.sem}
        self.seen = {e: {} for e in ENG_ATTR}
        self.state = {}
        self.ninstr = 0

    @staticmethod
    def _ov(a, b):
        return a is None or b is None or a == b

    def _collect(self, eng, reads, writes, is_dma):
        need = {}

        def add(ev, kind):
            if ev is None:
                return
            k, v = ev
            if (not is_dma) and k == eng:
                if kind == 'rar' or (eng == 'pe' and kind != 'raw'):
                    return
            if need.get(k, 0) < v:
                need[k] = v

        for (n, s) in reads:
            for slot, st in self.state.get(n, {}).items():
                if self._ov(slot, s):
                    add(st[0], 'raw')
                    if n.startswith('PS'):
                        for r in st[1]:
                            add(r, 'rar')
        for (n, s) in writes:
            for slot, st in self.state.get(n, {}).items():
                if self._ov(slot, s):
                    add(st[0], 'waw')
                    for r in st[1]:
                        add(r, 'war')
        return need

    def _emit_waits(self, eng, need):
        seen = self.seen[eng]
        for k, v in need.items():
            if seen.get(k, 0) >= v:
                continue
            seen[k] = v
            self.prog[eng].append(('wait', self.sem[k], v))

    def _mark(self, ev, reads, writes):
        for (n, s) in writes:
            d = self.state.setdefault(n, {})
            if s is None:
                d.clear()
                d[None] = [ev, []]
            else:
                d[s] = [ev, []]
        for (n, s) in reads:
            d = self.state.setdefault(n, {})
            st = d.setdefault(s, [None, []])
            st[1] = [r for r in st[1] if r[0] != ev[0]] + [ev]

    def op(self, eng, method, kw, reads=(), writes=(), inc=True):
        need = self._collect(eng, reads, writes, False)
        self._emit_waits(eng, need)
        self.ninstr += 1
        if inc:
            self.count[eng] += 1
            ev = (eng, self.count[eng])
            self.prog[eng].append(('ins', method, kw, self.sem[eng], 1))
        else:
            ev = (eng, self.count[eng] + 1)
            self.prog[eng].append(('ins', method, kw, None, 0))
        self._mark(ev, reads, writes)

    def dma(self, q, kw, reads=(), writes=(), method=None):
        nsem = NDMA_SEMS if q != 'pool' else NPOOL_SEMS
        i = self.dq[q] % nsem
        self.dq[q] += 1
        k = ('dma', q, i)
        need = self._collect(q, reads, writes, True)
        if self.count[k] > 0:
            need[k] = max(need.get(k, 0), self.count[k])
        self._emit_waits(q, need)
        self.ninstr += 1
        self.count[k] += 16
        ev = (k, self.count[k])
        if method is None:
            method = getattr(self.nc, ENG_ATTR[q]).dma_start
        self.prog[q].append(('ins', method, kw, self.sem[k], 16))
        self._mark(ev, reads, writes)

    def barrier(self):
        for e in ENG_ATTR:
            need = {k: v for k, v in self.count.items() if v > 0 and k != e}
            self._emit_waits(e, need)

    def emit(self):
        nc = self.nc
        if os.environ.get('DBGPROG'):
            names = {id(v): k for k, v in self.sem.items()}
            for e in ENG_ATTR:
                print('ENGINE', e)
                c = 0
                for it in self.prog[e][-int(os.environ['DBGPROG']):]:
                    if it[0] == 'wait':
                        print('   wait', names[id(it[1])], it[2])
                    else:
                        print('   ins', getattr(it[1], '__name__', it[1]), 'inc' if it[3] is not None else '-', [k for k in it[2] if k in ('func',)] and it[2].get('func'))
        with nc.Block() as block:
            for e, attr in ENG_ATTR.items():
                prog = self.prog[e]

                def body(engine, prog=prog):
                    for it in prog:
                        if it[0] == 'wait':
                            engine.wait_ge(it[1], it[2])
                        else:
                            ins = it[1](**it[2])
                            if it[3] is not None:
                                ins.then_inc(it[3], it[4])
                getattr(block, attr)(body)
        self.prog = {e: [] for e in ENG_ATTR}


def build_nc(dbg=False, stop=None):
    nc = bass.Bass("TRN2", target_bir_lowering=False)

    def din(name, shape, dt=F32):
        return nc.dram_tensor(name, shape, dt, kind="ExternalInput")

    def dscr(name, shape, dt=F32):
        return nc.dram_tensor(name, shape, dt, kind="Internal")

    xh = din("x", [T, D]); ctxh = din("ctx", [TC, D]); cvh = din("cvec", [D, 2])
    adawh = din("ada_w", [D, 6 * D]); vecsh = din("vecs", [64, 128]); b1h = din("b1r", [512, 128])
    winh = din("w_in", [D, INC]); gbh = din("gate_b", [16]); mngh = din("mng", [D]); dngh = din("dng", [D])
    fngh = din("fng", [D]); dlh = din("dlam", [256]); wah = din("w_a", [D, D]); wbh = din("w_b", [D, D])
    woh = din("w_o", [D, D]); rwh = din("rw", [D, NE]); rbh = din("rb", [NE])
    w1h = din("w1", [NE, D, 2 * D]); w2h = din("w2", [NE, D, D]); b2h = din("b2", [NE, D])
    idfh = din("idf", [128, 128]); idbh = din("idb", [128, 128], BF16)
    triuh = din("triu", [128, 128]); trilh = din("tril", [128, 128]); onesh = din("ones", [128, 128])
    cosh_ = din("cost", [128, T]); sinh_ = din("sint", [128, T])
    strih = din("stri", [128, 128]); thrBh = din("thrB", [128, NBLK]); pidxh = din("pidx", [128, 1])
    yh = nc.dram_tensor("y", [T, D], F32, kind="ExternalOutput")

    modscr = dscr("modscr", [48 * 128]); hfscr = dscr("hfscr", [4, 32, 128, 256])
    aTd = dscr("aTd", [128, 8, T], BF16); bTd = dscr("bTd", [128, 8, T], BF16)
    sgd = dscr("sgd", [128, 16, T], BF16); x1d = dscr("x1d", [T, D]); u2d = dscr("u2d", [128, 8, T], BF16)
    w1bd = dscr("w1bd", [NE * 128, 8 * 2 * D], BF16); w2bd = dscr("w2bd", [NE * 128, 8 * D], BF16)
    b1Td = dscr("b1Td", [NE * 128, 16]); u2tokd = dscr("u2tokd", [T, D], BF16)
    Xg = dscr("Xg", [NBUF, D], BF16); Yg = dscr("Yg", [NBUF, D])

    def bc(h, off, n, parts=128):
        return bass.AP(h, off, [[0, parts], [1, n]])

    with ExitStack() as gst:
        S = Sched(nc, gst)

        def sbg(name, shape, dt=F32):
            return gst.enter_context(nc.sbuf_tensor('sb_' + name, shape, dt))

        PSUM = [gst.enter_context(nc.psum_tensor("PS%d" % i, [128, 1024], F32)) for i in range(4)]

        def PS(i, b):
            return PSUM[i][:, b * 512:(b + 1) * 512]

        def PSb(i, b):
            return PSUM[i][:].bitcast(BF16)[:, b * 1024:(b + 1) * 1024]

        def PN(i, b):
            return ('PS%d' % i, b)

        def PE(out, lhsT, rhs, start, stop, R, W, inc=True):
            S.op('pe', nc.tensor.matmul, dict(out=out, lhsT=lhsT, rhs=rhs, start=start, stop=stop), R, W, inc)

        def PET(out, in_, ident, R, W, inc=True):
            S.op('pe', nc.tensor.transpose, dict(out=out, in_=in_, identity=ident), R, W, inc)

        def ACT(out, in_, func, R, W, **kw):
            S.op('act', nc.scalar.activation, dict(out=out, in_=in_, func=func, **kw), R, W)

        def TS(eng, out, in0, s1, s2, op0, op1, R, W):
            m = nc.vector.tensor_scalar if eng == 'dve' else nc.gpsimd.tensor_scalar
            kw = dict(out=out, in0=in0, scalar1=s1, scalar2=s2, op0=op0)
            if op1 is not None:
                kw['op1'] = op1
            S.op(eng, m, kw, R, W)

        def TTo(eng, out, in0, in1, op, R, W):
            m = nc.vector.tensor_tensor if eng == 'dve' else nc.gpsimd.tensor_tensor
            S.op(eng, m, dict(out=out, in0=in0, in1=in1, op=op), R, W)

        def STT(eng, out, in0, scalar, in1, op0, op1, R, W):
            m = nc.vector.scalar_tensor_tensor if eng == 'dve' else nc.gpsimd.scalar_tensor_tensor
            S.op(eng, m, dict(out=out, in0=in0, scalar=scalar, in1=in1, op0=op0, op1=op1), R, W)

        def CP(eng, out, in_, R, W):
            if eng == 'act':
                ACT(out, in_, AF.Copy, R, W)
            else:
                m = nc.vector.tensor_copy if eng == 'dve' else nc.gpsimd.tensor_copy
                S.op(eng, m, dict(out=out, in_=in_), R, W)

        def DMA(q, out, in_, R, W):
            S.dma(q, dict(out=out, in_=in_), R, W)


        def dump(name, src, shape, dt, reads):
            h_ = nc.dram_tensor(name, shape, dt, kind="ExternalOutput")
            DMA('sp', h_.ap(), src, reads, [(name, None)])

        def finish_dbg():
            S.barrier()
            S.emit()
            return nc
        idf = sbg("idf", [128, 128]); idb = sbg("idb", [128, 128], BF16)
        triu = sbg("triu", [128, 128]); tril = sbg("tril", [128, 128]); ones = sbg("ones", [128, 128])
        epsb = sbg("epsb", [128, 1])
        prm = sbg("prm", [128, 6, 8])
        DMA('sp', idf[:], idfh.ap(), [], [('idf', None)])
        DMA('sp', idb[:], idbh.ap(), [], [('idb', None)])
        DMA('sp', triu[:], triuh.ap(), [], [('triu', None)])
        DMA('sp', tril[:], trilh.ap(), [], [('tril', None)])
        DMA('sp', ones[:], onesh.ap(), [], [('ones', None)])
        S.op('dve', nc.vector.memset, dict(ap=epsb[:], constant=EPS), [], [('epsb', None)])

        with ExitStack() as st:
            def sb(name, shape, dt=F32):
                return st.enter_context(nc.sbuf_tensor('sb_' + name, shape, dt))
            cv = sb("cv", [128, 8, 2]); sc = sb("sc", [128, 8, 2]); vin = sb("vin", [64, 128]); vT = sb("vT", [128, 64])
            adw = [sb("adw%d" % i, [128, 8, 1024]) for i in range(2)]
            modT = sb("modT", [128, 48, 2]); mrow = sb("mrow", [48, 128]); tmpa = sb("tmpa", [128, 8])
            DMA('sp', cv[:], cvh.ap().rearrange("(k p) n -> p k n", p=128), [], [('cv', None)])
            DMA('sp', vin[:], vecsh.ap(), [], [('vin', None)])
            ACT(sc[:], cv[:], AF.Silu, [('cv', None)], [('sc', None)])
            PET(PS(0, 0)[:, 0:64], vin[:], idf[0:64, 0:64], [('vin', None), ('idf', None)], [PN(0, 0)])
            CP('dve', vT[:], PS(0, 0)[:, 0:64], [PN(0, 0)], [('vT', None)])
            for jb in range(6):
                DMA('sp', adw[jb % 2][:], adawh.ap()[:, jb * 1024:(jb + 1) * 1024].rearrange("(k p) n -> p k n", p=128),
                    [], [('adw', jb % 2)])
                for jj in range(8):
                    j = jb * 8 + jj
                    for kc in range(8):
                        PE(PS(0, 1)[:, 2 * j:2 * j + 2], adw[jb % 2][:, kc, jj * 128:(jj + 1) * 128], sc[:, kc, :],
                           kc == 0, kc == 7, [('adw', jb % 2), ('sc', None)], [PN(0, 1)], inc=(kc == 7))
            pm = PS(0, 1)[:, 0:96].rearrange("p (j n) -> p j n", n=2)
            for n_ in range(2):
                TTo('dve', modT[:, :, n_], pm[:, :, n_], vT[:, 0:48], ALU.add, [PN(0, 1), ('vT', None)], [('modT', n_)])
            for (pi, n_, gcol, sccol, shcol) in ((0, 0, 48, 8, 0), (2, 1, 48, 8, 0), (4, 0, 56, 32, 24)):
                TS('dve', tmpa[:], modT[:, sccol:sccol + 8, n_], 1.0, None, ALU.add, None, [('modT', n_)], [('tmpa', None)])
                TTo('dve', prm[:, pi, :], tmpa[:], vT[:, gcol:gcol + 8], ALU.mult, [('tmpa', None), ('vT', None)], [('prm', pi)])
                CP('dve', prm[:, pi + 1, :], modT[:, shcol:shcol + 8, n_], [('modT', n_)], [('prm', pi + 1)])
            PET(PS(0, 0)[0:48, 0:128], modT[:, :, 0], idf[:], [('modT', 0), ('idf', None)], [PN(0, 0)])
            CP('dve', mrow[:], PS(0, 0)[0:48, 0:128], [PN(0, 0)], [('mrow', None)])
            DMA('sp', modscr.ap().rearrange("(j p) -> j p", p=128), mrow[:], [('mrow', None)], [('modscr', None)])
            S.barrier()
            S.emit()

        if stop == 'A':
            dump('d_prm', prm[:], [128, 6, 8], F32, [('prm', None)])
            return finish_dbg()
        ust = ExitStack() if stop is None else gst
        uT = ust.enter_context(nc.sbuf_tensor("sb_uT", [128, 8, TT], BF16))

        def norm_rstd(src, junk, ssb, nm):
            ACT(junk, src, AF.Square, [(nm, None)], [('junk', None), (nm + 'ss', 0)], accum_out=ssb[:, 0:1])
            ACT(ssb[:, 1:2], ssb[:, 0:1], AF.Ln, [(nm + 'ss', 0), ('epsb', None)], [(nm + 'ss', 1)], scale=1.0 / D, bias=epsb[:, 0:1])
            ACT(ssb[:, 2:3], ssb[:, 1:2], AF.Exp, [(nm + 'ss', 1)], [(nm + 'ss', 2)], scale=-0.5)

        with ExitStack() as st:
            def sb(name, shape, dt=F32):
                return st.enter_context(nc.sbuf_tensor('sb_' + name, shape, dt))
            xin = [sb("xin%d" % i, [128, D]) for i in range(3)]
            ssb = [sb("ssb%d" % i, [128, 4]) for i in range(3)]
            xn = [sb("xn%d" % i, [128, D], BF16) for i in range(2)]
            junk = sb("junk", [128, D], BF16)
            for ti in range(NT):
                b3 = ti % 3
                src = ctxh.ap()[ti * 128:(ti + 1) * 128, :] if ti < 2 else xh.ap()[(ti - 2) * 128:(ti - 1) * 128, :]
                nm = 'xin%d' % b3
                DMA('sp', xin[b3][:], src, [], [(nm, None)])
                norm_rstd(xin[b3][:], junk[:], ssb[b3], nm)
                xnn = 'xn%d' % (ti % 2)
                TS('dve', xn[ti % 2][:], xin[b3][:], ssb[b3][:, 2:3], None, ALU.mult, None, [(nm, None), (nm + 'ss', 2)], [(xnn, None)])
                pi = 2 if ti < 2 else 0
                for g in range(2):
                    for j in range(4):
                        kc = g * 4 + j
                        PET(PSb(3, g)[:, j * 128:(j + 1) * 128], xn[ti % 2][:, kc * 128:(kc + 1) * 128], idb[:],
                            [(xnn, None), ('idb', None)], [PN(3, g)], inc=(j == 3))
                    for j in range(4):
                        kc = g * 4 + j
                        o = uT[:, kc, ti * 128:(ti + 1) * 128]
                        i_ = PSb(3, g)[:, j * 128:(j + 1) * 128]
                        if j % 2 == 0:
                            TS('dve', o, i_, prm[:, pi, kc:kc + 1], prm[:, pi + 1, kc:kc + 1], ALU.mult, ALU.add,
                               [PN(3, g), ('prm', pi), ('prm', pi + 1)], [('uT', ti)])
                        else:
                            ACT(o, i_, AF.Identity, [PN(3, g), ('prm', pi), ('prm', pi + 1)], [('uT', ti)],
                                scale=prm[:, pi, kc:kc + 1], bias=prm[:, pi + 1, kc:kc + 1])
            S.barrier()
            S.emit()

        if dbg:
            dbg_u = nc.dram_tensor("dbg_u", [128, 8, TT], BF16, kind="ExternalOutput")
            DMA('sp', dbg_u.ap(), uT[:], [('uT', None)], [('dbg_u', None)])

        if stop == 'B':
            dump('d_uT', uT[:], [128, 8, TT], BF16, [('uT', None)])
            return finish_dbg()
        def wslice(c0, n):
            return winh.ap()[:, c0:c0 + n].rearrange("(k p) n -> p k n", p=128)

        with ExitStack() as st:
            def sb(name, shape, dt=F32):
                return st.enter_context(nc.sbuf_tensor('sb_' + name, shape, dt))
            wg = sb("wg", [128, 8, 16], BF16); gb34 = sb("gb34", [128, NT, 16]); G = sb("G", [128, NT, 16])
            SP = [sb("SP%d" % d, [128, NT * 4]) for d in range(2)]
            EE = [sb("EE%d" % d, [128, NT, 4]) for d in range(2)]
            WW = [sb("WW%d" % d, [128, NT, 4]) for d in range(2)]
            EL = [sb("EL%d" % d, [128, NT, 4]) for d in range(2)]
            tmpw = sb("tmpw", [128, NT, 4])
            cst = [sb("cst%d" % d, [128, 272]) for d in range(2)]
            gm = sb("gm", [128, D])
            DMA('pool', wg[:], wslice(O_MG, 16), [], [('wg', None)])
            gb16 = sb("gb16", [128, 16])
            DMA('sp', gb16[:], bc(gbh, 0, 16), [], [('gb16', None)])
            DMA('sp', gm[:], bc(mngh, 0, D), [], [('gm', None)])
            for ti in range(NT):
                bank = 0 if ti < 32 else 1
                col = (ti % 32) * 16
                for kc in range(8):
                    PE(PS(0, bank)[:, col:col + 16], uT[:, kc, ti * 128:(ti + 1) * 128], wg[:, kc, :], kc == 0, kc == 7,
                       [('uT', None), ('wg', None)], [PN(0, bank)], inc=(kc == 7))
            for ti in range(NT):
                bank = 0 if ti < 32 else 1
                col = (ti % 32) * 16
                TTo('dve', G[:, ti, :], PS(0, bank)[:, col:col + 16], gb16[:], ALU.add, [PN(0, bank), ('gb16', None)], [('G', ti)])
            if stop == 'C0a':
                dump('d_G', G[:], [128, NT, 16], F32, [('G', None)])
                return finish_dbg()
            for d in range(2):
                spv = SP[d][:].rearrange("p (t n) -> p t n", n=4)
                ACT(spv, G[:, :, 8 * d + 4:8 * d + 8], AF.Exp, [('G', None)], [('SP', d)], scale=-1.0)
                ACT(SP[d][:], SP[d][:], AF.Ln, [('SP', d), ('ones', None)], [('SP', d)], bias=ones[:, 0:1])
                if stop == 'C0b':
                    continue
                tri = triu if d == 0 else tril
                PE(PS(1, d)[:, 0:136], tri[:], SP[d][:], True, True, [('SP', d), ('triu', None), ('tril', None)], [PN(1, d)])
                PE(PS(1, d)[:, 136:272], ones[:], SP[d][:], True, True, [('SP', d), ('ones', None)], [PN(1, d)])
                if stop == 'C0c':
                    CP('dve', EE[d][:].rearrange("p t n -> p (t n)"), PS(1, d)[:, 0:136], [PN(1, d)], [('EE', d)])
                    continue
                CP('dve', cst[d][:], PS(1, d)[:, 0:272], [PN(1, d)], [('cst', d)])
                cs = cst[d][:, 0:136].rearrange("p (t n) -> p t n", n=4)
                tt_ = cst[d][:, 136:272].rearrange("p (t n) -> p t n", n=4)
                SK = os.environ.get('DBGSKIP', '')
                if '1' not in SK:
                    ACT(EE[d][:], cs, AF.Exp, [('cst', d)], [('EE', d)], scale=-1.0)
                if '2' not in SK:
                    TTo('dve', tmpw[:], G[:, :, 8 * d:8 * d + 4], cs, ALU.add, [('G', None), ('cst', d)], [('tmpw', None)])
                if '3' not in SK:
                    ACT(WW[d][:], tmpw[:], AF.Exp, [('tmpw', None)], [('WW', d)])
                if '4' not in SK:
                    ACT(EL[d][:], tt_, AF.Exp, [('cst', d)], [('EL', d)], scale=-1.0)

            if stop == 'C0b':
                dump('d_SP', SP[0][:], [128, NT * 4], F32, [('SP', 0)])
                return finish_dbg()
            if stop == 'C0c':
                dump('d_EE', EE[0][:], [128, NT, 4], F32, [('EE', 0)])
                return finish_dbg()
            if stop == 'C0':
                dump('d_EE', EE[0][:], [128, NT, 4], F32, [('EE', 0)])
                dump('d_WW', WW[1][:], [128, NT, 4], F32, [('WW', 1)])
                dump('d_EL', EL[0][:], [128, NT, 4], F32, [('EL', 0)])
                dump('d_G', G[:], [128, NT, 16], F32, [('G', None)])
                return finish_dbg()
            wm = sb("wm", [128, 8, 768], BF16)
            qT = sb("qT", [128, TT], BF16); kT = sb("kT", [128, TT], BF16)
            vt = sb("vt", [128, NT, 257], BF16)
            kt = [sb("kt%d" % d, [128, NT, 128], BF16) for d in range(2)]
            SD = [sb("SD%d" % i, [128, 128], BF16) for i in range(2)]
            Cf = sb("Cf", [128, 257]); Cb = sb("Cb", [128, 257], BF16); tmpC = sb("tmpC", [128, 257])
            sml = [sb("sml%d" % i, [128, 8]) for i in range(2)]
            hst = [sb("hst%d" % i, [128, 256]) for i in range(3)]
            hfl = [sb("hfl%d" % i, [128, 256]) for i in range(3)]
            hs = [sb("hs%d" % i, [128, 256]) for i in range(2)]
            osg = [sb("osg%d" % i, [128, 256]) for i in range(2)]
            ab = [sb("ab%d" % i, [128, 256], BF16) for i in range(2)]
            aTs = [sb("aTs%d" % i, [128, 2, 512], BF16) for i in range(2)]
            junk2 = sb("junk2", [128, 256], BF16)
            S.op('pool', nc.gpsimd.memset, dict(ap=vt[:, :, 256:257], constant=1.0), [], [('vt1', None)])

            for h in range(4):
                DMA('pool', wm[:, :, 0:128], wslice(O_MQ + h * 128, 128), [], [('wm', 0)])
                DMA('pool', wm[:, :, 128:256], wslice(O_MK + h * 128, 128), [], [('wm', 1)])
                DMA('pool', wm[:, :, 256:512], wslice(O_MV + h * 256, 256), [], [('wm', 2)])
                DMA('pool', wm[:, :, 512:768], wslice(O_MO + h * 256, 256), [], [('wm', 3)])
                cnt = 0
                for blk in range(9):
                    c0 = blk * 512
                    n = min(512, TT - c0)
                    for which in range(2):
                        pi_, pb_ = (cnt // 2) % 2, cnt % 2
                        cnt += 1
                        for kc in range(8):
                            PE(PS(pi_, pb_)[:, 0:n], wm[:, kc, which * 128:(which + 1) * 128], uT[:, kc, c0:c0 + n], kc == 0, kc == 7,
                               [('wm', which), ('uT', None)], [PN(pi_, pb_)], inc=(kc == 7))
                        if which == 0:
                            ACT(qT[:, c0:c0 + n], PS(pi_, pb_)[:, 0:n], AF.Copy, [PN(pi_, pb_)], [('qT', blk)], scale=128.0 ** -0.5)
                        else:
                            CP('dve', kT[:, c0:c0 + n], PS(pi_, pb_)[:, 0:n], [PN(pi_, pb_)], [('kT', blk)])
                for ti in range(NT):
                    pb_ = ti % 2
                    for kc in range(8):
                        PE(PS(2, pb_)[:, 0:384], uT[:, kc, ti * 128:(ti + 1) * 128], wm[:, kc, 128:512], kc == 0, kc == 7,
                           [('uT', None), ('wm', 1), ('wm', 2)], [PN(2, pb_)], inc=(kc == 7))
                    TS('dve', kt[0][:, ti, :], PS(2, pb_)[:, 0:128], WW[0][:, ti, h:h + 1], None, ALU.mult, None,
                       [PN(2, pb_), ('WW', 0)], [('kt0', ti)])
                    TS('dve', kt[1][:, ti, :], PS(2, pb_)[:, 0:128], WW[1][:, ti, h:h + 1], None, ALU.mult, None,
                       [PN(2, pb_), ('WW', 1)], [('kt1', ti)])
                    CP('act', vt[:, ti, 0:256], PS(2, pb_)[:, 128:384], [PN(2, pb_)], [('vt', ti)])

                for d in range(2):
                    order = list(range(NT)) if d == 0 else [1, 0] + list(range(NT - 1, 1, -1))
                    mask = triu if d == 0 else tril
                    lat = [t_ for t_ in order if t_ >= 2]

                    def emit_sd(ti, li):
                        sl = li % 2
                        cs_ = slice(ti * 128, (ti + 1) * 128)
                        PE(PS(0, sl)[:, 0:128], kT[:, cs_], qT[:, cs_], True, True, [('kT', None), ('qT', None)], [PN(0, sl)])
                        STT('dve', SD[sl][:], PS(0, sl)[:, 0:128], WW[d][:, ti, h:h + 1], mask[:], ALU.mult, ALU.mult,
                            [PN(0, sl), ('WW', d), ('triu', None), ('tril', None)], [('SD', sl)])

                    def emit_oraw(li_):
                        ti_ = lat[li_]
                        b2_ = li_ % 2
                        for kc in range(8):
                            PE(PS(3, 0)[:, 0:256], uT[:, kc, ti_ * 128:(ti_ + 1) * 128], wm[:, kc, 512:768], kc == 0, kc == 7,
                               [('uT', None), ('wm', 3)], [PN(3, 0)], inc=(kc == 7))
                        ACT(osg[b2_][:], PS(3, 0)[:, 0:256], AF.Sigmoid, [PN(3, 0)], [('osg', b2_)])
                        TTo('pool', osg[b2_][:], osg[b2_][:], gm[:, h * 256:(h + 1) * 256], ALU.mult, [('osg', b2_), ('gm', None)], [('osg', b2_)])

                    def emit_hfload(li_):
                        c_ = lat[li_] - 2
                        DMA('sp', hfl[li_ % 3][:], hfscr.ap()[h, c_], [('hf', (h, c_))], [('hfl', li_ % 3)])

                    def emit_tr(li_):
                        c_ = lat[li_] - 2
                        b2_ = li_ % 2
                        blk, sub = c_ // 4, c_ % 4
                        ab_ = blk % 2
                        for jj in range(2):
                            PET(PSb(3, 1)[:, jj * 128:(jj + 1) * 128], ab[b2_][:, jj * 128:(jj + 1) * 128], idb[:],
                                [('ab', b2_), ('idb', None)], [PN(3, 1)], inc=(jj == 1))
                        CP('act', aTs[ab_][:, :, sub * 128:(sub + 1) * 128],
                           PSb(3, 1)[:, 0:256].rearrange("p (a b) -> p a b", b=128), [PN(3, 1)], [('aTs', ab_)])
                        if sub == 0:
                            DMA('sp', aTd.ap()[:, 2 * h:2 * h + 2, blk * 512:(blk + 1) * 512], aTs[ab_][:],
                                [('aTs', ab_)], [('aTd', (h, blk))])

                    emit_sd(lat[0], 0)
                    if d == 1:
                        emit_hfload(0)
                        emit_hfload(1)
                        emit_oraw(0)
                    li = 0
                    for j, ti in enumerate(order):
                        latent = ti >= 2
                        last = (j == NT - 1)
                        cs_ = slice(ti * 128, (ti + 1) * 128)
                        if latent and li + 1 < len(lat):
                            emit_sd(lat[li + 1], li + 1)
                        pb_ = j % 2
                        if not last:
                            PE(PS(2, pb_)[:, 0:257], kt[d][:, ti, :], vt[:, ti, :], True, True,
                               [('kt%d' % d, ti), ('vt', ti), ('vt1', None)], [PN(2, pb_)])
                        if latent:
                            sl = li % 2
                            PE(PS(1, sl)[:, 0:257], SD[sl][:], vt[:, ti, :], True, j == 0,
                               [('SD', sl), ('vt', ti), ('vt1', None)], [PN(1, sl)], inc=(j == 0))
                            if j > 0:
                                PE(PS(1, sl)[:, 0:257], qT[:, cs_], Cb[:], False, True, [('qT', None), ('Cb', None)], [PN(1, sl)])
                        if not last:
                            Ecol = EL[d][:, ti, h:h + 1]
                            if j == 0:
                                ACT(Cf[:], PS(2, pb_)[:, 0:257], AF.Copy, [PN(2, pb_), ('EL', d)], [('Cf', None)], scale=Ecol)
                                ACT(Cb[:], PS(2, pb_)[:, 0:257], AF.Copy, [PN(2, pb_), ('EL', d)], [('Cb', None)], scale=Ecol)
                            else:
                                TTo('dve', tmpC[:], PS(2, pb_)[:, 0:257], Cf[:], ALU.add, [PN(2, pb_), ('Cf', None)], [('tmpC', None)])
                                ACT(Cb[:], tmpC[:], AF.Copy, [('tmpC', None), ('EL', d)], [('Cb', None)], scale=Ecol)
                                ACT(Cf[:], tmpC[:], AF.Copy, [('tmpC', None), ('EL', d)], [('Cf', None)], scale=Ecol)
                        if latent:
                            sl = li % 2
                            c = ti - 2
                            num = PS(1, sl)
                            sm = sml[li % 2]
                            smn = 'sml%d' % (li % 2)
                            e_ = EE[d][:, ti, h:h + 1]
                            if d == 1:
                                if li + 1 < len(lat):
                                    emit_oraw(li + 1)
                                if li >= 1:
                                    emit_tr(li - 1)
                                if li + 2 < len(lat):
                                    emit_hfload(li + 2)
                            TS('dve', sm[:, 0:1], num[:, 256:257], e_, None, ALU.mult, None, [PN(1, sl), ('EE', d)], [(smn, 0)])
                            TS('dve', sm[:, 7:8], sm[:, 0:1], -1.0, None, ALU.mult, None, [(smn, 0)], [(smn, 7)])
                            TTo('dve', sm[:, 1:2], sm[:, 0:1], sm[:, 7:8], ALU.max, [(smn, 0), (smn, 7)], [(smn, 1)])
                            TS('dve', sm[:, 1:2], sm[:, 1:2], 1.0, None, ALU.max, None, [(smn, 1)], [(smn, 1)])
                            S.op('dve', nc.vector.reciprocal, dict(out=sm[:, 2:3], in_=sm[:, 1:2]), [(smn, 1)], [(smn, 2)])
                            TS('dve', sm[:, 3:4], sm[:, 2:3], e_, None, ALU.mult, None, [(smn, 2), ('EE', d)], [(smn, 3)])
                            if d == 0:
                                b3 = li % 3
                                ACT(hst[b3][:], num[:, 0:256], AF.Copy, [PN(1, sl), (smn, 3)], [('hst', b3)], scale=sm[:, 3:4])
                                DMA('sp', hfscr.ap()[h, c], hst[b3][:], [('hst', b3)], [('hf', (h, c))])
                            else:
                                b3 = li % 3
                                b2 = li % 2
                                STT('dve', hs[b2][:], num[:, 0:256], sm[:, 3:4], hfl[b3][:], ALU.mult, ALU.add,
                                    [PN(1, sl), (smn, 3), ('hfl', b3)], [('hs', b2)])
                                ACT(junk2[:], hs[b2][:], AF.Square, [('hs', b2)], [('junk2', None), (smn, 4)], accum_out=sm[:, 4:5])
                                ACT(sm[:, 5:6], sm[:, 4:5], AF.Ln, [(smn, 4), ('epsb', None)], [(smn, 5)], scale=1.0 / 256, bias=epsb[:, 0:1])
                                ACT(sm[:, 6:7], sm[:, 5:6], AF.Exp, [(smn, 5)], [(smn, 6)], scale=-0.5)
                                STT('dve', ab[b2][:], hs[b2][:], sm[:, 6:7], osg[b2][:], ALU.mult, ALU.mult,
                                    [('hs', b2), (smn, 6), ('osg', b2)], [('ab', b2)])
                                if li == len(lat) - 1:
                                    emit_tr(li)
                            li += 1
            S.barrier()
            S.emit()

        if stop == 'C1':
            dump('d_aT', aTd.ap(), [128, 8, T], BF16, [('aTd', None)])
            return finish_dbg()
        with ExitStack() as st:
            def sb(name, shape, dt=F32):
                return st.enter_context(nc.sbuf_tensor('sb_' + name, shape, dt))
            cost = sb("cost", [128, T]); sint = sb("sint", [128, T])
            DMA('sp', cost[:], cosh_.ap(), [], [('cost', None)])
            DMA('sp', sint[:], sinh_.ap(), [], [('sint', None)])
            lamb = sb("lamb", [128, 256]); lt = sb("lt", [128, 8])
            gd = sb("gd", [128, D])
            DMA('sp', lamb[:], bc(dlh, 0, 256), [], [('lamb', None)])
            DMA('sp', gd[:], bc(dngh, 0, D), [], [('gd', None)])
            TS('pool', gd[:], gd[:], 1.0 - LAM_INIT, None, ALU.mult, None, [('gd', None)], [('gd', None)])
            lp = sb("lp", [128, 128])
            TTo('dve', lp[:, 0:64], lamb[:, 0:64], lamb[:, 64:128], ALU.mult, [('lamb', None)], [('lp', 0)])
            TTo('dve', lp[:, 64:128], lamb[:, 128:192], lamb[:, 192:256], ALU.mult, [('lamb', None)], [('lp', 1)])
            S.op('dve', nc.vector.reduce_sum, dict(out=lt[:, 0:1], in_=lp[:, 0:64], axis=AX.X), [('lp', 0)], [('lt', 0)])
            S.op('dve', nc.vector.reduce_sum, dict(out=lt[:, 1:2], in_=lp[:, 64:128], axis=AX.X), [('lp', 1)], [('lt', 1)])
            ACT(lt[:, 2:4], lt[:, 0:2], AF.Exp, [('lt', 0), ('lt', 1)], [('lt', 2)])
            TTo('dve', lt[:, 4:5], lt[:, 3:4], lt[:, 2:3], ALU.subtract, [('lt', 2)], [('lt', 4)])
            TS('dve', lt[:, 5:6], lt[:, 4:5], -LAM_INIT, None, ALU.add, None, [('lt', 4)], [('lt', 5)])
            neglam = lt[:, 5:6]

            wd = sb("wd", [128, 5, 1024], BF16)
            qT = sb("dqT", [128, T], BF16); kT = sb("dkT", [128, TT], BF16)
            vd = sb("vd", [128, NT, 129], BF16)
            t1 = [sb("t1_%d" % i, [128, 512]) for i in range(2)]
            t2 = [sb("t2_%d" % i, [128, 512]) for i in range(2)]
            Pex = [sb("Pex%d" % i, [128, 1024], BF16) for i in range(3)]
            es = [sb("es%d" % i, [128, 8]) for i in range(2)]
            Ocp = sb("Ocp", [128, 8 * 129]); es4 = sb("es4", [128, 24]); A4 = sb("A4", [128, 4, 128]); sq4 = sb("sq4", [128, 4, 128])
            bb4 = sb("bb4", [128, 4, 128], BF16)
            pending = []

            def flush(upto):
                keep = []
                for (trig, fn) in pending:
                    if upto is None or trig <= upto:
                        fn()
                    else:
                        keep.append((trig, fn))
                pending[:] = keep
            A1 = [sb("A1_%d" % i, [128, 128]) for i in range(2)]
            bb = [sb("bb%d" % i, [128, 128], BF16) for i in range(2)]
            bst = [sb("bst%d" % i, [128, 512], BF16) for i in range(2)]
            junk3 = sb("junk3", [128, 128], BF16)
            S.op('pool', nc.gpsimd.memset, dict(ap=vd[:, :, 128:129], constant=1.0), [], [('vd1', None)])
            cw = [sb("cw_%d" % i, [128, 8, D], BF16) for i in range(2)]
            ccnt = [0]

            def oreg(m, sub):
                r = m * 4 + sub
                if r < 3:
                    return PS(2, 0)[:, r * 129:(r + 1) * 129], PN(2, 0)
                if r < 6:
                    return PS(2, 1)[:, (r - 3) * 129:(r - 2) * 129], PN(2, 1)
                return PS(3, 0)[:, (r - 6) * 129:(r - 5) * 129], PN(3, 0)

            for h in range(8):
                for pi_, c0 in ((0, O_DQ + h * 128), (2, O_DK + h * 128), (4, O_DV + h * 128)):
                    DMA('pool', wd[:, pi_, :].rearrange("p (k n) -> p k n", n=128), wslice(c0, 128), [], [('wd', pi_)])
                for pi_ in (0, 2):
                    s4 = wd[:, pi_, :].rearrange("p (a two j) -> p a two j", two=2, j=16)
                    d4 = wd[:, pi_ + 1, :].rearrange("p (a two j) -> p a two j", two=2, j=16)
                    CP('pool', d4[:, :, 0, :], s4[:, :, 1, :], [('wd', pi_)], [('wd', pi_ + 1)])
                    CP('pool', d4[:, :, 1, :], s4[:, :, 0, :], [('wd', pi_)], [('wd', pi_ + 1)])
                def emit_vgroup(g):
                    tiles = list(range(g * 4, min(NT, g * 4 + 4)))
                    pb_ = g % 2
                    for ii, ti in enumerate(tiles):
                        for kc in range(8):
                            PE(PS(2, pb_)[:, ii * 128:(ii + 1) * 128], uT[:, kc, ti * 128:(ti + 1) * 128], wd[:, 4, kc * 128:(kc + 1) * 128],
                               kc == 0, kc == 7, [('uT', None), ('wd', 4)], [PN(2, pb_)], inc=(kc == 7 and ii == len(tiles) - 1))
                    nt_ = len(tiles)
                    CP('act', vd[:, tiles[0]:tiles[0] + nt_, 0:128], PS(2, pb_)[:, 0:nt_ * 128].rearrange("p (a b) -> p a b", b=128),
                       [PN(2, pb_)], [('vd', g)])

                vg = [0]
                cnt = 0
                for which in range(2):
                    dst = qT if which == 0 else kT
                    dn = 'dqT' if which == 0 else 'dkT'
                    off = 0 if which == 0 else TC
                    wp = 0 if which == 0 else 2
                    for blk in range(8):
                        c0 = TC + blk * 512
                        pi_ = cnt % 2
                        cnt += 1
                        for ab_ in range(2):
                            for kc in range(8):
                                PE(PS(pi_, ab_), wd[:, wp + ab_, kc * 128:(kc + 1) * 128], uT[:, kc, c0:c0 + 512], kc == 0, kc == 7,
                                   [('wd', wp + ab_), ('uT', None)], [PN(pi_, ab_)], inc=(kc == 7))
                        if vg[0] < 9 and (cnt % 2 == 1 or vg[0] < cnt // 2):
                            emit_vgroup(vg[0])
                            vg[0] += 1
                        tb_ = blk % 2
                        TTo('dve', t1[tb_][:], PS(pi_, 0), cost[:, blk * 512:(blk + 1) * 512], ALU.mult, [PN(pi_, 0), ('cost', None)], [('t1', tb_)])
                        TTo('dve', t2[tb_][:], PS(pi_, 1), sint[:, blk * 512:(blk + 1) * 512], ALU.mult, [PN(pi_, 1), ('sint', None)], [('t2', tb_)])
                        TTo('pool', dst[:, off + blk * 512:off + (blk + 1) * 512], t1[tb_][:], t2[tb_][:], ALU.add,
                            [('t1', tb_), ('t2', tb_)], [(dn, blk)])
                    if which == 0:
                        flush(None)
                for kc in range(8):
                    PE(PS(0, 0)[:, 0:256], wd[:, 2, kc * 128:(kc + 1) * 128], uT[:, kc, 0:TC], kc == 0, kc == 7,
                       [('wd', 2), ('uT', None)], [PN(0, 0)], inc=(kc == 7))
                CP('act', kT[:, 0:TC], PS(0, 0)[:, 0:256], [PN(0, 0)], [('dkT', 'c')])
                while vg[0] < 9:
                    emit_vgroup(vg[0])
                    vg[0] += 1
                for e in range(4 * h, 4 * h + 4):
                    for g in range(3):
                        cb_ = ccnt[0] % 2
                        ccnt[0] += 1
                        if g < 2:
                            src = w1h.ap()[e][:, g * D:(g + 1) * D].rearrange("(k p) n -> p k n", p=128)
                            dst = w1bd.ap()[e * 128:(e + 1) * 128, :].rearrange("p (k n) -> p k n", n=2 * D)[:, :, g * D:(g + 1) * D]
                            dn = ('w1bd', (e, g))
                        else:
                            src = w2h.ap()[e].rearrange("(k p) n -> p k n", p=128)
                            dst = w2bd.ap()[e * 128:(e + 1) * 128, :].rearrange("p (k n) -> p k n", n=D)
                            dn = ('w2bd', e)
                        DMA('pool', cw[cb_][:], src, [], [('cw', cb_)])
                        DMA('sp', dst, cw[cb_][:], [('cw', cb_)], [dn])
                for qb in range(8):
                    q0 = qb * 512

                    def emit_qk(kc, n):
                        s_ = n % 2
                        for m in range(2):
                            PE(PS(s_, m), kT[64 * m:64 * m + 64, kc * 128:(kc + 1) * 128], qT[64 * m:64 * m + 64, q0:q0 + 512], True, True,
                               [('dkT', None), ('dqT', None)], [PN(s_, m)], inc=(m == 1))
                        ACT(Pex[n % 3][:], PSUM[s_][:], AF.Exp, [PN(s_, 0), PN(s_, 1)], [('Pex', n % 3)], scale=0.125)

                    emit_qk(0, 0)
                    for kc in range(NT):
                        if kc + 1 < NT:
                            emit_qk(kc + 1, kc + 1)
                        pe_ = Pex[kc % 3]
                        for m in range(2):
                            for sub in range(4):
                                oap, on = oreg(m, sub)
                                r_ = m * 4 + sub
                                PE(oap, pe_[:, m * 512 + sub * 128:m * 512 + (sub + 1) * 128], vd[:, kc, :],
                                   kc == 0 and r_ in (0, 3, 6), kc == NT - 1 and r_ in (2, 5, 7),
                                   [('Pex', kc % 3), ('vd', None), ('vd1', None)], [on], inc=(m == 1 and sub == 3))
                        flush(kc)
                    CP('dve', Ocp[:, 0:387], PS(2, 0)[:, 0:387], [PN(2, 0)], [('Ocp', 0)])
                    CP('dve', Ocp[:, 387:774], PS(2, 1)[:, 0:387], [PN(2, 1)], [('Ocp', 1)])
                    CP('dve', Ocp[:, 774:1032], PS(3, 0)[:, 0:258], [PN(3, 0)], [('Ocp', 2)])

                    def st2(h=h):
                        for sub in range(4):
                            o0 = Ocp[:, sub * 129:(sub + 1) * 129]
                            o1 = Ocp[:, (4 + sub) * 129:(5 + sub) * 129]
                            S.op('dve', nc.vector.reciprocal, dict(out=es4[:, sub:sub + 1], in_=o0[:, 128:129]), [('Ocp', None)], [('es4', (0, sub))])
                            S.op('dve', nc.vector.reciprocal, dict(out=es4[:, 4 + sub:5 + sub], in_=o1[:, 128:129]), [('Ocp', None)], [('es4', (1, sub))])
                            TS('dve', es4[:, 8 + sub:9 + sub], es4[:, 4 + sub:5 + sub], neglam, None, ALU.mult, None, [('es4', (1, sub)), ('lt', 5)], [('es4', (2, sub))])
                            TS('dve', A4[:, sub, :], o0[:, 0:128], es4[:, sub:sub + 1], None, ALU.mult, None, [('Ocp', None), ('es4', (0, sub))], [('A4', sub)])
                            STT('dve', A4[:, sub, :], o1[:, 0:128], es4[:, 8 + sub:9 + sub], A4[:, sub, :], ALU.mult, ALU.add,
                                [('Ocp', None), ('es4', (2, sub)), ('A4', sub)], [('A4', sub)])
                            TTo('pool', sq4[:, sub, :], A4[:, sub, :], A4[:, sub, :], ALU.mult, [('A4', sub)], [('sq4', sub)])
                        S.op('dve', nc.vector.reduce_sum, dict(out=es4[:, 12:16], in_=sq4[:], axis=AX.X), [('sq4', None)], [('es4', 3)])

                    def st3():
                        ACT(es4[:, 16:20], es4[:, 12:16], AF.Ln, [('es4', 3), ('epsb', None)], [('es4', 4)], scale=1.0 / 128, bias=epsb[:, 0:1])
                        ACT(es4[:, 20:24], es4[:, 16:20], AF.Exp, [('es4', 4)], [('es4', 5)], scale=-0.5)

                    def st4(h=h):
                        for sub in range(4):
                            STT('dve', bb4[:, sub, :], A4[:, sub, :], es4[:, 20 + sub:21 + sub], gd[:, h * 128:(h + 1) * 128], ALU.mult, ALU.mult,
                                [('A4', sub), ('es4', 5), ('gd', None)], [('bb4', sub)])

                    def st5():
                        for sub in range(4):
                            PET(PSb(3, 1)[:, sub * 128:(sub + 1) * 128], bb4[:, sub, :], idb[:], [('bb4', sub), ('idb', None)], [PN(3, 1)], inc=(sub == 3))

                    def st6(h=h, qb=qb, q0=q0):
                        sb_ = qb % 2
                        CP('dve', bst[sb_][:], PSb(3, 1)[:, 0:512], [PN(3, 1)], [('bst', sb_)])
                        DMA('sp', bTd.ap()[:, h, q0:q0 + 512], bst[sb_][:], [('bst', sb_)], [('bTd', (h, qb))])

                    pending.extend([(0, st2), (4, st3), (5, st4), (8, st5), (9, st6)])

            flush(None)
            S.barrier()
            S.emit()
        with ExitStack() as st:
            def sb(name, shape, dt=F32):
                return st.enter_context(nc.sbuf_tensor('sb_' + name, shape, dt))
            wgc = [sb("wgc%d" % i, [128, 8, 128], BF16) for i in range(2)]
            sgs = [sb("sgs%d" % i, [128, T], BF16) for i in range(2)]
            for cc in range(16):
                b_ = cc % 2
                DMA('pool', wgc[b_][:], wslice(O_GA + cc * 128, 128), [], [('wgc', b_)])
                for blk in range(8):
                    pi_, pb_ = (blk // 2) % 2, blk % 2
                    for kc in range(8):
                        PE(PS(pi_, pb_), wgc[b_][:, kc, :], uT[:, kc, TC + blk * 512:TC + (blk + 1) * 512], kc == 0, kc == 7,
                           [('wgc', b_), ('uT', None)], [PN(pi_, pb_)], inc=(kc == 7))
                    ACT(sgs[b_][:, blk * 512:(blk + 1) * 512], PS(pi_, pb_), AF.Sigmoid, [PN(pi_, pb_)], [('sgs', b_)])
                DMA('sp', sgd.ap()[:, cc, :], sgs[b_][:], [('sgs', b_)], [('sgd', cc)])
            S.barrier()
            S.emit()
        if stop is None:
            ust.close()

        if stop == 'C2':
            dump('d_bT', bTd.ap(), [128, 8, T], BF16, [('bTd', None)])
            dump('d_sg', sgd.ap(), [128, 16, T], BF16, [('sgd', None)])
            return finish_dbg()
        L = sbg("L", [128, 32, NE])
        with ExitStack() as st:
            G1 = st.enter_context(nc.sbuf_tensor("sb_G1", [128, D], F32))
            DMA('sp', G1[:], bc(modscr, 16 * 128, D), [('modscr', None)], [('G1', None)])
            def sb(name, shape, dt=F32):
                return st.enter_context(nc.sbuf_tensor('sb_' + name, shape, dt))
            wa = sb("wa", [128, 8, D], BF16); wb = sb("wb", [128, 8, D], BF16); wo = sb("wo", [128, 8, D], BF16)
            DMA('pool', wa[:], wah.ap().rearrange("(k p) n -> p k n", p=128), [], [('wa', None)])
            DMA('pool', wb[:], wbh.ap().rearrange("(k p) n -> p k n", p=128), [], [('wb', None)])
            DMA('pool', wo[:], woh.ap().rearrange("(k p) n -> p k n", p=128), [], [('wo', None)])
            rw = sb("rw", [128, 8, NE]); rbb = sb("rbb", [128, NE])
            DMA('sp', rw[:], rwh.ap().rearrange("(k p) n -> p k n", p=128), [], [('rw', None)])
            DMA('sp', rbb[:], bc(rbh, 0, NE), [], [('rbb', None)])
            aTb = [sb("aTb%d" % i, [128, 8, 512], BF16) for i in range(1)] * 2
            bTb = [sb("bTb%d" % i, [128, 8, 512], BF16) for i in range(1)] * 2
            sAb = [sb("sAb%d" % i, [128, 8, 512], BF16) for i in range(1)] * 2
            sBb = [sb("sBb%d" % i, [128, 8, 512], BF16) for i in range(1)] * 2
            yT = [sb("yT%d" % i, [128, 8, 512], BF16) for i in range(1)] * 2
            t1 = [sb("m1_%d" % i, [128, 512]) for i in range(2)]
            t2 = [sb("m2_%d" % i, [128, 512]) for i in range(2)]
            xin = [sb("xi%d" % i, [128, D]) for i in range(2)]
            x1t = [sb("x1t%d" % i, [128, D]) for i in range(2)]
            xn2 = [sb("xn2_%d" % i, [128, D]) for i in range(2)]
            ssb = [sb("ss2_%d" % i, [128, 4]) for i in range(2)]
            u2f = [sb("u2f%d" % i, [128, 8, 128]) for i in range(2)]
            junk = sb("junk4", [128, D], BF16)
            MUL2t = sb("MUL2t", [128, D]); ADD2t = sb("ADD2t", [128, D]); N2Gt = sb("N2Gt", [128, D])
            u2tmp = sb("u2tmp", [128, D]); u2tb = [sb("u2tb%d" % i, [128, D], BF16) for i in range(2)]
            DMA('sp', ADD2t[:], bc(modscr, 3072, D), [('modscr', None)], [('ADD2t', None)])
            DMA('sp', MUL2t[:], bc(modscr, 4096, D), [('modscr', None)], [('MUL2t', None)])
            DMA('sp', N2Gt[:], bc(vecsh, 56 * 128, D), [], [('N2Gt', None)])
            TS('pool', MUL2t[:], MUL2t[:], 1.0, None, ALU.add, None, [('MUL2t', None)], [('MUL2t', None)])
            TTo('pool', MUL2t[:], MUL2t[:], N2Gt[:], ALU.mult, [('MUL2t', None), ('N2Gt', None)], [('MUL2t', None)])
            for tb in range(8):
                b_ = 0
                tsl = slice(tb * 512, (tb + 1) * 512)
                DMA('sp', aTb[b_][:], aTd.ap()[:, :, tsl], [('aTd', None)], [('aTb', b_)])
                DMA('sp', bTb[b_][:], bTd.ap()[:, :, tsl], [('bTd', None)], [('bTb', b_)])
                DMA('sp', sAb[b_][:], sgd.ap()[:, 0:8, tsl], [('sgd', None)], [('sAb', b_)])
                DMA('sp', sBb[b_][:], sgd.ap()[:, 8:16, tsl], [('sgd', None)], [('sBb', b_)])
                for cc in range(8):
                    pi_ = cc % 2
                    for (pb_, w_, w_n, src, srcn) in ((0, wa, 'wa', aTb, 'aTb'), (1, wb, 'wb', bTb, 'bTb')):
                        for kc in range(8):
                            PE(PS(pi_, pb_), w_[:, kc, cc * 128:(cc + 1) * 128], src[b_][:, kc, :], kc == 0, kc == 7,
                               [(w_n, None), (srcn, b_)], [PN(pi_, pb_)], inc=(kc == 7))
                    TTo('dve', t1[pi_][:], PS(pi_, 0), sAb[b_][:, cc, :], ALU.mult, [PN(pi_, 0), ('sAb', b_)], [('m1', pi_)])
                    TTo('dve', t2[pi_][:], PS(pi_, 1), sBb[b_][:, cc, :], ALU.mult, [PN(pi_, 1), ('sBb', b_)], [('m2', pi_)])
                    TTo('pool', yT[b_][:, cc, :], t1[pi_][:], t2[pi_][:], ALU.add, [('m1', pi_), ('m2', pi_)], [('yT', (b_, cc))])
                for sub in range(4):
                    tt = tb * 4 + sub
                    xb_ = tt % 2
                    DMA('sp', xin[xb_][:], xh.ap()[tt * 128:(tt + 1) * 128, :], [], [('xi', xb_)])
                    for half in range(2):
                        for cc in range(8):
                            PE(PS(2, half), yT[b_][:, cc, sub * 128:(sub + 1) * 128], wo[:, cc, half * 512:(half + 1) * 512], cc == 0, cc == 7,
                               [('yT', (b_, cc)), ('wo', None)], [PN(2, half)], inc=(cc == 7))
                        hs_ = slice(half * 512, (half + 1) * 512)
                        TTo('dve', x1t[xb_][:, hs_], PS(2, half), G1[:, hs_], ALU.mult, [PN(2, half), ('G1', None)], [('x1t', (xb_, half))])
                        TTo('pool', x1t[xb_][:, hs_], x1t[xb_][:, hs_], xin[xb_][:, hs_], ALU.add,
                            [('x1t', (xb_, half)), ('xi', xb_)], [('x1t', (xb_, half))])
                    DMA('sp', x1d.ap()[tt * 128:(tt + 1) * 128, :], x1t[xb_][:], [('x1t', (xb_, 0)), ('x1t', (xb_, 1))], [('x1d', tt)])
                    sn = 'ss2_%d' % xb_
                    ACT(junk[:], x1t[xb_][:], AF.Square, [('x1t', (xb_, 0)), ('x1t', (xb_, 1))], [('junk4', None), (sn, 0)], accum_out=ssb[xb_][:, 0:1])
                    ACT(ssb[xb_][:, 1:2], ssb[xb_][:, 0:1], AF.Ln, [(sn, 0), ('epsb', None)], [(sn, 1)], scale=1.0 / D, bias=epsb[:, 0:1])
                    ACT(ssb[xb_][:, 2:3], ssb[xb_][:, 1:2], AF.Exp, [(sn, 1)], [(sn, 2)], scale=-0.5)
                    TS('dve', xn2[xb_][:], x1t[xb_][:], ssb[xb_][:, 2:3], None, ALU.mult, None,
                       [('x1t', (xb_, 0)), ('x1t', (xb_, 1)), (sn, 2)], [('xn2', xb_)])
                    for g in range(2):
                        for j in range(4):
                            kc = g * 4 + j
                            PET(PS(3, g)[:, j * 128:(j + 1) * 128], xn2[xb_][:, kc * 128:(kc + 1) * 128], idf[:],
                                [('xn2', xb_), ('idf', None)], [PN(3, g)], inc=(j == 3))
                        for j in range(4):
                            kc = g * 4 + j
                            o = u2f[xb_][:, kc, :]
                            i_ = PS(3, g)[:, j * 128:(j + 1) * 128]
                            if j % 2 == 0:
                                TS('dve', o, i_, prm[:, 4, kc:kc + 1], prm[:, 5, kc:kc + 1], ALU.mult, ALU.add,
                                   [PN(3, g), ('prm', 4), ('prm', 5)], [('u2f', (xb_, kc))])
                            else:
                                ACT(o, i_, AF.Identity, [PN(3, g), ('prm', 4), ('prm', 5)], [('u2f', (xb_, kc))],
                                    scale=prm[:, 4, kc:kc + 1], bias=prm[:, 5, kc:kc + 1])
                    TTo('pool', u2tmp[:], xn2[xb_][:], MUL2t[:], ALU.mult, [('xn2', xb_), ('MUL2t', None)], [('u2tmp', None)])
                    TTo('pool', u2tb[xb_][:], u2tmp[:], ADD2t[:], ALU.add, [('u2tmp', None), ('ADD2t', None)], [('u2tb', xb_)])
                    DMA('sp', u2tokd.ap()[tt * 128:(tt + 1) * 128, :], u2tb[xb_][:], [('u2tb', xb_)], [('u2tokd', tt)])
                    for kc in range(8):
                        PE(PS(3, 1)[:, 0:NE], u2f[xb_][:, kc, :], rw[:, kc, :], kc == 0, kc == 7,
                           [('u2f', (xb_, kc)), ('rw', None)], [PN(3, 1)], inc=(kc == 7))
                    TTo('dve', L[:, tt, :], PS(3, 1)[:, 0:NE], rbb[:], ALU.add, [PN(3, 1), ('rbb', None)], [('L', tt)])
            S.barrier()
            S.emit()

        if stop == 'D':
            dump('d_x1', x1d.ap(), [T, D], F32, [('x1d', None)])
            dump('d_L', L[:], [128, 32, NE], F32, [('L', None)])
            return finish_dbg()
        IOA = bass.IndirectOffsetOnAxis
        with ExitStack() as st:
            def sb(name, shape, dt=F32):
                return st.enter_context(nc.sbuf_tensor('sb_' + name, shape, dt))
            G2 = sb("G2", [128, D])
            DMA('sp', G2[:], bc(modscr, 40 * 128, D), [('modscr', None)], [('G2', None)])
            stri = sb("stri", [128, 128]); thrB = sb("thrB", [128, NBLK]); pidx = sb("pidx", [128, 1])
            DMA('sp', stri[:], strih.ap(), [], [('stri', None)])
            DMA('sp', thrB[:], thrBh.ap(), [], [('thrB', None)])
            DMA('sp', pidx[:], pidxh.ap(), [], [('pidx', None)])
            gate = sb("gate", [128, 32, NE]); MK = sb("MK", [128, 32, NE]); POS = sb("POS", [128, 32, NE])
            m8 = [sb("m8_%d" % i, [128, 16]) for i in range(2)]
            ex = [sb("ex%d" % i, [128, NE]) for i in range(2)]
            b1in = sb("b1in", [128, 4, 128]); b1T = sb("b1T", [128, 512]); b2s = sb("b2s", [NE, D]); fg = sb("fg", [128, D])
            DMA('sp', b1in[:], b1h.ap().rearrange("(a p) n -> p a n", p=128), [], [('b1in', None)])
            DMA('sp', b2s[:], b2h.ap(), [], [('b2s', None)])
            DMA('sp', fg[:], bc(fngh, 0, D), [], [('fg', None)])
            for a_ in range(4):
                PET(PS(0, 0)[:, a_ * 128:(a_ + 1) * 128], b1in[:, a_, :], idf[:], [('b1in', None), ('idf', None)], [PN(0, 0)], inc=(a_ == 3))
            CP('dve', b1T[:], PS(0, 0), [PN(0, 0)], [('b1T', None)])
            b1v = b1T[:].rearrange("p (e j) -> p e j", j=16)
            TS('dve', b1v[:, :, 8:16], b1v[:, :, 8:16], 1.0, None, ALU.add, None, [('b1T', None)], [('b1T', None)])
            DMA('sp', b1Td.ap().rearrange("(e p) j -> p e j", p=128), b1T[:].rearrange("p (e j) -> p e j", j=16), [('b1T', None)], [('b1Td', None)])
            base = sb("base", [128, NE])
            S.op('dve', nc.vector.memset, dict(ap=base[:], constant=0.0), [], [('base', None)])
            for tt in range(32):
                b_ = tt % 2
                mn = 'm8_%d' % b_
                S.op('dve', nc.vector.max, dict(out=m8[b_][:, 0:8], in_=L[:, tt, :]), [('L', tt)], [(mn, 0)])
                TS('dve', MK[:, tt, :], L[:, tt, :], m8[b_][:, 3:4], None, ALU.is_ge, None, [('L', tt), (mn, 0)], [('MK', tt)])
                TS('dve', m8[b_][:, 8:9], m8[b_][:, 0:1], -1.0, None, ALU.mult, None, [(mn, 0)], [(mn, 1)])
                ACT(ex[b_][:], L[:, tt, :], AF.Exp, [('L', tt), (mn, 1)], [('ex', b_)], bias=m8[b_][:, 8:9])
                TTo('dve', ex[b_][:], ex[b_][:], MK[:, tt, :], ALU.mult, [('ex', b_), ('MK', tt)], [('ex', b_)])
                S.op('dve', nc.vector.reduce_sum, dict(out=m8[b_][:, 9:10], in_=ex[b_][:], axis=AX.X), [('ex', b_)], [(mn, 2)])
                S.op('dve', nc.vector.reciprocal, dict(out=m8[b_][:, 10:11], in_=m8[b_][:, 9:10]), [(mn, 2)], [(mn, 3)])
                TS('dve', gate[:, tt, :], ex[b_][:], m8[b_][:, 10:11], None, ALU.mult, None, [('ex', b_), (mn, 3)], [('gate', tt)])
                PE(PS(0, b_)[:, 0:NE], stri[:], MK[:, tt, :], True, True, [('stri', None), ('MK', tt)], [PN(0, b_)])
                PE(PS(0, b_)[:, NE:2 * NE], ones[:], MK[:, tt, :], True, True, [('ones', None), ('MK', tt)], [PN(0, b_)])
                TTo('dve', POS[:, tt, :], PS(0, b_)[:, 0:NE], base[:], ALU.add, [PN(0, b_), ('base', None)], [('POS', tt)])
                TTo('dve', base[:], base[:], PS(0, b_)[:, NE:2 * NE], ALU.add, [PN(0, b_), ('base', None)], [('base', None)])
            nb = sb("nb", [128, NE]); cA = sb("cA", [128, NE]); cB = sb("cB", [128, NE]); pst = sb("pst", [128, NE])
            S.op('dve', nc.vector.memset, dict(ap=nb[:], constant=0.0), [], [('nb', None)])
            for k in range(T // BLK):
                STT('dve', nb[:], base[:], float(BLK) * k, nb[:], ALU.is_gt, ALU.add, [('base', None), ('nb', None)], [('nb', None)])
            TS('dve', nb[:], nb[:], float(BLK), None, ALU.mult, None, [('nb', None)], [('nb', None)])
            CP('dve', cA[:], nb[:], [('nb', None)], [('cA', None)])
            cur, oth, cn, on = cA, cB, 'cA', 'cB'
            for sh in (1, 2, 4, 8, 16):
                CP('dve', oth[:, 0:sh], cur[:, 0:sh], [(cn, None)], [(on, 0)])
                TTo('dve', oth[:, sh:NE], cur[:, sh:NE], cur[:, 0:NE - sh], ALU.add, [(cn, None)], [(on, 1)])
                cur, oth, cn, on = oth, cur, on, cn
            pend, pendn = cur, cn
            TTo('dve', pst[:], pend[:], nb[:], ALU.subtract, [(pendn, None), ('nb', None)], [('pst', None)])
            D4 = sb("D4", [128, 32, 4], mybir.dt.int32); g4 = sb("g4", [128, 32, 4])
            key = [sb("key%d" % i, [128, NE]) for i in range(2)]
            oh = [sb("oh%d" % i, [128, NE]) for i in range(2)]
            k8 = [sb("k8_%d" % i, [128, 8]) for i in range(2)]
            for tt in range(32):
                b_ = tt % 2
                kn, k8n = 'key%d' % b_, 'k8_%d' % b_
                TTo('dve', POS[:, tt, :], POS[:, tt, :], pst[:], ALU.add, [('POS', tt), ('pst', None)], [('POS', tt)])
                STT('dve', key[b_][:], POS[:, tt, :], 1.0, MK[:, tt, :], ALU.add, ALU.mult, [('POS', tt), ('MK', tt)], [(kn, None)])
                S.op('dve', nc.vector.max, dict(out=k8[b_][:], in_=key[b_][:]), [(kn, None)], [(k8n, None)])
                TS('dve', D4[:, tt, :], k8[b_][:, 0:4], -1.0, None, ALU.add, None, [(k8n, None)], [('D4', tt)])
                for j in range(4):
                    on_ = 'oh%d' % (j % 2)
                    TS('dve', oh[j % 2][:], key[b_][:], k8[b_][:, j:j + 1], None, ALU.is_equal, None, [(kn, None), (k8n, None)], [(on_, None)])
                    TTo('dve', oh[j % 2][:], oh[j % 2][:], gate[:, tt, :], ALU.mult, [(on_, None), ('gate', tt)], [(on_, None)])
                    S.op('dve', nc.vector.reduce_sum, dict(out=g4[:, tt, j:j + 1], in_=oh[j % 2][:], axis=AX.X), [(on_, None)], [('g4', (tt, j))])
            be = sb("be", [128, NBLK]); OFFS = sb("OFFS", [128, NBLK], mybir.dt.int32)
            S.op('dve', nc.vector.memset, dict(ap=be[:], constant=0.0), [], [('be', None)])
            for e in range(NE):
                STT('dve', be[:], thrB[:], pend[:, e:e + 1], be[:], ALU.is_ge, ALU.add, [('thrB', None), (pendn, None), ('be', None)], [('be', None)])
            TS('dve', be[:], be[:], float(NE - 1), 128.0, ALU.min, ALU.mult, [('be', None)], [('be', None)])
            TS('dve', OFFS[:], be[:], pidx[:, 0:1], None, ALU.add, None, [('be', None), ('pidx', None)], [('OFFS', None)])
            ut = [sb("ut%d" % i, [128, D], BF16) for i in range(2)]
            for tt in range(32):
                b_ = tt % 2
                DMA('sp', ut[b_][:], u2tokd.ap()[tt * 128:(tt + 1) * 128, :], [('u2tokd', tt)], [('ut', b_)])
                for j in range(4):
                    S.dma('pool', dict(out=Xg.ap(), out_offset=IOA(ap=D4[:, tt, j:j + 1], axis=0), in_=ut[b_][:], in_offset=None),
                          [('ut', b_), ('D4', tt)], [('Xg', (tt, j))], method=nc.gpsimd.indirect_dma_start)

            with ExitStack() as st2:
                def sb2(name, shape, dt=F32):
                    return st2.enter_context(nc.sbuf_tensor('sb_' + name, shape, dt))
                w1s = [sb2("w1s%d" % i, [128, 8 * 2 * D], BF16) for i in range(2)]
                w2s = [sb2("w2s%d" % i, [128, 8 * D], BF16) for i in range(2)]
                b1g = [sb2("b1g%d" % i, [128, 16]) for i in range(2)]
                xg = [sb2("xg%d" % i, [128, D], BF16) for i in range(4)]
                xT = [sb2("xT%d" % i, [128, 8, BLK], BF16) for i in range(2)]
                actT = [sb2("actT%d" % i, [128, 8, BLK], BF16) for i in range(2)]
                hg = [sb2("hg%d" % i, [128, BLK]) for i in range(2)]
                hl = [sb2("hl%d" % i, [128, BLK]) for i in range(2)]
                yst = [sb2("yst%d" % i, [128, D]) for i in range(2)]

                def load_w(b):
                    wb_ = b % 2
                    off = IOA(ap=OFFS[:, b:b + 1], axis=0)
                    for (dst, dn, src, sn) in ((w1s, 'w1s', w1bd, 'w1bd'), (w2s, 'w2s', w2bd, 'w2bd'), (b1g, 'b1g', b1Td, 'b1Td')):
                        S.dma('pool', dict(out=dst[wb_][:], out_offset=None, in_=src.ap(), in_offset=off),
                              [('OFFS', None), (sn, None)], [(dn, wb_)], method=nc.gpsimd.indirect_dma_start)

                def load_x(b):
                    for sub in range(2):
                        t_ = 2 * b + sub
                        xi_ = 2 * (b % 2) + sub
                        DMA('sp', xg[xi_][:], Xg.ap()[t_ * 128:(t_ + 1) * 128, :], [('Xg', None)], [('xg', xi_)])

                load_w(0)
                ycnt = 0
                for b in range(NBLK):
                    wb_ = b % 2
                    if b + 1 < NBLK:
                        load_w(b + 1)
                    if b == 0:
                        load_x(0)
                    if b + 1 < NBLK:
                        load_x(b + 1)
                    for sub in range(2):
                        t_ = 2 * b + sub
                        xi_ = 2 * (b % 2) + sub
                        for kc in range(8):
                            PET(PSb(3, 0)[:, kc * 128:(kc + 1) * 128], xg[xi_][:, kc * 128:(kc + 1) * 128], idb[:], [('xg', xi_), ('idb', None)], [PN(3, 0)], inc=(kc == 7))
                        CP('act', xT[wb_][:, :, sub * 128:(sub + 1) * 128], PSb(3, 0).rearrange("p (k n) -> p k n", n=128), [PN(3, 0)], [('xT', (wb_, sub))])
                    for cp in range(8):
                        pi_ = cp % 2
                        for gl in range(2):
                            for kc in range(8):
                                c0 = kc * 2 * D + gl * D + cp * 128
                                PE(PS(pi_, gl)[:, 0:BLK], w1s[wb_][:, c0:c0 + 128], xT[wb_][:, kc, :], kc == 0, kc == 7,
                                   [('w1s', wb_), ('xT', (wb_, 0)), ('xT', (wb_, 1))], [PN(pi_, gl)], inc=(kc == 7))
                        bg = b1g[wb_][:, cp:cp + 1]
                        bl = b1g[wb_][:, 8 + cp:9 + cp]
                        TS('dve', hg[pi_][:], PS(pi_, 0)[:, 0:BLK], bg, 7.0, ALU.add, ALU.min, [PN(pi_, 0), ('b1g', wb_)], [('hg', pi_)])
                        ACT(hg[pi_][:], hg[pi_][:], AF.Silu, [('hg', pi_)], [('hg', pi_)], scale=1.702)
                        TS('dve', hl[pi_][:], PS(pi_, 1)[:, 0:BLK], bl, 8.0, ALU.add, ALU.min, [PN(pi_, 1), ('b1g', wb_)], [('hl', pi_)])
                        STT('dve', actT[wb_][:, cp, :], hl[pi_][:], -6.0, hg[pi_][:], ALU.max, ALU.mult, [('hg', pi_), ('hl', pi_)], [('actT', (wb_, cp))])
                    for sub in range(2):
                        t_ = 2 * b + sub
                        ys_ = ycnt % 2
                        ycnt += 1
                        for half in range(2):
                            for cp in range(8):
                                c0 = cp * D + half * 512
                                PE(PS(2, half), actT[wb_][:, cp, sub * 128:(sub + 1) * 128], w2s[wb_][:, c0:c0 + 512], cp == 0, cp == 7,
                                   [('actT', (wb_, cp)), ('w2s', wb_)], [PN(2, half)], inc=(cp == 7))
                            hs_ = slice(half * 512, (half + 1) * 512)
                            if half == 0:
                                ACT(yst[ys_][:, hs_], PS(2, half), AF.Copy, [PN(2, half)], [('yst', (ys_, half))], scale=1.0 / 1.702)
                            else:
                                TS('dve', yst[ys_][:, hs_], PS(2, half), 1.0 / 1.702, None, ALU.mult, None, [PN(2, half)], [('yst', (ys_, half))])
                        DMA('sp', Yg.ap()[t_ * 128:(t_ + 1) * 128, :], yst[ys_][:], [('yst', (ys_, 0)), ('yst', (ys_, 1))], [('Yg', t_)])


                S.barrier()
                S.emit()

            yg = [[sb("yg%d_%d" % (i, j), [128, D]) for j in range(4)] for i in range(2)]
            Yacc = [sb("Yacc%d" % i, [128, D]) for i in range(2)]; gTt = [sb("gTt%d" % i, [NE, 128]) for i in range(2)]
            x1l = [sb("x1l%d" % i, [128, D]) for i in range(2)]; ot = [sb("ot%d" % i, [128, D]) for i in range(2)]
            ssf = [sb("ssf%d" % i, [128, 4]) for i in range(2)]
            for tt in range(32):
                b_ = tt % 2
                ya, x1, o_, sf, gt = Yacc[b_], x1l[b_], ot[b_], ssf[b_], gTt[b_]
                yn, xn_, on_, sn, gn = ('Yacc', b_), ('x1l', b_), ('ot', b_), 'ssf%d' % b_, ('gTt', b_)
                for j in range(4):
                    S.dma('pool', dict(out=yg[b_][j][:], out_offset=None, in_=Yg.ap(), in_offset=IOA(ap=D4[:, tt, j:j + 1], axis=0)),
                          [('D4', tt), ('Yg', None)], [('yg', (b_, j))], method=nc.gpsimd.indirect_dma_start)
                DMA('sp', x1[:], x1d.ap()[tt * 128:(tt + 1) * 128, :], [('x1d', tt)], [xn_])
                TS('dve', ya[:], yg[b_][0][:], g4[:, tt, 0:1], None, ALU.mult, None, [('yg', (b_, 0)), ('g4', (tt, 0))], [yn])
                for j in range(1, 4):
                    STT('dve', ya[:], yg[b_][j][:], g4[:, tt, j:j + 1], ya[:], ALU.mult, ALU.add,
                        [('yg', (b_, j)), ('g4', (tt, j)), yn], [yn])
                PET(PS(3, b_)[0:NE, 0:128], gate[:, tt, :], idf[:], [('gate', tt), ('idf', None)], [PN(3, b_)])
                CP('act', gt[:], PS(3, b_)[0:NE, 0:128], [PN(3, b_)], [gn])
                for half in range(2):
                    hs_ = slice(half * 512, (half + 1) * 512)
                    PE(PS(b_, half), gt[:], b2s[:, hs_], True, True, [gn, ('b2s', None)], [PN(b_, half)])
                    TTo('dve', ya[:, hs_], ya[:, hs_], PS(b_, half), ALU.add, [PN(b_, half), yn], [yn])
                TTo('dve', ya[:], ya[:], G2[:], ALU.mult, [yn, ('G2', None)], [yn])
                TTo('pool', x1[:], x1[:], ya[:], ALU.add, [xn_, yn], [xn_])
                ACT(o_[:], x1[:], AF.Square, [xn_], [on_, (sn, 0)], accum_out=sf[:, 0:1])
                ACT(sf[:, 1:2], sf[:, 0:1], AF.Ln, [(sn, 0), ('epsb', None)], [(sn, 1)], scale=1.0 / D, bias=epsb[:, 0:1])
                ACT(sf[:, 2:3], sf[:, 1:2], AF.Exp, [(sn, 1)], [(sn, 2)], scale=-0.5)
                STT('dve', o_[:], x1[:], sf[:, 2:3], fg[:], ALU.mult, ALU.mult, [xn_, (sn, 2), ('fg', None)], [on_])
                DMA('sp', yh.ap()[tt * 128:(tt + 1) * 128, :], o_[:], [on_], [('y', tt)])
            S.barrier()
            S.emit()
        print("ninstr", S.ninstr)
    return nc


def _consts():
    idf = np.eye(128, dtype=np.float32)
    s = np.arange(128)
    triu = (s[:, None] <= s[None, :]).astype(np.float32)
    tril = (s[:, None] >= s[None, :]).astype(np.float32)
    ones = np.ones((128, 128), np.float32)
    t = np.arange(T)
    row = (t // 64).astype(np.float64)
    col = (t % 64).astype(np.float64)
    inv = 10000.0 ** (-np.arange(16, dtype=np.float64) / 16.0)
    inv32 = inv.astype(np.float32).astype(np.float64)
    cost = np.zeros((128, T), np.float32)
    sint = np.zeros((128, T), np.float32)
    for p in range(128):
        d = p % 64
        pos = row if d < 32 else col
        j = d % 16
        ang = (pos.astype(np.float32) * np.float32(inv32[j])).astype(np.float32)
        first = (d % 32) < 16
        cost[p] = np.cos(ang)
        sint[p] = -np.sin(ang) if first else np.sin(ang)
    stri = (s[:, None] < s[None, :]).astype(np.float32)
    thrB = np.tile((float(BLK) * np.arange(NBLK, dtype=np.float32))[None, :], (128, 1))
    pidx = np.arange(128, dtype=np.float32).reshape(128, 1)
    return dict(idf=idf, idb=idf.astype(ml_dtypes.bfloat16), triu=triu, tril=tril, ones=ones, cost=cost, sint=sint,
                stri=stri, thrB=np.ascontiguousarray(thrB), pidx=pidx)


def _in_maps(x, c, ctx, c_ctx, ada_w, ada_b, norm1_g, norm2_g, w_in, mlstm_gate_b, mlstm_norm_g,
             diff_lambda, diff_norm_g, w_branch_a, w_branch_b, w_out, router_w, router_b,
             exp_w1, exp_b1, exp_w2, exp_b2, final_norm_g):
    f = lambda a: np.ascontiguousarray(np.asarray(a, dtype=np.float32))
    cons = _consts()
    vecs = np.concatenate([f(ada_b)[0].reshape(48, 128), f(norm1_g)[0].reshape(8, 128), f(norm2_g)[0].reshape(8, 128)], axis=0)
    shared = dict(
        ada_w=f(ada_w)[0], vecs=f(vecs), b1r=f(exp_b1)[0].reshape(512, 128), w_in=f(w_in)[0], gate_b=f(mlstm_gate_b)[0],
        mng=f(mlstm_norm_g)[0].reshape(D), dng=f(diff_norm_g)[0].reshape(D), fng=f(final_norm_g), dlam=f(diff_lambda)[0].reshape(256),
        w_a=f(w_branch_a)[0], w_b=f(w_branch_b)[0], w_o=f(w_out)[0], rw=f(router_w)[0], rb=f(router_b)[0],
        w1=f(exp_w1)[0], w2=f(exp_w2)[0], b2=f(exp_b2)[0], **cons)
    maps = []
    xf, cf, ctxf, ccf = f(x), f(c), f(ctx), f(c_ctx)
    for b in range(8):
        m = dict(shared)
        m["x"] = xf[b]
        m["ctx"] = ctxf[b]
        m["cvec"] = np.ascontiguousarray(np.stack([cf[b], ccf], axis=1))
        maps.append(m)
    return maps


def kernel(**inputs):
    maps = _in_maps(**inputs)
    nc = build_nc()
    res = run_bass_kernel_spmd(nc, maps, core_ids=list(range(8)))
    return np.stack([np.asarray(r["y"], dtype=np.float32) for r in res.results], axis=0)
```

```python
import math
import os
from contextlib import ExitStack

import ml_dtypes
import numpy as np

import concourse.bass as bass
import concourse.mybir as mybir
from concourse.bass_utils import run_bass_kernel_spmd

F32 = mybir.dt.float32
BF16 = mybir.dt.bfloat16
ALU = mybir.AluOpType
AF = mybir.ActivationFunctionType
AX = mybir.AxisListType

D = 1024
T = 4096
TC = 256
TT = T + TC
NT = TT // 128
NE = 32
BLK = 256
NBLK = 96
NBUF = NBLK * BLK
EPS = 1e-6
LAM_INIT = 0.8 - 0.6 * math.exp(0.0)
O_MQ, O_MK, O_MV, O_MG, O_MO, O_DQ, O_DK, O_DV, O_GA, O_GB = 0, 512, 1024, 2048, 2064, 3088, 4112, 5136, 6160, 7184
INC = 8208

ENG_ATTR = {'pe': 'tensor', 'act': 'scalar', 'dve': 'vector', 'pool': 'gpsimd', 'sp': 'sync'}
NDMA_SEMS = 8
NPOOL_SEMS = 4


class Sched:
    def __init__(self, nc, stack):
        self.nc = nc
        self.prog = {e: [] for e in ENG_ATTR}
        self.sem = {}
        for e in ENG_ATTR:
            self.sem[e] = stack.enter_context(nc.semaphore('s_' + e))
        self.dq = {}
        for q in ('sp', 'pool'):
            for i in range(NDMA_SEMS):
                k = ('dma', q, i)
                self.sem[k] = stack.enter_context(nc.semaphore('d_%s%d' % (q, i)))
            self.dq[q] = 0
        self.count = {k: 0 for k in self.sem}
        self.seen = {e: {} for e in ENG_ATTR}
        self.state = {}
        self.ninstr = 0

    @staticmethod
    def _ov(a, b):
        return a is None or b is None or a == b

    def _collect(self, eng, reads, writes, is_dma):
        need = {}

        def add(ev, kind):
            if ev is None:
                return
            k, v = ev
            if (not is_dma) and k == eng:
                if kind == 'rar' or (eng == 'pe' and kind != 'raw'):
                    return
            if need.get(k, 0) < v:
                need[k] = v

        for (n, s) in reads:
            for slot, st in self.state.get(n, {}).items():
                if self._ov(slot, s):
                    add(st[0], 'raw')
                    if n.startswith('PS'):
                        for r in st[1]:
                            add(r, 'rar')
        for (n, s) in writes:
            for slot, st in self.state.get(n, {}).items():
                if self._ov(slot, s):
                    add(st[0], 'waw')
                    for r in st[1]:
                        add(r, 'war')
        return need

    def _emit_waits(self, eng, need):
        seen = self.seen[eng]
        for k, v in need.items():
            if seen.get(k, 0) >= v:
                continue
            seen[k] = v
            self.prog[eng].append(('wait', self.sem[k], v))

    def _mark(self, ev, reads, writes):
        for (n, s) in writes:
            d = self.state.setdefault(n, {})
            if s is None:
                d.clear()
                d[None] = [ev, []]
            else:
                d[s] = [ev, []]
        for (n, s) in reads:
            d = self.state.setdefault(n, {})
            st = d.setdefault(s, [None, []])
            st[1] = [r for r in st[1] if r[0] != ev[0]] + [ev]

    def op(self, eng, method, kw, reads=(), writes=(), inc=True):
        need = self._collect(eng, reads, writes, False)
        self._emit_waits(eng, need)
        self.ninstr += 1
        if inc:
            self.count[eng] += 1
            ev = (eng, self.count[eng])
            self.prog[eng].append(('ins', method, kw, self.sem[eng], 1))
        else:
            ev = (eng, self.count[eng] + 1)
            self.prog[eng].append(('ins', method, kw, None, 0))
        self._mark(ev, reads, writes)

    def dma(self, q, kw, reads=(), writes=(), method=None):
        nsem = NDMA_SEMS if q != 'pool' else NPOOL_SEMS
        i = self.dq[q] % nsem
        self.dq[q] += 1
        k = ('dma', q, i)
        need = self._collect(q, reads, writes, True)
        if self.count[k] > 0:
            need[k] = max(need.get(k, 0), self.count[k])
        self._emit_waits(q, need)
        self.ninstr += 1
        self.count[k] += 16
        ev = (k, self.count[k])
        if method is None:
            method = getattr(self.nc, ENG_ATTR[q]).dma_start
        self.prog[q].append(('ins', method, kw, self.sem[k], 16))
        self._mark(ev, reads, writes)

    def barrier(self):
        for e in ENG_ATTR:
            need = {k: v for k, v in self.count.items() if v > 0 and k != e}
            self._emit_waits(e, need)

    def emit(self):
        nc = self.nc
        if os.environ.get('DBGPROG'):
            names = {id(v): k for k, v in self.sem.items()}
            for e in ENG_ATTR:
                print('ENGINE', e)
                c = 0
                for it in self.prog[e][-int(os.environ['DBGPROG']):]:
                    if it[0] == 'wait':
                        print('   wait', names[id(it[1])], it[2])
                    else:
                        print('   ins', getattr(it[1], '__name__', it[1]), 'inc' if it[3] is not None else '-', [k for k in it[2] if k in ('func',)] and it[2].get('func'))
        with nc.Block() as block:
            for e, attr in ENG_ATTR.items():
                prog = self.prog[e]

                def body(engine, prog=prog):
                    for it in prog:
                        if it[0] == 'wait':
                            engine.wait_ge(it[1], it[2])
                        else:
                            ins = it[1](**it[2])
                            if it[3] is not None:
                                ins.then_inc(it[3], it[4])
                getattr(block, attr)(body)
        self.prog = {e: [] for e in ENG_ATTR}


def build_nc(dbg=False, stop=None):
    nc = bass.Bass("TRN2", target_bir_lowering=False)

    def din(name, shape, dt=F32):
        return nc.dram_tensor(name, shape, dt, kind="ExternalInput")

    def dscr(name, shape, dt=F32):
        return nc.dram_tensor(name, shape, dt, kind="Internal")

    xh = din("x", [T, D]); ctxh = din("ctx", [TC, D]); cvh = din("cvec", [D, 2])
    adawh = din("ada_w", [D, 6 * D]); vecsh = din("vecs", [64, 128]); b1h = din("b1r", [512, 128])
    winh = din("w_in", [D, INC]); gbh = din("gate_b", [16]); mngh = din("mng", [D]); dngh = din("dng", [D])
    fngh = din("fng", [D]); dlh = din("dlam", [256]); wah = din("w_a", [D, D]); wbh = din("w_b", [D, D])
    woh = din("w_o", [D, D]); rwh = din("rw", [D, NE]); rbh = din("rb", [NE])
    w1h = din("w1", [NE, D, 2 * D]); w2h = din("w2", [NE, D, D]); b2h = din("b2", [NE, D])
    idfh = din("idf", [128, 128]); idbh = din("idb", [128, 128], BF16)
    triuh = din("triu", [128, 128]); trilh = din("tril", [128, 128]); onesh = din("ones", [128, 128])
    cosh_ = din("cost", [128, T]); sinh_ = din("sint", [128, T])
    strih = din("stri", [128, 128]); thrBh = din("thrB", [128, NBLK]); pidxh = din("pidx", [128, 1])
    yh = nc.dram_tensor("y", [T, D], F32, kind="ExternalOutput")

    modscr = dscr("modscr", [48 * 128]); hfscr = dscr("hfscr", [4, 32, 128, 256])
    aTd = dscr("aTd", [128, 8, T], BF16); bTd = dscr("bTd", [128, 8, T], BF16)
    sgd = dscr("sgd", [128, 16, T], BF16); x1d = dscr("x1d", [T, D]); u2d = dscr("u2d", [128, 8, T], BF16)
    w1bd = dscr("w1bd", [NE * 128, 8 * 2 * D], BF16); w2bd = dscr("w2bd", [NE * 128, 8 * D], BF16)
    b1Td = dscr("b1Td", [NE * 128, 16]); u2tokd = dscr("u2tokd", [T, D], BF16)
    Xg = dscr("Xg", [NBUF, D], BF16); Yg = dscr("Yg", [NBUF, D])

    def bc(h, off, n, parts=128):
        return bass.AP(h, off, [[0, parts], [1, n]])

    with ExitStack() as gst:
        S = Sched(nc, gst)

        def sbg(name, shape, dt=F32):
            return gst.enter_context(nc.sbuf_tensor('sb_' + name, shape, dt))

        PSUM = [gst.enter_context(nc.psum_tensor("PS%d" % i, [128, 1024], F32)) for i in range(4)]

        def PS(i, b):
            return PSUM[i][:, b * 512:(b + 1) * 512]

        def PSb(i, b):
            return PSUM[i][:].bitcast(BF16)[:, b * 1024:(b + 1) * 1024]

        def PN(i, b):
            return ('PS%d' % i, b)

        def PE(out, lhsT, rhs, start, stop, R, W, inc=True):
            S.op('pe', nc.tensor.matmul, dict(out=out, lhsT=lhsT, rhs=rhs, start=start, stop=stop), R, W, inc)

        def PET(out, in_, ident, R, W, inc=True):
            S.op('pe', nc.tensor.transpose, dict(out=out, in_=in_, identity=ident), R, W, inc)

        def ACT(out, in_, func, R, W, **kw):
            S.op('act', nc.scalar.activation, dict(out=out, in_=in_, func=func, **kw), R, W)

        def TS(eng, out, in0, s1, s2, op0, op1, R, W):
            m = nc.vector.tensor_scalar if eng == 'dve' else nc.gpsimd.tensor_scalar
            kw = dict(out=out, in0=in0, scalar1=s1, scalar2=s2, op0=op0)
            if op1 is not None:
                kw['op1'] = op1
            S.op(eng, m, kw, R, W)

        def TTo(eng, out, in0, in1, op, R, W):
            m = nc.vector.tensor_tensor if eng == 'dve' else nc.gpsimd.tensor_tensor
            S.op(eng, m, dict(out=out, in0=in0, in1=in1, op=op), R, W)

        def STT(eng, out, in0, scalar, in1, op0, op1, R, W):
            m = nc.vector.scalar_tensor_tensor if eng == 'dve' else nc.gpsimd.scalar_tensor_tensor
            S.op(eng, m, dict(out=out, in0=in0, scalar=scalar, in1=in1, op0=op0, op1=op1), R, W)

        def CP(eng, out, in_, R, W):
            if eng == 'act':
                ACT(out, in_, AF.Copy, R, W)
            else:
                m = nc.vector.tensor_copy if eng == 'dve' else nc.gpsimd.tensor_copy
                S.op(eng, m, dict(out=out, in_=in_), R, W)

        def DMA(q, out, in_, R, W):
            S.dma(q, dict(out=out, in_=in_), R, W)


        def dump(name, src, shape, dt, reads):
            h_ = nc.dram_tensor(name, shape, dt, kind="ExternalOutput")
            DMA('sp', h_.ap(), src, reads, [(name, None)])

        def finish_dbg():
            S.barrier()
            S.emit()
            return nc
        idf = sbg("idf", [128, 128]); idb = sbg("idb", [128, 128], BF16)
        triu = sbg("triu", [128, 128]); tril = sbg("tril", [128, 128]); ones = sbg("ones", [128, 128])
        epsb = sbg("epsb", [128, 1])
        prm = sbg("prm", [128, 6, 8])
        DMA('sp', idf[:], idfh.ap(), [], [('idf', None)])
        DMA('sp', idb[:], idbh.ap(), [], [('idb', None)])
        DMA('sp', triu[:], triuh.ap(), [], [('triu', None)])
        DMA('sp', tril[:], trilh.ap(), [], [('tril', None)])
        DMA('sp', ones[:], onesh.ap(), [], [('ones', None)])
        S.op('dve', nc.vector.memset, dict(ap=epsb[:], constant=EPS), [], [('epsb', None)])

        with ExitStack() as st:
            def sb(name, shape, dt=F32):
                return st.enter_context(nc.sbuf_tensor('sb_' + name, shape, dt))
            cv = sb("cv", [128, 8, 2]); sc = sb("sc", [128, 8, 2]); vin = sb("vin", [64, 128]); vT = sb("vT", [128, 64])
            adw = [sb("adw%d" % i, [128, 8, 1024]) for i in range(2)]
            modT = sb("modT", [128, 48, 2]); mrow = sb("mrow", [48, 128]); tmpa = sb("tmpa", [128, 8])
            DMA('sp', cv[:], cvh.ap().rearrange("(k p) n -> p k n", p=128), [], [('cv', None)])
            DMA('sp', vin[:], vecsh.ap(), [], [('vin', None)])
            ACT(sc[:], cv[:], AF.Silu, [('cv', None)], [('sc', None)])
            PET(PS(0, 0)[:, 0:64], vin[:], idf[0:64, 0:64], [('vin', None), ('idf', None)], [PN(0, 0)])
            CP('dve', vT[:], PS(0, 0)[:, 0:64], [PN(0, 0)], [('vT', None)])
            for jb in range(6):
                DMA('sp', adw[jb % 2][:], adawh.ap()[:, jb * 1024:(jb + 1) * 1024].rearrange("(k p) n -> p k n", p=128),
                    [], [('adw', jb % 2)])
                for jj in range(8):
                    j = jb * 8 + jj
                    for kc in range(8):
                        PE(PS(0, 1)[:, 2 * j:2 * j + 2], adw[jb % 2][:, kc, jj * 128:(jj + 1) * 128], sc[:, kc, :],
                           kc == 0, kc == 7, [('adw', jb % 2), ('sc', None)], [PN(0, 1)], inc=(kc == 7))
            pm = PS(0, 1)[:, 0:96].rearrange("p (j n) -> p j n", n=2)
            for n_ in range(2):
                TTo('dve', modT[:, :, n_], pm[:, :, n_], vT[:, 0:48], ALU.add, [PN(0, 1), ('vT', None)], [('modT', n_)])
            for (pi, n_, gcol, sccol, shcol) in ((0, 0, 48, 8, 0), (2, 1, 48, 8, 0), (4, 0, 56, 32, 24)):
                TS('dve', tmpa[:], modT[:, sccol:sccol + 8, n_], 1.0, None, ALU.add, None, [('modT', n_)], [('tmpa', None)])
                TTo('dve', prm[:, pi, :], tmpa[:], vT[:, gcol:gcol + 8], ALU.mult, [('tmpa', None), ('vT', None)], [('prm', pi)])
                CP('dve', prm[:, pi + 1, :], modT[:, shcol:shcol + 8, n_], [('modT', n_)], [('prm', pi + 1)])
            PET(PS(0, 0)[0:48, 0:128], modT[:, :, 0], idf[:], [('modT', 0), ('idf', None)], [PN(0, 0)])
            CP('dve', mrow[:], PS(0, 0)[0:48, 0:128], [PN(0, 0)], [('mrow', None)])
            DMA('sp', modscr.ap().rearrange("(j p) -> j p", p=128), mrow[:], [('mrow', None)], [('modscr', None)])
            S.barrier()
            S.emit()

        if stop == 'A':
            dump('d_prm', prm[:], [128, 6, 8], F32, [('prm', None)])
            return finish_dbg()
        ust = ExitStack() if stop is None else gst
        uT = ust.enter_context(nc.sbuf_tensor("sb_uT", [128, 8, TT], BF16))

        def norm_rstd(src, junk, ssb, nm):
            ACT(junk, src, AF.Square, [(nm, None)], [('junk', None), (nm + 'ss', 0)], accum_out=ssb[:, 0:1])
            ACT(ssb[:, 1:2], ssb[:, 0:1], AF.Ln, [(nm + 'ss', 0), ('epsb', None)], [(nm + 'ss', 1)], scale=1.0 / D, bias=epsb[:, 0:1])
            ACT(ssb[:, 2:3], ssb[:, 1:2], AF.Exp, [(nm + 'ss', 1)], [(nm + 'ss', 2)], scale=-0.5)

        with ExitStack() as st:
            def sb(name, shape, dt=F32):
                return st.enter_context(nc.sbuf_tensor('sb_' + name, shape, dt))
            xin = [sb("xin%d" % i, [128, D]) for i in range(3)]
            ssb = [sb("ssb%d" % i, [128, 4]) for i in range(3)]
            xn = [sb("xn%d" % i, [128, D], BF16) for i in range(2)]
            junk = sb("junk", [128, D], BF16)
            for ti in range(NT):
                b3 = ti % 3
                src = ctxh.ap()[ti * 128:(ti + 1) * 128, :] if ti < 2 else xh.ap()[(ti - 2) * 128:(ti - 1) * 128, :]
                nm = 'xin%d' % b3
                DMA('sp', xin[b3][:], src, [], [(nm, None)])
                norm_rstd(xin[b3][:], junk[:], ssb[b3], nm)
                xnn = 'xn%d' % (ti % 2)
                TS('dve', xn[ti % 2][:], xin[b3][:], ssb[b3][:, 2:3], None, ALU.mult, None, [(nm, None), (nm + 'ss', 2)], [(xnn, None)])
                pi = 2 if ti < 2 else 0
                for g in range(2):
                    for j in range(4):
                        kc = g * 4 + j
                        PET(PSb(3, g)[:, j * 128:(j + 1) * 128], xn[ti % 2][:, kc * 128:(kc + 1) * 128], idb[:],
                            [(xnn, None), ('idb', None)], [PN(3, g)], inc=(j == 3))
                    for j in range(4):
                        kc = g * 4 + j
                        o = uT[:, kc, ti * 128:(ti + 1) * 128]
                        i_ = PSb(3, g)[:, j * 128:(j + 1) * 128]
                        if j % 2 == 0:
                            TS('dve', o, i_, prm[:, pi, kc:kc + 1], prm[:, pi + 1, kc:kc + 1], ALU.mult, ALU.add,
                               [PN(3, g), ('prm', pi), ('prm', pi + 1)], [('uT', ti)])
                        else:
                            ACT(o, i_, AF.Identity, [PN(3, g), ('prm', pi), ('prm', pi + 1)], [('uT', ti)],
                                scale=prm[:, pi, kc:kc + 1], bias=prm[:, pi + 1, kc:kc + 1])
            S.barrier()
            S.emit()

        if dbg:
            dbg_u = nc.dram_tensor("dbg_u", [128, 8, TT], BF16, kind="ExternalOutput")
            DMA('sp', dbg_u.ap(), uT[:], [('uT', None)], [('dbg_u', None)])

        if stop == 'B':
            dump('d_uT', uT[:], [128, 8, TT], BF16, [('uT', None)])
            return finish_dbg()
        def wslice(c0, n):
            return winh.ap()[:, c0:c0 + n].rearrange("(k p) n -> p k n", p=128)

        with ExitStack() as st:
            def sb(name, shape, dt=F32):
                return st.enter_context(nc.sbuf_tensor('sb_' + name, shape, dt))
            wg = sb("wg", [128, 8, 16], BF16); gb34 = sb("gb34", [128, NT, 16]); G = sb("G", [128, NT, 16])
            SP = [sb("SP%d" % d, [128, NT * 4]) for d in range(2)]
            EE = [sb("EE%d" % d, [128, NT, 4]) for d in range(2)]
            WW = [sb("WW%d" % d, [128, NT, 4]) for d in range(2)]
            EL = [sb("EL%d" % d, [128, NT, 4]) for d in range(2)]
            tmpw = sb("tmpw", [128, NT, 4])
            cst = [sb("cst%d" % d, [128, 272]) for d in range(2)]
            gm = sb("gm", [128, D])
            DMA('pool', wg[:], wslice(O_MG, 16), [], [('wg', None)])
            gb16 = sb("gb16", [128, 16])
            DMA('sp', gb16[:], bc(gbh, 0, 16), [], [('gb16', None)])
            DMA('sp', gm[:], bc(mngh, 0, D), [], [('gm', None)])
            for ti in range(NT):
                bank = 0 if ti < 32 else 1
                col = (ti % 32) * 16
                for kc in range(8):
                    PE(PS(0, bank)[:, col:col + 16], uT[:, kc, ti * 128:(ti + 1) * 128], wg[:, kc, :], kc == 0, kc == 7,
                       [('uT', None), ('wg', None)], [PN(0, bank)], inc=(kc == 7))
            for ti in range(NT):
                bank = 0 if ti < 32 else 1
                col = (ti % 32) * 16
                TTo('dve', G[:, ti, :], PS(0, bank)[:, col:col + 16], gb16[:], ALU.add, [PN(0, bank), ('gb16', None)], [('G', ti)])
            if stop == 'C0a':
                dump('d_G', G[:], [128, NT, 16], F32, [('G', None)])
                return finish_dbg()
            for d in range(2):
                spv = SP[d][:].rearrange("p (t n) -> p t n", n=4)
                ACT(spv, G[:, :, 8 * d + 4:8 * d + 8], AF.Exp, [('G', None)], [('SP', d)], scale=-1.0)
                ACT(SP[d][:], SP[d][:], AF.Ln, [('SP', d), ('ones', None)], [('SP', d)], bias=ones[:, 0:1])
                if stop == 'C0b':
                    continue
                tri = triu if d == 0 else tril
                PE(PS(1, d)[:, 0:136], tri[:], SP[d][:], True, True, [('SP', d), ('triu', None), ('tril', None)], [PN(1, d)])
                PE(PS(1, d)[:, 136:272], ones[:], SP[d][:], True, True, [('SP', d), ('ones', None)], [PN(1, d)])
                if stop == 'C0c':
                    CP('dve', EE[d][:].rearrange("p t n -> p (t n)"), PS(1, d)[:, 0:136], [PN(1, d)], [('EE', d)])
                    continue
                CP('dve', cst[d][:], PS(1, d)[:, 0:272], [PN(1, d)], [('cst', d)])
                cs = cst[d][:, 0:136].rearrange("p (t n) -> p t n", n=4)
                tt_ = cst[d][:, 136:272].rearrange("p (t n) -> p t n", n=4)
                SK = os.environ.get('DBGSKIP', '')
                if '1' not in SK:
                    ACT(EE[d][:], cs, AF.Exp, [('cst', d)], [('EE', d)], scale=-1.0)
                if '2' not in SK:
                    TTo('dve', tmpw[:], G[:, :, 8 * d:8 * d + 4], cs, ALU.add, [('G', None), ('cst', d)], [('tmpw', None)])
                if '3' not in SK:
                    ACT(WW[d][:], tmpw[:], AF.Exp, [('tmpw', None)], [('WW', d)])
                if '4' not in SK:
                    ACT(EL[d][:], tt_, AF.Exp, [('cst', d)], [('EL', d)], scale=-1.0)

            if stop == 'C0b':
                dump('d_SP', SP[0][:], [128, NT * 4], F32, [('SP', 0)])
                return finish_dbg()
            if stop == 'C0c':
                dump('d_EE', EE[0][:], [128, NT, 4], F32, [('EE', 0)])
                return finish_dbg()
            if stop == 'C0':
                dump('d_EE', EE[0][:], [128, NT, 4], F32, [('EE', 0)])
                dump('d_WW', WW[1][:], [128, NT, 4], F32, [('WW', 1)])
                dump('d_EL', EL[0][:], [128, NT, 4], F32, [('EL', 0)])
                dump('d_G', G[:], [128, NT, 16], F32, [('G', None)])
                return finish_dbg()
            wm = sb("wm", [128, 8, 768], BF16)
            qT = sb("qT", [128, TT], BF16); kT = sb("kT", [128, TT], BF16)
            vt = sb("vt", [128, NT, 257], BF16)
            kt = [sb("kt%d" % d, [128, NT, 128], BF16) for d in range(2)]
            SD = [sb("SD%d" % i, [128, 128], BF16) for i in range(2)]
            Cf = sb("Cf", [128, 257]); Cb = sb("Cb", [128, 257], BF16); tmpC = sb("tmpC", [128, 257])
            sml = [sb("sml%d" % i, [128, 8]) for i in range(2)]
            hst = [sb("hst%d" % i, [128, 256]) for i in range(3)]
            hfl = [sb("hfl%d" % i, [128, 256]) for i in range(3)]
            hs = [sb("hs%d" % i, [128, 256]) for i in range(2)]
            osg = [sb("osg%d" % i, [128, 256]) for i in range(2)]
            ab = [sb("ab%d" % i, [128, 256], BF16) for i in range(2)]
            aTs = [sb("aTs%d" % i, [128, 2, 512], BF16) for i in range(2)]
            junk2 = sb("junk2", [128, 256], BF16)
            S.op('pool', nc.gpsimd.memset, dict(ap=vt[:, :, 256:257], constant=1.0), [], [('vt1', None)])

            for h in range(4):
                DMA('pool', wm[:, :, 0:128], wslice(O_MQ + h * 128, 128), [], [('wm', 0)])
                DMA('pool', wm[:, :, 128:256], wslice(O_MK + h * 128, 128), [], [('wm', 1)])
                DMA('pool', wm[:, :, 256:512], wslice(O_MV + h * 256, 256), [], [('wm', 2)])
                DMA('pool', wm[:, :, 512:768], wslice(O_MO + h * 256, 256), [], [('wm', 3)])
                cnt = 0
                for blk in range(9):
                    c0 = blk * 512
                    n = min(512, TT - c0)
                    for which in range(2):
                        pi_, pb_ = (cnt // 2) % 2, cnt % 2
                        cnt += 1
                        for kc in range(8):
                            PE(PS(pi_, pb_)[:, 0:n], wm[:, kc, which * 128:(which + 1) * 128], uT[:, kc, c0:c0 + n], kc == 0, kc == 7,
                               [('wm', which), ('uT', None)], [PN(pi_, pb_)], inc=(kc == 7))
                        if which == 0:
                            ACT(qT[:, c0:c0 + n], PS(pi_, pb_)[:, 0:n], AF.Copy, [PN(pi_, pb_)], [('qT', blk)], scale=128.0 ** -0.5)
                        else:
                            CP('dve', kT[:, c0:c0 + n], PS(pi_, pb_)[:, 0:n], [PN(pi_, pb_)], [('kT', blk)])
                for ti in range(NT):
                    pb_ = ti % 2
                    for kc in range(8):
                        PE(PS(2, pb_)[:, 0:384], uT[:, kc, ti * 128:(ti + 1) * 128], wm[:, kc, 128:512], kc == 0, kc == 7,
                           [('uT', None), ('wm', 1), ('wm', 2)], [PN(2, pb_)], inc=(kc == 7))
                    TS('dve', kt[0][:, ti, :], PS(2, pb_)[:, 0:128], WW[0][:, ti, h:h + 1], None, ALU.mult, None,
                       [PN(2, pb_), ('WW', 0)], [('kt0', ti)])
                    TS('dve', kt[1][:, ti, :], PS(2, pb_)[:, 0:128], WW[1][:, ti, h:h + 1], None, ALU.mult, None,
                       [PN(2, pb_), ('WW', 1)], [('kt1', ti)])
                    CP('act', vt[:, ti, 0:256], PS(2, pb_)[:, 128:384], [PN(2, pb_)], [('vt', ti)])

                for d in range(2):
                    order = list(range(NT)) if d == 0 else [1, 0] + list(range(NT - 1, 1, -1))
                    mask = triu if d == 0 else tril
                    lat = [t_ for t_ in order if t_ >= 2]

                    def emit_sd(ti, li):
                        sl = li % 2
                        cs_ = slice(ti * 128, (ti + 1) * 128)
                        PE(PS(0, sl)[:, 0:128], kT[:, cs_], qT[:, cs_], True, True, [('kT', None), ('qT', None)], [PN(0, sl)])
                        STT('dve', SD[sl][:], PS(0, sl)[:, 0:128], WW[d][:, ti, h:h + 1], mask[:], ALU.mult, ALU.mult,
                            [PN(0, sl), ('WW', d), ('triu', None), ('tril', None)], [('SD', sl)])

                    def emit_oraw(li_):
                        ti_ = lat[li_]
                        b2_ = li_ % 2
                        for kc in range(8):
                            PE(PS(3, 0)[:, 0:256], uT[:, kc, ti_ * 128:(ti_ + 1) * 128], wm[:, kc, 512:768], kc == 0, kc == 7,
                               [('uT', None), ('wm', 3)], [PN(3, 0)], inc=(kc == 7))
                        ACT(osg[b2_][:], PS(3, 0)[:, 0:256], AF.Sigmoid, [PN(3, 0)], [('osg', b2_)])
                        TTo('pool', osg[b2_][:], osg[b2_][:], gm[:, h * 256:(h + 1) * 256], ALU.mult, [('osg', b2_), ('gm', None)], [('osg', b2_)])

                    def emit_hfload(li_):
                        c_ = lat[li_] - 2
                        DMA('sp', hfl[li_ % 3][:], hfscr.ap()[h, c_], [('hf', (h, c_))], [('hfl', li_ % 3)])

                    def emit_tr(li_):
                        c_ = lat[li_] - 2
                        b2_ = li_ % 2
                        blk, sub = c_ // 4, c_ % 4
                        ab_ = blk % 2
                        for jj in range(2):
                            PET(PSb(3, 1)[:, jj * 128:(jj + 1) * 128], ab[b2_][:, jj * 128:(jj + 1) * 128], idb[:],
                                [('ab', b2_), ('idb', None)], [PN(3, 1)], inc=(jj == 1))
                        CP('act', aTs[ab_][:, :, sub * 128:(sub + 1) * 128],
                           PSb(3, 1)[:, 0:256].rearrange("p (a b) -> p a b", b=128), [PN(3, 1)], [('aTs', ab_)])
                        if sub == 0:
                            DMA('sp', aTd.ap()[:, 2 * h:2 * h + 2, blk * 512:(blk + 1) * 512], aTs[ab_][:],
                                [('aTs', ab_)], [('aTd', (h, blk))])

                    emit_sd(lat[0], 0)
                    if d == 1:
                        emit_hfload(0)
                        emit_hfload(1)
                        emit_oraw(0)
                    li = 0
                    for j, ti in enumerate(order):
                        latent = ti >= 2
                        last = (j == NT - 1)
                        cs_ = slice(ti * 128, (ti + 1) * 128)
                        if latent and li + 1 < len(lat):
                            emit_sd(lat[li + 1], li + 1)
                        pb_ = j % 2
                        if not last:
                            PE(PS(2, pb_)[:, 0:257], kt[d][:, ti, :], vt[:, ti, :], True, True,
                               [('kt%d' % d, ti), ('vt', ti), ('vt1', None)], [PN(2, pb_)])
                        if latent:
                            sl = li % 2
                            PE(PS(1, sl)[:, 0:257], SD[sl][:], vt[:, ti, :], True, j == 0,
                               [('SD', sl), ('vt', ti), ('vt1', None)], [PN(1, sl)], inc=(j == 0))
                            if j > 0:
                                PE(PS(1, sl)[:, 0:257], qT[:, cs_], Cb[:], False, True, [('qT', None), ('Cb', None)], [PN(1, sl)])
                        if not last:
                            Ecol = EL[d][:, ti, h:h + 1]
                            if j == 0:
                                ACT(Cf[:], PS(2, pb_)[:, 0:257], AF.Copy, [PN(2, pb_), ('EL', d)], [('Cf', None)], scale=Ecol)
                                ACT(Cb[:], PS(2, pb_)[:, 0:257], AF.Copy, [PN(2, pb_), ('EL', d)], [('Cb', None)], scale=Ecol)
                            else:
                                TTo('dve', tmpC[:], PS(2, pb_)[:, 0:257], Cf[:], ALU.add, [PN(2, pb_), ('Cf', None)], [('tmpC', None)])
                                ACT(Cb[:], tmpC[:], AF.Copy, [('tmpC', None), ('EL', d)], [('Cb', None)], scale=Ecol)
                                ACT(Cf[:], tmpC[:], AF.Copy, [('tmpC', None), ('EL', d)], [('Cf', None)], scale=Ecol)
                        if latent:
                            sl = li % 2
                            c = ti - 2
                            num = PS(1, sl)
                            sm = sml[li % 2]
                            smn = 'sml%d' % (li % 2)
                            e_ = EE[d][:, ti, h:h + 1]
                            if d == 1:
                                if li + 1 < len(lat):
                                    emit_oraw(li + 1)
                                if li >= 1:
                                    emit_tr(li - 1)
                                if li + 2 < len(lat):
                                    emit_hfload(li + 2)
                            TS('dve', sm[:, 0:1], num[:, 256:257], e_, None, ALU.mult, None, [PN(1, sl), ('EE', d)], [(smn, 0)])
                            TS('dve', sm[:, 7:8], sm[:, 0:1], -1.0, None, ALU.mult, None, [(smn, 0)], [(smn, 7)])
                            TTo('dve', sm[:, 1:2], sm[:, 0:1], sm[:, 7:8], ALU.max, [(smn, 0), (smn, 7)], [(smn, 1)])
                            TS('dve', sm[:, 1:2], sm[:, 1:2], 1.0, None, ALU.max, None, [(smn, 1)], [(smn, 1)])
                            S.op('dve', nc.vector.reciprocal, dict(out=sm[:, 2:3], in_=sm[:, 1:2]), [(smn, 1)], [(smn, 2)])
                            TS('dve', sm[:, 3:4], sm[:, 2:3], e_, None, ALU.mult, None, [(smn, 2), ('EE', d)], [(smn, 3)])
                            if d == 0:
                                b3 = li % 3
                                ACT(hst[b3][:], num[:, 0:256], AF.Copy, [PN(1, sl), (smn, 3)], [('hst', b3)], scale=sm[:, 3:4])
                                DMA('sp', hfscr.ap()[h, c], hst[b3][:], [('hst', b3)], [('hf', (h, c))])
                            else:
                                b3 = li % 3
                                b2 = li % 2
                                STT('dve', hs[b2][:], num[:, 0:256], sm[:, 3:4], hfl[b3][:], ALU.mult, ALU.add,
                                    [PN(1, sl), (smn, 3), ('hfl', b3)], [('hs', b2)])
                                ACT(junk2[:], hs[b2][:], AF.Square, [('hs', b2)], [('junk2', None), (smn, 4)], accum_out=sm[:, 4:5])
                                ACT(sm[:, 5:6], sm[:, 4:5], AF.Ln, [(smn, 4), ('epsb', None)], [(smn, 5)], scale=1.0 / 256, bias=epsb[:, 0:1])
                                ACT(sm[:, 6:7], sm[:, 5:6], AF.Exp, [(smn, 5)], [(smn, 6)], scale=-0.5)
                                STT('dve', ab[b2][:], hs[b2][:], sm[:, 6:7], osg[b2][:], ALU.mult, ALU.mult,
                                    [('hs', b2), (smn, 6), ('osg', b2)], [('ab', b2)])
                                if li == len(lat) - 1:
                                    emit_tr(li)
                            li += 1
            S.barrier()
            S.emit()

        if stop == 'C1':
            dump('d_aT', aTd.ap(), [128, 8, T], BF16, [('aTd', None)])
            return finish_dbg()
        with ExitStack() as st:
            def sb(name, shape, dt=F32):
                return st.enter_context(nc.sbuf_tensor('sb_' + name, shape, dt))
            cost = sb("cost", [128, T]); sint = sb("sint", [128, T])
            DMA('sp', cost[:], cosh_.ap(), [], [('cost', None)])
            DMA('sp', sint[:], sinh_.ap(), [], [('sint', None)])
            lamb = sb("lamb", [128, 256]); lt = sb("lt", [128, 8])
            gd = sb("gd", [128, D])
            DMA('sp', lamb[:], bc(dlh, 0, 256), [], [('lamb', None)])
            DMA('sp', gd[:], bc(dngh, 0, D), [], [('gd', None)])
            TS('pool', gd[:], gd[:], 1.0 - LAM_INIT, None, ALU.mult, None, [('gd', None)], [('gd', None)])
            lp = sb("lp", [128, 128])
            TTo('dve', lp[:, 0:64], lamb[:, 0:64], lamb[:, 64:128], ALU.mult, [('lamb', None)], [('lp', 0)])
            TTo('dve', lp[:, 64:128], lamb[:, 128:192], lamb[:, 192:256], ALU.mult, [('lamb', None)], [('lp', 1)])
            S.op('dve', nc.vector.reduce_sum, dict(out=lt[:, 0:1], in_=lp[:, 0:64], axis=AX.X), [('lp', 0)], [('lt', 0)])
            S.op('dve', nc.vector.reduce_sum, dict(out=lt[:, 1:2], in_=lp[:, 64:128], axis=AX.X), [('lp', 1)], [('lt', 1)])
            ACT(lt[:, 2:4], lt[:, 0:2], AF.Exp, [('lt', 0), ('lt', 1)], [('lt', 2)])
            TTo('dve', lt[:, 4:5], lt[:, 3:4], lt[:, 2:3], ALU.subtract, [('lt', 2)], [('lt', 4)])
            TS('dve', lt[:, 5:6], lt[:, 4:5], -LAM_INIT, None, ALU.add, None, [('lt', 4)], [('lt', 5)])
            neglam = lt[:, 5:6]

            wd = sb("wd", [128, 5, 1024], BF16)
            qT = sb("dqT", [128, T], BF16); kT = sb("dkT", [128, TT], BF16)
            vd = sb("vd", [128, NT, 129], BF16)
            t1 = [sb("t1_%d" % i, [128, 512]) for i in range(2)]
            t2 = [sb("t2_%d" % i, [128, 512]) for i in range(2)]
            Pex = [sb("Pex%d" % i, [128, 1024], BF16) for i in range(3)]
            es = [sb("es%d" % i, [128, 8]) for i in range(2)]
            Ocp = sb("Ocp", [128, 8 * 129]); es4 = sb("es4", [128, 24]); A4 = sb("A4", [128, 4, 128]); sq4 = sb("sq4", [128, 4, 128])
            bb4 = sb("bb4", [128, 4, 128], BF16)
            pending = []

            def flush(upto):
                keep = []
                for (trig, fn) in pending:
                    if upto is None or trig <= upto:
                        fn()
                    else:
                        keep.append((trig, fn))
                pending[:] = keep
            A1 = [sb("A1_%d" % i, [128, 128]) for i in range(2)]
            bb = [sb("bb%d" % i, [128, 128], BF16) for i in range(2)]
            bst = [sb("bst%d" % i, [128, 512], BF16) for i in range(2)]
            junk3 = sb("junk3", [128, 128], BF16)
            S.op('pool', nc.gpsimd.memset, dict(ap=vd[:, :, 128:129], constant=1.0), [], [('vd1', None)])
            cw = [sb("cw_%d" % i, [128, 8, D], BF16) for i in range(2)]
            ccnt = [0]

            def oreg(m, sub):
                r = m * 4 + sub
                if r < 3:
                    return PS(2, 0)[:, r * 129:(r + 1) * 129], PN(2, 0)
                if r < 6:
                    return PS(2, 1)[:, (r - 3) * 129:(r - 2) * 129], PN(2, 1)
                return PS(3, 0)[:, (r - 6) * 129:(r - 5) * 129], PN(3, 0)

            for h in range(8):
                for pi_, c0 in ((0, O_DQ + h * 128), (2, O_DK + h * 128), (4, O_DV + h * 128)):
                    DMA('pool', wd[:, pi_, :].rearrange("p (k n) -> p k n", n=128), wslice(c0, 128), [], [('wd', pi_)])
                for pi_ in (0, 2):
                    s4 = wd[:, pi_, :].rearrange("p (a two j) -> p a two j", two=2, j=16)
                    d4 = wd[:, pi_ + 1, :].rearrange("p (a two j) -> p a two j", two=2, j=16)
                    CP('pool', d4[:, :, 0, :], s4[:, :, 1, :], [('wd', pi_)], [('wd', pi_ + 1)])
                    CP('pool', d4[:, :, 1, :], s4[:, :, 0, :], [('wd', pi_)], [('wd', pi_ + 1)])
                def emit_vgroup(g):
                    tiles = list(range(g * 4, min(NT, g * 4 + 4)))
                    pb_ = g % 2
                    for ii, ti in enumerate(tiles):
                        for kc in range(8):
                            PE(PS(2, pb_)[:, ii * 128:(ii + 1) * 128], uT[:, kc, ti * 128:(ti + 1) * 128], wd[:, 4, kc * 128:(kc + 1) * 128],
                               kc == 0, kc == 7, [('uT', None), ('wd', 4)], [PN(2, pb_)], inc=(kc == 7 and ii == len(tiles) - 1))
                    nt_ = len(tiles)
                    CP('act', vd[:, tiles[0]:tiles[0] + nt_, 0:128], PS(2, pb_)[:, 0:nt_ * 128].rearrange("p (a b) -> p a b", b=128),
                       [PN(2, pb_)], [('vd', g)])

                vg = [0]
                cnt = 0
                for which in range(2):
                    dst = qT if which == 0 else kT
                    dn = 'dqT' if which == 0 else 'dkT'
                    off = 0 if which == 0 else TC
                    wp = 0 if which == 0 else 2
                    for blk in range(8):
                        c0 = TC + blk * 512
                        pi_ = cnt % 2
                        cnt += 1
                        for ab_ in range(2):
                            for kc in range(8):
                                PE(PS(pi_, ab_), wd[:, wp + ab_, kc * 128:(kc + 1) * 128], uT[:, kc, c0:c0 + 512], kc == 0, kc == 7,
                                   [('wd', wp + ab_), ('uT', None)], [PN(pi_, ab_)], inc=(kc == 7))
                        if vg[0] < 9 and (cnt % 2 == 1 or vg[0] < cnt // 2):
                            emit_vgroup(vg[0])
                            vg[0] += 1
                        tb_ = blk % 2
                        TTo('dve', t1[tb_][:], PS(pi_, 0), cost[:, blk * 512:(blk + 1) * 512], ALU.mult, [PN(pi_, 0), ('cost', None)], [('t1', tb_)])
                        TTo('dve', t2[tb_][:], PS(pi_, 1), sint[:, blk * 512:(blk + 1) * 512], ALU.mult, [PN(pi_, 1), ('sint', None)], [('t2', tb_)])
                        TTo('pool', dst[:, off + blk * 512:off + (blk + 1) * 512], t1[tb_][:], t2[tb_][:], ALU.add,
                            [('t1', tb_), ('t2', tb_)], [(dn, blk)])
                    if which == 0:
                        flush(None)
                for kc in range(8):
                    PE(PS(0, 0)[:, 0:256], wd[:, 2, kc * 128:(kc + 1) * 128], uT[:, kc, 0:TC], kc == 0, kc == 7,
                       [('wd', 2), ('uT', None)], [PN(0, 0)], inc=(kc == 7))
                CP('act', kT[:, 0:TC], PS(0, 0)[:, 0:256], [PN(0, 0)], [('dkT', 'c')])
                while vg[0] < 9:
                    emit_vgroup(vg[0])
                    vg[0] += 1
                for e in range(4 * h, 4 * h + 4):
                    for g in range(3):
                        cb_ = ccnt[0] % 2
                        ccnt[0] += 1
                        if g < 2:
                            src = w1h.ap()[e][:, g * D:(g + 1) * D].rearrange("(k p) n -> p k n", p=128)
                            dst = w1bd.ap()[e * 128:(e + 1) * 128, :].rearrange("p (k n) -> p k n", n=2 * D)[:, :, g * D:(g + 1) * D]
                            dn = ('w1bd', (e, g))
                        else:
                            src = w2h.ap()[e].rearrange("(k p) n -> p k n", p=128)
                            dst = w2bd.ap()[e * 128:(e + 1) * 128, :].rearrange("p (k n) -> p k n", n=D)
                            dn = ('w2bd', e)
                        DMA('pool', cw[cb_][:], src, [], [('cw', cb_)])
                        DMA('sp', dst, cw[cb_][:], [('cw', cb_)], [dn])
                for qb in range(8):
                    q0 = qb * 512

                    def emit_qk(kc, n):
                        s_ = n % 2
                        for m in range(2):
                            PE(PS(s_, m), kT[64 * m:64 * m + 64, kc * 128:(kc + 1) * 128], qT[64 * m:64 * m + 64, q0:q0 + 512], True, True,
                               [('dkT', None), ('dqT', None)], [PN(s_, m)], inc=(m == 1))
                        ACT(Pex[n % 3][:], PSUM[s_][:], AF.Exp, [PN(s_, 0), PN(s_, 1)], [('Pex', n % 3)], scale=0.125)

                    emit_qk(0, 0)
                    for kc in range(NT):
                        if kc + 1 < NT:
                            emit_qk(kc + 1, kc + 1)
                        pe_ = Pex[kc % 3]
                        for m in range(2):
                            for sub in range(4):
                                oap, on = oreg(m, sub)
                                r_ = m * 4 + sub
                                PE(oap, pe_[:, m * 512 + sub * 128:m * 512 + (sub + 1) * 128], vd[:, kc, :],
                                   kc == 0 and r_ in (0, 3, 6), kc == NT - 1 and r_ in (2, 5, 7),
                                   [('Pex', kc % 3), ('vd', None), ('vd1', None)], [on], inc=(m == 1 and sub == 3))
                        flush(kc)
                    CP('dve', Ocp[:, 0:387], PS(2, 0)[:, 0:387], [PN(2, 0)], [('Ocp', 0)])
                    CP('dve', Ocp[:, 387:774], PS(2, 1)[:, 0:387], [PN(2, 1)], [('Ocp', 1)])
                    CP('dve', Ocp[:, 774:1032], PS(3, 0)[:, 0:258], [PN(3, 0)], [('Ocp', 2)])

                    def st2(h=h):
                        for sub in range(4):
                            o0 = Ocp[:, sub * 129:(sub + 1) * 129]
                            o1 = Ocp[:, (4 + sub) * 129:(5 + sub) * 129]
                            S.op('dve', nc.vector.reciprocal, dict(out=es4[:, sub:sub + 1], in_=o0[:, 128:129]), [('Ocp', None)], [('es4', (0, sub))])
                            S.op('dve', nc.vector.reciprocal, dict(out=es4[:, 4 + sub:5 + sub], in_=o1[:, 128:129]), [('Ocp', None)], [('es4', (1, sub))])
                            TS('dve', es4[:, 8 + sub:9 + sub], es4[:, 4 + sub:5 + sub], neglam, None, ALU.mult, None, [('es4', (1, sub)), ('lt', 5)], [('es4', (2, sub))])
                            TS('dve', A4[:, sub, :], o0[:, 0:128], es4[:, sub:sub + 1], None, ALU.mult, None, [('Ocp', None), ('es4', (0, sub))], [('A4', sub)])
                            STT('dve', A4[:, sub, :], o1[:, 0:128], es4[:, 8 + sub:9 + sub], A4[:, sub, :], ALU.mult, ALU.add,
                                [('Ocp', None), ('es4', (2, sub)), ('A4', sub)], [('A4', sub)])
                            TTo('pool', sq4[:, sub, :], A4[:, sub, :], A4[:, sub, :], ALU.mult, [('A4', sub)], [('sq4', sub)])
                        S.op('dve', nc.vector.reduce_sum, dict(out=es4[:, 12:16], in_=sq4[:], axis=AX.X), [('sq4', None)], [('es4', 3)])

                    def st3():
                        ACT(es4[:, 16:20], es4[:, 12:16], AF.Ln, [('es4', 3), ('epsb', None)], [('es4', 4)], scale=1.0 / 128, bias=epsb[:, 0:1])
                        ACT(es4[:, 20:24], es4[:, 16:20], AF.Exp, [('es4', 4)], [('es4', 5)], scale=-0.5)

                    def st4(h=h):
                        for sub in range(4):
                            STT('dve', bb4[:, sub, :], A4[:, sub, :], es4[:, 20 + sub:21 + sub], gd[:, h * 128:(h + 1) * 128], ALU.mult, ALU.mult,
                                [('A4', sub), ('es4', 5), ('gd', None)], [('bb4', sub)])

                    def st5():
                        for sub in range(4):
                            PET(PSb(3, 1)[:, sub * 128:(sub + 1) * 128], bb4[:, sub, :], idb[:], [('bb4', sub), ('idb', None)], [PN(3, 1)], inc=(sub == 3))

                    def st6(h=h, qb=qb, q0=q0):
                        sb_ = qb % 2
                        CP('dve', bst[sb_][:], PSb(3, 1)[:, 0:512], [PN(3, 1)], [('bst', sb_)])
                        DMA('sp', bTd.ap()[:, h, q0:q0 + 512], bst[sb_][:], [('bst', sb_)], [('bTd', (h, qb))])

                    pending.extend([(0, st2), (4, st3), (5, st4), (8, st5), (9, st6)])

            flush(None)
            S.barrier()
            S.emit()
        with ExitStack() as st:
            def sb(name, shape, dt=F32):
                return st.enter_context(nc.sbuf_tensor('sb_' + name, shape, dt))
            wgc = [sb("wgc%d" % i, [128, 8, 128], BF16) for i in range(2)]
            sgs = [sb("sgs%d" % i, [128, T], BF16) for i in range(2)]
            for cc in range(16):
                b_ = cc % 2
                DMA('pool', wgc[b_][:], wslice(O_GA + cc * 128, 128), [], [('wgc', b_)])
                for blk in range(8):
                    pi_, pb_ = (blk // 2) % 2, blk % 2
                    for kc in range(8):
                        PE(PS(pi_, pb_), wgc[b_][:, kc, :], uT[:, kc, TC + blk * 512:TC + (blk + 1) * 512], kc == 0, kc == 7,
                           [('wgc', b_), ('uT', None)], [PN(pi_, pb_)], inc=(kc == 7))
                    ACT(sgs[b_][:, blk * 512:(blk + 1) * 512], PS(pi_, pb_), AF.Sigmoid, [PN(pi_, pb_)], [('sgs', b_)])
                DMA('sp', sgd.ap()[:, cc, :], sgs[b_][:], [('sgs', b_)], [('sgd', cc)])
            S.barrier()
            S.emit()
        if stop is None:
            ust.close()

        if stop == 'C2':
            dump('d_bT', bTd.ap(), [128, 8, T], BF16, [('bTd', None)])
            dump('d_sg', sgd.ap(), [128, 16, T], BF16, [('sgd', None)])
            return finish_dbg()
        L = sbg("L", [128, 32, NE])
        with ExitStack() as st:
            G1 = st.enter_context(nc.sbuf_tensor("sb_G1", [128, D], F32))
            DMA('sp', G1[:], bc(modscr, 16 * 128, D), [('modscr', None)], [('G1', None)])
            def sb(name, shape, dt=F32):
                return st.enter_context(nc.sbuf_tensor('sb_' + name, shape, dt))
            wa = sb("wa", [128, 8, D], BF16); wb = sb("wb", [128, 8, D], BF16); wo = sb("wo", [128, 8, D], BF16)
            DMA('pool', wa[:], wah.ap().rearrange("(k p) n -> p k n", p=128), [], [('wa', None)])
            DMA('pool', wb[:], wbh.ap().rearrange("(k p) n -> p k n", p=128), [], [('wb', None)])
            DMA('pool', wo[:], woh.ap().rearrange("(k p) n -> p k n", p=128), [], [('wo', None)])
            rw = sb("rw", [128, 8, NE]); rbb = sb("rbb", [128, NE])
            DMA('sp', rw[:], rwh.ap().rearrange("(k p) n -> p k n", p=128), [], [('rw', None)])
            DMA('sp', rbb[:], bc(rbh, 0, NE), [], [('rbb', None)])
            aTb = [sb("aTb%d" % i, [128, 8, 512], BF16) for i in range(1)] * 2
            bTb = [sb("bTb%d" % i, [128, 8, 512], BF16) for i in range(1)] * 2
            sAb = [sb("sAb%d" % i, [128, 8, 512], BF16) for i in range(1)] * 2
            sBb = [sb("sBb%d" % i, [128, 8, 512], BF16) for i in range(1)] * 2
            yT = [sb("yT%d" % i, [128, 8, 512], BF16) for i in range(1)] * 2
            t1 = [sb("m1_%d" % i, [128, 512]) for i in range(2)]
            t2 = [sb("m2_%d" % i, [128, 512]) for i in range(2)]
            xin = [sb("xi%d" % i, [128, D]) for i in range(2)]
            x1t = [sb("x1t%d" % i, [128, D]) for i in range(2)]
            xn2 = [sb("xn2_%d" % i, [128, D]) for i in range(2)]
            ssb = [sb("ss2_%d" % i, [128, 4]) for i in range(2)]
            u2f = [sb("u2f%d" % i, [128, 8, 128]) for i in range(2)]
            junk = sb("junk4", [128, D], BF16)
            MUL2t = sb("MUL2t", [128, D]); ADD2t = sb("ADD2t", [128, D]); N2Gt = sb("N2Gt", [128, D])
            u2tmp = sb("u2tmp", [128, D]); u2tb = [sb("u2tb%d" % i, [128, D], BF16) for i in range(2)]
            DMA('sp', ADD2t[:], bc(modscr, 3072, D), [('modscr', None)], [('ADD2t', None)])
            DMA('sp', MUL2t[:], bc(modscr, 4096, D), [('modscr', None)], [('MUL2t', None)])
            DMA('sp', N2Gt[:], bc(vecsh, 56 * 128, D), [], [('N2Gt', None)])
            TS('pool', MUL2t[:], MUL2t[:], 1.0, None, ALU.add, None, [('MUL2t', None)], [('MUL2t', None)])
            TTo('pool', MUL2t[:], MUL2t[:], N2Gt[:], ALU.mult, [('MUL2t', None), ('N2Gt', None)], [('MUL2t', None)])
            for tb in range(8):
                b_ = 0
                tsl = slice(tb * 512, (tb + 1) * 512)
                DMA('sp', aTb[b_][:], aTd.ap()[:, :, tsl], [('aTd', None)], [('aTb', b_)])
                DMA('sp', bTb[b_][:], bTd.ap()[:, :, tsl], [('bTd', None)], [('bTb', b_)])
                DMA('sp', sAb[b_][:], sgd.ap()[:, 0:8, tsl], [('sgd', None)], [('sAb', b_)])
                DMA('sp', sBb[b_][:], sgd.ap()[:, 8:16, tsl], [('sgd', None)], [('sBb', b_)])
                for cc in range(8):
                    pi_ = cc % 2
                    for (pb_, w_, w_n, src, srcn) in ((0, wa, 'wa', aTb, 'aTb'), (1, wb, 'wb', bTb, 'bTb')):
                        for kc in range(8):
                            PE(PS(pi_, pb_), w_[:, kc, cc * 128:(cc + 1) * 128], src[b_][:, kc, :], kc == 0, kc == 7,
                               [(w_n, None), (srcn, b_)], [PN(pi_, pb_)], inc=(kc == 7))
                    TTo('dve', t1[pi_][:], PS(pi_, 0), sAb[b_][:, cc, :], ALU.mult, [PN(pi_, 0), ('sAb', b_)], [('m1', pi_)])
                    TTo('dve', t2[pi_][:], PS(pi_, 1), sBb[b_][:, cc, :], ALU.mult, [PN(pi_, 1), ('sBb', b_)], [('m2', pi_)])
                    TTo('pool', yT[b_][:, cc, :], t1[pi_][:], t2[pi_][:], ALU.add, [('m1', pi_), ('m2', pi_)], [('yT', (b_, cc))])
                for sub in range(4):
                    tt = tb * 4 + sub
                    xb_ = tt % 2
                    DMA('sp', xin[xb_][:], xh.ap()[tt * 128:(tt + 1) * 128, :], [], [('xi', xb_)])
                    for half in range(2):
                        for cc in range(8):
                            PE(PS(2, half), yT[b_][:, cc, sub * 128:(sub + 1) * 128], wo[:, cc, half * 512:(half + 1) * 512], cc == 0, cc == 7,
                               [('yT', (b_, cc)), ('wo', None)], [PN(2, half)], inc=(cc == 7))
                        hs_ = slice(half * 512, (half + 1) * 512)
                        TTo('dve', x1t[xb_][:, hs_], PS(2, half), G1[:, hs_], ALU.mult, [PN(2, half), ('G1', None)], [('x1t', (xb_, half))])
                        TTo('pool', x1t[xb_][:, hs_], x1t[xb_][:, hs_], xin[xb_][:, hs_], ALU.add,
                            [('x1t', (xb_, half)), ('xi', xb_)], [('x1t', (xb_, half))])
                    DMA('sp', x1d.ap()[tt * 128:(tt + 1) * 128, :], x1t[xb_][:], [('x1t', (xb_, 0)), ('x1t', (xb_, 1))], [('x1d', tt)])
                    sn = 'ss2_%d' % xb_
                    ACT(junk[:], x1t[xb_][:], AF.Square, [('x1t', (xb_, 0)), ('x1t', (xb_, 1))], [('junk4', None), (sn, 0)], accum_out=ssb[xb_][:, 0:1])
                    ACT(ssb[xb_][:, 1:2], ssb[xb_][:, 0:1], AF.Ln, [(sn, 0), ('epsb', None)], [(sn, 1)], scale=1.0 / D, bias=epsb[:, 0:1])
                    ACT(ssb[xb_][:, 2:3], ssb[xb_][:, 1:2], AF.Exp, [(sn, 1)], [(sn, 2)], scale=-0.5)
                    TS('dve', xn2[xb_][:], x1t[xb_][:], ssb[xb_][:, 2:3], None, ALU.mult, None,
                       [('x1t', (xb_, 0)), ('x1t', (xb_, 1)), (sn, 2)], [('xn2', xb_)])
                    for g in range(2):
                        for j in range(4):
                            kc = g * 4 + j
                            PET(PS(3, g)[:, j * 128:(j + 1) * 128], xn2[xb_][:, kc * 128:(kc + 1) * 128], idf[:],
                                [('xn2', xb_), ('idf', None)], [PN(3, g)], inc=(j == 3))
                        for j in range(4):
                            kc = g * 4 + j
                            o = u2f[xb_][:, kc, :]
                            i_ = PS(3, g)[:, j * 128:(j + 1) * 128]
                            if j % 2 == 0:
                                TS('dve', o, i_, prm[:, 4, kc:kc + 1], prm[:, 5, kc:kc + 1], ALU.mult, ALU.add,
                                   [PN(3, g), ('prm', 4), ('prm', 5)], [('u2f', (xb_, kc))])
                            else:
                                ACT(o, i_, AF.Identity, [PN(3, g), ('prm', 4), ('prm', 5)], [('u2f', (xb_, kc))],
                                    scale=prm[:, 4, kc:kc + 1], bias=prm[:, 5, kc:kc + 1])
                    TTo('pool', u2tmp[:], xn2[xb_][:], MUL2t[:], ALU.mult, [('xn2', xb_), ('MUL2t', None)], [('u2tmp', None)])
                    TTo('pool', u2tb[xb_][:], u2tmp[:], ADD2t[:], ALU.add, [('u2tmp', None), ('ADD2t', None)], [('u2tb', xb_)])
                    DMA('sp', u2tokd.ap()[tt * 128:(tt + 1) * 128, :], u2tb[xb_][:], [('u2tb', xb_)], [('u2tokd', tt)])
                    for kc in range(8):
                        PE(PS(3, 1)[:, 0:NE], u2f[xb_][:, kc, :], rw[:, kc, :], kc == 0, kc == 7,
                           [('u2f', (xb_, kc)), ('rw', None)], [PN(3, 1)], inc=(kc == 7))
                    TTo('dve', L[:, tt, :], PS(3, 1)[:, 0:NE], rbb[:], ALU.add, [PN(3, 1), ('rbb', None)], [('L', tt)])
            S.barrier()
            S.emit()

        if stop == 'D':
            dump('d_x1', x1d.ap(), [T, D], F32, [('x1d', None)])
            dump('d_L', L[:], [128, 32, NE], F32, [('L', None)])
            return finish_dbg()
        IOA = bass.IndirectOffsetOnAxis
        with ExitStack() as st:
            def sb(name, shape, dt=F32):
                return st.enter_context(nc.sbuf_tensor('sb_' + name, shape, dt))
            G2 = sb("G2", [128, D])
            DMA('sp', G2[:], bc(modscr, 40 * 128, D), [('modscr', None)], [('G2', None)])
            stri = sb("stri", [128, 128]); thrB = sb("thrB", [128, NBLK]); pidx = sb("pidx", [128, 1])
            DMA('sp', stri[:], strih.ap(), [], [('stri', None)])
            DMA('sp', thrB[:], thrBh.ap(), [], [('thrB', None)])
            DMA('sp', pidx[:], pidxh.ap(), [], [('pidx', None)])
            gate = sb("gate", [128, 32, NE]); MK = sb("MK", [128, 32, NE]); POS = sb("POS", [128, 32, NE])
            m8 = [sb("m8_%d" % i, [128, 16]) for i in range(2)]
            ex = [sb("ex%d" % i, [128, NE]) for i in range(2)]
            b1in = sb("b1in", [128, 4, 128]); b1T = sb("b1T", [128, 512]); b2s = sb("b2s", [NE, D]); fg = sb("fg", [128, D])
            DMA('sp', b1in[:], b1h.ap().rearrange("(a p) n -> p a n", p=128), [], [('b1in', None)])
            DMA('sp', b2s[:], b2h.ap(), [], [('b2s', None)])
            DMA('sp', fg[:], bc(fngh, 0, D), [], [('fg', None)])
            for a_ in range(4):
                PET(PS(0, 0)[:, a_ * 128:(a_ + 1) * 128], b1in[:, a_, :], idf[:], [('b1in', None), ('idf', None)], [PN(0, 0)], inc=(a_ == 3))
            CP('dve', b1T[:], PS(0, 0), [PN(0, 0)], [('b1T', None)])
            b1v = b1T[:].rearrange("p (e j) -> p e j", j=16)
            TS('dve', b1v[:, :, 8:16], b1v[:, :, 8:16], 1.0, None, ALU.add, None, [('b1T', None)], [('b1T', None)])
            DMA('sp', b1Td.ap().rearrange("(e p) j -> p e j", p=128), b1T[:].rearrange("p (e j) -> p e j", j=16), [('b1T', None)], [('b1Td', None)])
            base = sb("base", [128, NE])
            S.op('dve', nc.vector.memset, dict(ap=base[:], constant=0.0), [], [('base', None)])
            for tt in range(32):
                b_ = tt % 2
                mn = 'm8_%d' % b_
                S.op('dve', nc.vector.max, dict(out=m8[b_][:, 0:8], in_=L[:, tt, :]), [('L', tt)], [(mn, 0)])
                TS('dve', MK[:, tt, :], L[:, tt, :], m8[b_][:, 3:4], None, ALU.is_ge, None, [('L', tt), (mn, 0)], [('MK', tt)])
                TS('dve', m8[b_][:, 8:9], m8[b_][:, 0:1], -1.0, None, ALU.mult, None, [(mn, 0)], [(mn, 1)])
                ACT(ex[b_][:], L[:, tt, :], AF.Exp, [('L', tt), (mn, 1)], [('ex', b_)], bias=m8[b_][:, 8:9])
                TTo('dve', ex[b_][:], ex[b_][:], MK[:, tt, :], ALU.mult, [('ex', b_), ('MK', tt)], [('ex', b_)])
                S.op('dve', nc.vector.reduce_sum, dict(out=m8[b_][:, 9:10], in_=ex[b_][:], axis=AX.X), [('ex', b_)], [(mn, 2)])
                S.op('dve', nc.vector.reciprocal, dict(out=m8[b_][:, 10:11], in_=m8[b_][:, 9:10]), [(mn, 2)], [(mn, 3)])
                TS('dve', gate[:, tt, :], ex[b_][:], m8[b_][:, 10:11], None, ALU.mult, None, [('ex', b_), (mn, 3)], [('gate', tt)])
                PE(PS(0, b_)[:, 0:NE], stri[:], MK[:, tt, :], True, True, [('stri', None), ('MK', tt)], [PN(0, b_)])
                PE(PS(0, b_)[:, NE:2 * NE], ones[:], MK[:, tt, :], True, True, [('ones', None), ('MK', tt)], [PN(0, b_)])
                TTo('dve', POS[:, tt, :], PS(0, b_)[:, 0:NE], base[:], ALU.add, [PN(0, b_), ('base', None)], [('POS', tt)])
                TTo('dve', base[:], base[:], PS(0, b_)[:, NE:2 * NE], ALU.add, [PN(0, b_), ('base', None)], [('base', None)])
            nb = sb("nb", [128, NE]); cA = sb("cA", [128, NE]); cB = sb("cB", [128, NE]); pst = sb("pst", [128, NE])
            S.op('dve', nc.vector.memset, dict(ap=nb[:], constant=0.0), [], [('nb', None)])
            for k in range(T // BLK):
                STT('dve', nb[:], base[:], float(BLK) * k, nb[:], ALU.is_gt, ALU.add, [('base', None), ('nb', None)], [('nb', None)])
            TS('dve', nb[:], nb[:], float(BLK), None, ALU.mult, None, [('nb', None)], [('nb', None)])
            CP('dve', cA[:], nb[:], [('nb', None)], [('cA', None)])
            cur, oth, cn, on = cA, cB, 'cA', 'cB'
            for sh in (1, 2, 4, 8, 16):
                CP('dve', oth[:, 0:sh], cur[:, 0:sh], [(cn, None)], [(on, 0)])
                TTo('dve', oth[:, sh:NE], cur[:, sh:NE], cur[:, 0:NE - sh], ALU.add, [(cn, None)], [(on, 1)])
                cur, oth, cn, on = oth, cur, on, cn
            pend, pendn = cur, cn
            TTo('dve', pst[:], pend[:], nb[:], ALU.subtract, [(pendn, None), ('nb', None)], [('pst', None)])
            D4 = sb("D4", [128, 32, 4], mybir.dt.int32); g4 = sb("g4", [128, 32, 4])
            key = [sb("key%d" % i, [128, NE]) for i in range(2)]
            oh = [sb("oh%d" % i, [128, NE]) for i in range(2)]
            k8 = [sb("k8_%d" % i, [128, 8]) for i in range(2)]
            for tt in range(32):
                b_ = tt % 2
                kn, k8n = 'key%d' % b_, 'k8_%d' % b_
                TTo('dve', POS[:, tt, :], POS[:, tt, :], pst[:], ALU.add, [('POS', tt), ('pst', None)], [('POS', tt)])
                STT('dve', key[b_][:], POS[:, tt, :], 1.0, MK[:, tt, :], ALU.add, ALU.mult, [('POS', tt), ('MK', tt)], [(kn, None)])
                S.op('dve', nc.vector.max, dict(out=k8[b_][:], in_=key[b_][:]), [(kn, None)], [(k8n, None)])
                TS('dve', D4[:, tt, :], k8[b_][:, 0:4], -1.0, None, ALU.add, None, [(k8n, None)], [('D4', tt)])
                for j in range(4):
                    on_ = 'oh%d' % (j % 2)
                    TS('dve', oh[j % 2][:], key[b_][:], k8[b_][:, j:j + 1], None, ALU.is_equal, None, [(kn, None), (k8n, None)], [(on_, None)])
                    TTo('dve', oh[j % 2][:], oh[j % 2][:], gate[:, tt, :], ALU.mult, [(on_, None), ('gate', tt)], [(on_, None)])
                    S.op('dve', nc.vector.reduce_sum, dict(out=g4[:, tt, j:j + 1], in_=oh[j % 2][:], axis=AX.X), [(on_, None)], [('g4', (tt, j))])
            be = sb("be", [128, NBLK]); OFFS = sb("OFFS", [128, NBLK], mybir.dt.int32)
            S.op('dve', nc.vector.memset, dict(ap=be[:], constant=0.0), [], [('be', None)])
            for e in range(NE):
                STT('dve', be[:], thrB[:], pend[:, e:e + 1], be[:], ALU.is_ge, ALU.add, [('thrB', None), (pendn, None), ('be', None)], [('be', None)])
            TS('dve', be[:], be[:], float(NE - 1), 128.0, ALU.min, ALU.mult, [('be', None)], [('be', None)])
            TS('dve', OFFS[:], be[:], pidx[:, 0:1], None, ALU.add, None, [('be', None), ('pidx', None)], [('OFFS', None)])
            ut = [sb("ut%d" % i, [128, D], BF16) for i in range(2)]
            for tt in range(32):
                b_ = tt % 2
                DMA('sp', ut[b_][:], u2tokd.ap()[tt * 128:(tt + 1) * 128, :], [('u2tokd', tt)], [('ut', b_)])
                for j in range(4):
                    S.dma('pool', dict(out=Xg.ap(), out_offset=IOA(ap=D4[:, tt, j:j + 1], axis=0), in_=ut[b_][:], in_offset=None),
                          [('ut', b_), ('D4', tt)], [('Xg', (tt, j))], method=nc.gpsimd.indirect_dma_start)

            with ExitStack() as st2:
                def sb2(name, shape, dt=F32):
                    return st2.enter_context(nc.sbuf_tensor('sb_' + name, shape, dt))
                w1s = [sb2("w1s%d" % i, [128, 8 * 2 * D], BF16) for i in range(2)]
                w2s = [sb2("w2s%d" % i, [128, 8 * D], BF16) for i in range(2)]
                b1g = [sb2("b1g%d" % i, [128, 16]) for i in range(2)]
                xg = [sb2("xg%d" % i, [128, D], BF16) for i in range(4)]
                xT = [sb2("xT%d" % i, [128, 8, BLK], BF16) for i in range(2)]
                actT = [sb2("actT%d" % i, [128, 8, BLK], BF16) for i in range(2)]
                hg = [sb2("hg%d" % i, [128, BLK]) for i in range(2)]
                hl = [sb2("hl%d" % i, [128, BLK]) for i in range(2)]
                yst = [sb2("yst%d" % i, [128, D]) for i in range(2)]

                def load_w(b):
                    wb_ = b % 2
                    off = IOA(ap=OFFS[:, b:b + 1], axis=0)
                    for (dst, dn, src, sn) in ((w1s, 'w1s', w1bd, 'w1bd'), (w2s, 'w2s', w2bd, 'w2bd'), (b1g, 'b1g', b1Td, 'b1Td')):
                        S.dma('pool', dict(out=dst[wb_][:], out_offset=None, in_=src.ap(), in_offset=off),
                              [('OFFS', None), (sn, None)], [(dn, wb_)], method=nc.gpsimd.indirect_dma_start)

                def load_x(b):
                    for sub in range(2):
                        t_ = 2 * b + sub
                        xi_ = 2 * (b % 2) + sub
                        DMA('sp', xg[xi_][:], Xg.ap()[t_ * 128:(t_ + 1) * 128, :], [('Xg', None)], [('xg', xi_)])

                load_w(0)
                ycnt = 0
                for b in range(NBLK):
                    wb_ = b % 2
                    if b + 1 < NBLK:
                        load_w(b + 1)
                    if b == 0:
                        load_x(0)
                    if b + 1 < NBLK:
                        load_x(b + 1)
                    for sub in range(2):
                        t_ = 2 * b + sub
                        xi_ = 2 * (b % 2) + sub
                        for kc in range(8):
                            PET(PSb(3, 0)[:, kc * 128:(kc + 1) * 128], xg[xi_][:, kc * 128:(kc + 1) * 128], idb[:], [('xg', xi_), ('idb', None)], [PN(3, 0)], inc=(kc == 7))
                        CP('act', xT[wb_][:, :, sub * 128:(sub + 1) * 128], PSb(3, 0).rearrange("p (k n) -> p k n", n=128), [PN(3, 0)], [('xT', (wb_, sub))])
                    for cp in range(8):
                        pi_ = cp % 2
                        for gl in range(2):
                            for kc in range(8):
                                c0 = kc * 2 * D + gl * D + cp * 128
                                PE(PS(pi_, gl)[:, 0:BLK], w1s[wb_][:, c0:c0 + 128], xT[wb_][:, kc, :], kc == 0, kc == 7,
                                   [('w1s', wb_), ('xT', (wb_, 0)), ('xT', (wb_, 1))], [PN(pi_, gl)], inc=(kc == 7))
                        bg = b1g[wb_][:, cp:cp + 1]
                        bl = b1g[wb_][:, 8 + cp:9 + cp]
                        TS('dve', hg[pi_][:], PS(pi_, 0)[:, 0:BLK], bg, 7.0, ALU.add, ALU.min, [PN(pi_, 0), ('b1g', wb_)], [('hg', pi_)])
                        ACT(hg[pi_][:], hg[pi_][:], AF.Silu, [('hg', pi_)], [('hg', pi_)], scale=1.702)
                        TS('dve', hl[pi_][:], PS(pi_, 1)[:, 0:BLK], bl, 8.0, ALU.add, ALU.min, [PN(pi_, 1), ('b1g', wb_)], [('hl', pi_)])
                        STT('dve', actT[wb_][:, cp, :], hl[pi_][:], -6.0, hg[pi_][:], ALU.max, ALU.mult, [('hg', pi_), ('hl', pi_)], [('actT', (wb_, cp))])
                    for sub in range(2):
                        t_ = 2 * b + sub
                        ys_ = ycnt % 2
                        ycnt += 1
                        for half in range(2):
                            for cp in range(8):
                                c0 = cp * D + half * 512
                                PE(PS(2, half), actT[wb_][:, cp, sub * 128:(sub + 1) * 128], w2s[wb_][:, c0:c0 + 512], cp == 0, cp == 7,
                                   [('actT', (wb_, cp)), ('w2s', wb_)], [PN(2, half)], inc=(cp == 7))
                            hs_ = slice(half * 512, (half + 1) * 512)
                            if half == 0:
                                ACT(yst[ys_][:, hs_], PS(2, half), AF.Copy, [PN(2, half)], [('yst', (ys_, half))], scale=1.0 / 1.702)
                            else:
                                TS('dve', yst[ys_][:, hs_], PS(2, half), 1.0 / 1.702, None, ALU.mult, None, [PN(2, half)], [('yst', (ys_, half))])
                        DMA('sp', Yg.ap()[t_ * 128:(t_ + 1) * 128, :], yst[ys_][:], [('yst', (ys_, 0)), ('yst', (ys_, 1))], [('Yg', t_)])


                S.barrier()
                S.emit()

            yg = [[sb("yg%d_%d" % (i, j), [128, D]) for j in range(4)] for i in range(2)]
            Yacc = [sb("Yacc%d" % i, [128, D]) for i in range(2)]; gTt = [sb("gTt%d" % i, [NE, 128]) for i in range(2)]
            x1l = [sb("x1l%d" % i, [128, D]) for i in range(2)]; ot = [sb("ot%d" % i, [128, D]) for i in range(2)]
            ssf = [sb("ssf%d" % i, [128, 4]) for i in range(2)]
            for tt in range(32):
                b_ = tt % 2
                ya, x1, o_, sf, gt = Yacc[b_], x1l[b_], ot[b_], ssf[b_], gTt[b_]
                yn, xn_, on_, sn, gn = ('Yacc', b_), ('x1l', b_), ('ot', b_), 'ssf%d' % b_, ('gTt', b_)
                for j in range(4):
                    S.dma('pool', dict(out=yg[b_][j][:], out_offset=None, in_=Yg.ap(), in_offset=IOA(ap=D4[:, tt, j:j + 1], axis=0)),
                          [('D4', tt), ('Yg', None)], [('yg', (b_, j))], method=nc.gpsimd.indirect_dma_start)
                DMA('sp', x1[:], x1d.ap()[tt * 128:(tt + 1) * 128, :], [('x1d', tt)], [xn_])
                TS('dve', ya[:], yg[b_][0][:], g4[:, tt, 0:1], None, ALU.mult, None, [('yg', (b_, 0)), ('g4', (tt, 0))], [yn])
                for j in range(1, 4):
                    STT('dve', ya[:], yg[b_][j][:], g4[:, tt, j:j + 1], ya[:], ALU.mult, ALU.add,
                        [('yg', (b_, j)), ('g4', (tt, j)), yn], [yn])
                PET(PS(3, b_)[0:NE, 0:128], gate[:, tt, :], idf[:], [('gate', tt), ('idf', None)], [PN(3, b_)])
                CP('act', gt[:], PS(3, b_)[0:NE, 0:128], [PN(3, b_)], [gn])
                for half in range(2):
                    hs_ = slice(half * 512, (half + 1) * 512)
                    PE(PS(b_, half), gt[:], b2s[:, hs_], True, True, [gn, ('b2s', None)], [PN(b_, half)])
                    TTo('dve', ya[:, hs_], ya[:, hs_], PS(b_, half), ALU.add, [PN(b_, half), yn], [yn])
                TTo('dve', ya[:], ya[:], G2[:], ALU.mult, [yn, ('G2', None)], [yn])
                TTo('dve', x1[:], x1[:], ya[:], ALU.add, [xn_, yn], [xn_])
                ACT(o_[:], x1[:], AF.Square, [xn_], [on_, (sn, 0)], accum_out=sf[:, 0:1])
                ACT(sf[:, 1:2], sf[:, 0:1], AF.Ln, [(sn, 0), ('epsb', None)], [(sn, 1)], scale=1.0 / D, bias=epsb[:, 0:1])
                ACT(sf[:, 2:3], sf[:, 1:2], AF.Exp, [(sn, 1)], [(sn, 2)], scale=-0.5)
                STT('dve', o_[:], x1[:], sf[:, 2:3], fg[:], ALU.mult, ALU.mult, [xn_, (sn, 2), ('fg', None)], [on_])
                DMA('sp', yh.ap()[tt * 128:(tt + 1) * 128, :], o_[:], [on_], [('y', tt)])
            S.barrier()
            S.emit()
        print("ninstr", S.ninstr)
    return nc


def _consts():
    idf = np.eye(128, dtype=np.float32)
    s = np.arange(128)
    triu = (s[:, None] <= s[None, :]).astype(np.float32)
    tril = (s[:, None] >= s[None, :]).astype(np.float32)
    ones = np.ones((128, 128), np.float32)
    t = np.arange(T)
    row = (t // 64).astype(np.float64)
    col = (t % 64).astype(np.float64)
    inv = 10000.0 ** (-np.arange(16, dtype=np.float64) / 16.0)
    inv32 = inv.astype(np.float32).astype(np.float64)
    cost = np.zeros((128, T), np.float32)
    sint = np.zeros((128, T), np.float32)
    for p in range(128):
        d = p % 64
        pos = row if d < 32 else col
        j = d % 16
        ang = (pos.astype(np.float32) * np.float32(inv32[j])).astype(np.float32)
        first = (d % 32) < 16
        cost[p] = np.cos(ang)
        sint[p] = -np.sin(ang) if first else np.sin(ang)
    stri = (s[:, None] < s[None, :]).astype(np.float32)
    thrB = np.tile((float(BLK) * np.arange(NBLK, dtype=np.float32))[None, :], (128, 1))
    pidx = np.arange(128, dtype=np.float32).reshape(128, 1)
    return dict(idf=idf, idb=idf.astype(ml_dtypes.bfloat16), triu=triu, tril=tril, ones=ones, cost=cost, sint=sint,
                stri=stri, thrB=np.ascontiguousarray(thrB), pidx=pidx)


def _in_maps(x, c, ctx, c_ctx, ada_w, ada_b, norm1_g, norm2_g, w_in, mlstm_gate_b, mlstm_norm_g,
             diff_lambda, diff_norm_g, w_branch_a, w_branch_b, w_out, router_w, router_b,
             exp_w1, exp_b1, exp_w2, exp_b2, final_norm_g):
    f = lambda a: np.ascontiguousarray(np.asarray(a, dtype=np.float32))
    cons = _consts()
    vecs = np.concatenate([f(ada_b)[0].reshape(48, 128), f(norm1_g)[0].reshape(8, 128), f(norm2_g)[0].reshape(8, 128)], axis=0)
    shared = dict(
        ada_w=f(ada_w)[0], vecs=f(vecs), b1r=f(exp_b1)[0].reshape(512, 128), w_in=f(w_in)[0], gate_b=f(mlstm_gate_b)[0],
        mng=f(mlstm_norm_g)[0].reshape(D), dng=f(diff_norm_g)[0].reshape(D), fng=f(final_norm_g), dlam=f(diff_lambda)[0].reshape(256),
        w_a=f(w_branch_a)[0], w_b=f(w_branch_b)[0], w_o=f(w_out)[0], rw=f(router_w)[0], rb=f(router_b)[0],
        w1=f(exp_w1)[0], w2=f(exp_w2)[0], b2=f(exp_b2)[0], **cons)
    maps = []
    xf, cf, ctxf, ccf = f(x), f(c), f(ctx), f(c_ctx)
    for b in range(8):
        m = dict(shared)
        m["x"] = xf[b]
        m["ctx"] = ctxf[b]
        m["cvec"] = np.ascontiguousarray(np.stack([cf[b], ccf], axis=1))
        maps.append(m)
    return maps


def kernel(**inputs):
    maps = _in_maps(**inputs)
    nc = build_nc()
    res = run_bass_kernel_spmd(nc, maps, core_ids=list(range(8)))
    return np.stack([np.asarray(r["y"], dtype=np.float32) for r in res.results], axis=0)
```

```python
import math
import os
from contextlib import ExitStack

import ml_dtypes
import numpy as np

import concourse.bass as bass
import concourse.mybir as mybir
from concourse.bass_utils import run_bass_kernel_spmd

F32 = mybir.dt.float32
BF16 = mybir.dt.bfloat16
ALU = mybir.AluOpType
AF = mybir.ActivationFunctionType
AX = mybir.AxisListType

D = 1024
T = 4096
TC = 256
TT = T + TC
NT = TT // 128
NE = 32
BLK = 256
NBLK = 96
NBUF = NBLK * BLK
EPS = 1e-6
LAM_INIT = 0.8 - 0.6 * math.exp(0.0)
O_MQ, O_MK, O_MV, O_MG, O_MO, O_DQ, O_DK, O_DV, O_GA, O_GB = 0, 512, 1024, 2048, 2064, 3088, 4112, 5136, 6160, 7184
INC = 8208

ENG_ATTR = {'pe': 'tensor', 'act': 'scalar', 'dve': 'vector', 'pool': 'gpsimd', 'sp': 'sync'}
NDMA_SEMS = 8
NPOOL_SEMS = 4


class Sched:
    def __init__(self, nc, stack):
        self.nc = nc
        self.prog = {e: [] for e in ENG_ATTR}
        self.sem = {}
        for e in ENG_ATTR:
            self.sem[e] = stack.enter_context(nc.semaphore('s_' + e))
        self.dq = {}
        for q in ('sp', 'pool'):
            for i in range(NDMA_SEMS):
                k = ('dma', q, i)
                self.sem[k] = stack.enter_context(nc.semaphore('d_%s%d' % (q, i)))
            self.dq[q] = 0
        self.count = {k: 0 for k in self.sem}
        self.seen = {e: {} for e in ENG_ATTR}
        self.state = {}
        self.ninstr = 0

    @staticmethod
    def _ov(a, b):
        return a is None or b is None or a == b

    def _collect(self, eng, reads, writes, is_dma):
        need = {}

        def add(ev, kind):
            if ev is None:
                return
            k, v = ev
            if (not is_dma) and k == eng:
                if kind == 'rar' or (eng == 'pe' and kind != 'raw'):
                    return
            if need.get(k, 0) < v:
                need[k] = v

        for (n, s) in reads:
            for slot, st in self.state.get(n, {}).items():
                if self._ov(slot, s):
                    add(st[0], 'raw')
                    if n.startswith('PS'):
                        for r in st[1]:
                            add(r, 'rar')
        for (n, s) in writes:
            for slot, st in self.state.get(n, {}).items():
                if self._ov(slot, s):
                    add(st[0], 'waw')
                    for r in st[1]:
                        add(r, 'war')
        return need

    def _emit_waits(self, eng, need):
        seen = self.seen[eng]
        for k, v in need.items():
            if seen.get(k, 0) >= v:
                continue
            seen[k] = v
            self.prog[eng].append(('wait', self.sem[k], v))

    def _mark(self, ev, reads, writes):
        for (n, s) in writes:
            d = self.state.setdefault(n, {})
            if s is None:
                d.clear()
                d[None] = [ev, []]
            else:
                d[s] = [ev, []]
        for (n, s) in reads:
            d = self.state.setdefault(n, {})
            st = d.setdefault(s, [None, []])
            st[1] = [r for r in st[1] if r[0] != ev[0]] + [ev]

    def op(self, eng, method, kw, reads=(), writes=(), inc=True):
        need = self._collect(eng, reads, writes, False)
        self._emit_waits(eng, need)
        self.ninstr += 1
        if inc:
            self.count[eng] += 1
            ev = (eng, self.count[eng])
            self.prog[eng].append(('ins', method, kw, self.sem[eng], 1))
        else:
            ev = (eng, self.count[eng] + 1)
            self.prog[eng].append(('ins', method, kw, None, 0))
        self._mark(ev, reads, writes)

    def dma(self, q, kw, reads=(), writes=(), method=None):
        nsem = NDMA_SEMS if q != 'pool' else NPOOL_SEMS
        i = self.dq[q] % nsem
        self.dq[q] += 1
        k = ('dma', q, i)
        need = self._collect(q, reads, writes, True)
        if self.count[k] > 0:
            need[k] = max(need.get(k, 0), self.count[k])
        self._emit_waits(q, need)
        self.ninstr += 1
        self.count[k] += 16
        ev = (k, self.count[k])
        if method is None:
            method = getattr(self.nc, ENG_ATTR[q]).dma_start
        self.prog[q].append(('ins', method, kw, self.sem[k], 16))
        self._mark(ev, reads, writes)

    def barrier(self):
        for e in ENG_ATTR:
            need = {k: v for k, v in self.count.items() if v > 0 and k != e}
            self._emit_waits(e, need)

    def emit(self):
        nc = self.nc
        if os.environ.get('DBGPROG'):
            names = {id(v): k for k, v in self.sem.items()}
            for e in ENG_ATTR:
                print('ENGINE', e)
                c = 0
                for it in self.prog[e][-int(os.environ['DBGPROG']):]:
                    if it[0] == 'wait':
                        print('   wait', names[id(it[1])], it[2])
                    else:
                        print('   ins', getattr(it[1], '__name__', it[1]), 'inc' if it[3] is not None else '-', [k for k in it[2] if k in ('func',)] and it[2].get('func'))
        with nc.Block() as block:
            for e, attr in ENG_ATTR.items():
                prog = self.prog[e]

                def body(engine, prog=prog):
                    for it in prog:
                        if it[0] == 'wait':
                            engine.wait_ge(it[1], it[2])
                        else:
                            ins = it[1](**it[2])
                            if it[3] is not None:
                                ins.then_inc(it[3], it[4])
                getattr(block, attr)(body)
        self.prog = {e: [] for e in ENG_ATTR}


def build_nc(dbg=False, stop=None):
    nc = bass.Bass("TRN2", target_bir_lowering=False)

    def din(name, shape, dt=F32):
        return nc.dram_tensor(name, shape, dt, kind="ExternalInput")

    def dscr(name, shape, dt=F32):
        return nc.dram_tensor(name, shape, dt, kind="Internal")

    xh = din("x", [T, D]); ctxh = din("ctx", [TC, D]); cvh = din("cvec", [D, 2])
    adawh = din("ada_w", [D, 6 * D]); vecsh = din("vecs", [64, 128]); b1h = din("b1r", [512, 128])
    winh = din("w_in", [D, INC]); gbh = din("gate_b", [16]); mngh = din("mng", [D]); dngh = din("dng", [D])
    fngh = din("fng", [D]); dlh = din("dlam", [256]); wah = din("w_a", [D, D]); wbh = din("w_b", [D, D])
    woh = din("w_o", [D, D]); rwh = din("rw", [D, NE]); rbh = din("rb", [NE])
    w1h = din("w1", [NE, D, 2 * D]); w2h = din("w2", [NE, D, D]); b2h = din("b2", [NE, D])
    idfh = din("idf", [128, 128]); idbh = din("idb", [128, 128], BF16)
    triuh = din("triu", [128, 128]); trilh = din("tril", [128, 128]); onesh = din("ones", [128, 128])
    cosh_ = din("cost", [128, T]); sinh_ = din("sint", [128, T])
    strih = din("stri", [128, 128]); thrBh = din("thrB", [128, NBLK]); pidxh = din("pidx", [128, 1])
    yh = nc.dram_tensor("y", [T, D], F32, kind="ExternalOutput")

    modscr = dscr("modscr", [48 * 128]); hfscr = dscr("hfscr", [4, 32, 128, 256])
    aTd = dscr("aTd", [128, 8, T], BF16); bTd = dscr("bTd", [128, 8, T], BF16)
    sgd = dscr("sgd", [128, 16, T], BF16); x1d = dscr("x1d", [T, D]); u2d = dscr("u2d", [128, 8, T], BF16)
    w1bd = dscr("w1bd", [NE * 128, 8 * 2 * D], BF16); w2bd = dscr("w2bd", [NE * 128, 8 * D], BF16)
    b1Td = dscr("b1Td", [NE * 128, 16]); u2tokd = dscr("u2tokd", [T, D], BF16)
    Xg = dscr("Xg", [NBUF, D], BF16); Yg = dscr("Yg", [NBUF, D])

    def bc(h, off, n, parts=128):
        return bass.AP(h, off, [[0, parts], [1, n]])

    with ExitStack() as gst:
        S = Sched(nc, gst)

        def sbg(name, shape, dt=F32):
            return gst.enter_context(nc.sbuf_tensor('sb_' + name, shape, dt))

        PSUM = [gst.enter_context(nc.psum_tensor("PS%d" % i, [128, 1024], F32)) for i in range(4)]

        def PS(i, b):
            return PSUM[i][:, b * 512:(b + 1) * 512]

        def PSb(i, b):
            return PSUM[i][:].bitcast(BF16)[:, b * 1024:(b + 1) * 1024]

        def PN(i, b):
            return ('PS%d' % i, b)

        def PE(out, lhsT, rhs, start, stop, R, W, inc=True):
            S.op('pe', nc.tensor.matmul, dict(out=out, lhsT=lhsT, rhs=rhs, start=start, stop=stop), R, W, inc)

        def PET(out, in_, ident, R, W, inc=True):
            S.op('pe', nc.tensor.transpose, dict(out=out, in_=in_, identity=ident), R, W, inc)

        def ACT(out, in_, func, R, W, **kw):
            S.op('act', nc.scalar.activation, dict(out=out, in_=in_, func=func, **kw), R, W)

        def TS(eng, out, in0, s1, s2, op0, op1, R, W):
            m = nc.vector.tensor_scalar if eng == 'dve' else nc.gpsimd.tensor_scalar
            kw = dict(out=out, in0=in0, scalar1=s1, scalar2=s2, op0=op0)
            if op1 is not None:
                kw['op1'] = op1
            S.op(eng, m, kw, R, W)

        def TTo(eng, out, in0, in1, op, R, W):
            m = nc.vector.tensor_tensor if eng == 'dve' else nc.gpsimd.tensor_tensor
            S.op(eng, m, dict(out=out, in0=in0, in1=in1, op=op), R, W)

        def STT(eng, out, in0, scalar, in1, op0, op1, R, W):
            m = nc.vector.scalar_tensor_tensor if eng == 'dve' else nc.gpsimd.scalar_tensor_tensor
            S.op(eng, m, dict(out=out, in0=in0, scalar=scalar, in1=in1, op0=op0, op1=op1), R, W)

        def CP(eng, out, in_, R, W):
            if eng == 'act':
                ACT(out, in_, AF.Copy, R, W)
            else:
                m = nc.vector.tensor_copy if eng == 'dve' else nc.gpsimd.tensor_copy
                S.op(eng, m, dict(out=out, in_=in_), R, W)

        def DMA(q, out, in_, R, W):
            S.dma(q, dict(out=out, in_=in_), R, W)


        def dump(name, src, shape, dt, reads):
            h_ = nc.dram_tensor(name, shape, dt, kind="ExternalOutput")
            DMA('sp', h_.ap(), src, reads, [(name, None)])

        def finish_dbg():
            S.barrier()
            S.emit()
            return nc
        idf = sbg("idf", [128, 128]); idb = sbg("idb", [128, 128], BF16)
        triu = sbg("triu", [128, 128]); tril = sbg("tril", [128, 128]); ones = sbg("ones", [128, 128])
        epsb = sbg("epsb", [128, 1])
        prm = sbg("prm", [128, 6, 8])
        DMA('sp', idf[:], idfh.ap(), [], [('idf', None)])
        DMA('sp', idb[:], idbh.ap(), [], [('idb', None)])
        DMA('sp', triu[:], triuh.ap(), [], [('triu', None)])
        DMA('sp', tril[:], trilh.ap(), [], [('tril', None)])
        DMA('sp', ones[:], onesh.ap(), [], [('ones', None)])
        S.op('dve', nc.vector.memset, dict(ap=epsb[:], constant=EPS), [], [('epsb', None)])

        with ExitStack() as st:
            def sb(name, shape, dt=F32):
                return st.enter_context(nc.sbuf_tensor('sb_' + name, shape, dt))
            cv = sb("cv", [128, 8, 2]); sc = sb("sc", [128, 8, 2]); vin = sb("vin", [64, 128]); vT = sb("vT", [128, 64])
            adw = [sb("adw%d" % i, [128, 8, 1024]) for i in range(2)]
            modT = sb("modT", [128, 48, 2]); mrow = sb("mrow", [48, 128]); tmpa = sb("tmpa", [128, 8])
            DMA('sp', cv[:], cvh.ap().rearrange("(k p) n -> p k n", p=128), [], [('cv', None)])
            DMA('sp', vin[:], vecsh.ap(), [], [('vin', None)])
            ACT(sc[:], cv[:], AF.Silu, [('cv', None)], [('sc', None)])
            PET(PS(0, 0)[:, 0:64], vin[:], idf[0:64, 0:64], [('vin', None), ('idf', None)], [PN(0, 0)])
            CP('dve', vT[:], PS(0, 0)[:, 0:64], [PN(0, 0)], [('vT', None)])
            for jb in range(6):
                DMA('sp', adw[jb % 2][:], adawh.ap()[:, jb * 1024:(jb + 1) * 1024].rearrange("(k p) n -> p k n", p=128),
                    [], [('adw', jb % 2)])
                for jj in range(8):
                    j = jb * 8 + jj
                    for kc in range(8):
                        PE(PS(0, 1)[:, 2 * j:2 * j + 2], adw[jb % 2][:, kc, jj * 128:(jj + 1) * 128], sc[:, kc, :],
                           kc == 0, kc == 7, [('adw', jb % 2), ('sc', None)], [PN(0, 1)], inc=(kc == 7))
            pm = PS(0, 1)[:, 0:96].rearrange("p (j n) -> p j n", n=2)
            for n_ in range(2):
                TTo('dve', modT[:, :, n_], pm[:, :, n_], vT[:, 0:48], ALU.add, [PN(0, 1), ('vT', None)], [('modT', n_)])
            for (pi, n_, gcol, sccol, shcol) in ((0, 0, 48, 8, 0), (2, 1, 48, 8, 0), (4, 0, 56, 32, 24)):
                TS('dve', tmpa[:], modT[:, sccol:sccol + 8, n_], 1.0, None, ALU.add, None, [('modT', n_)], [('tmpa', None)])
                TTo('dve', prm[:, pi, :], tmpa[:], vT[:, gcol:gcol + 8], ALU.mult, [('tmpa', None), ('vT', None)], [('prm', pi)])
                CP('dve', prm[:, pi + 1, :], modT[:, shcol:shcol + 8, n_], [('modT', n_)], [('prm', pi + 1)])
            PET(PS(0, 0)[0:48, 0:128], modT[:, :, 0], idf[:], [('modT', 0), ('idf', None)], [PN(0, 0)])
            CP('dve', mrow[:], PS(0, 0)[0:48, 0:128], [PN(0, 0)], [('mrow', None)])
            DMA('sp', modscr.ap().rearrange("(j p) -> j p", p=128), mrow[:], [('mrow', None)], [('modscr', None)])
            S.barrier()
            S.emit()

        if stop == 'A':
            dump('d_prm', prm[:], [128, 6, 8], F32, [('prm', None)])
            return finish_dbg()
        ust = ExitStack() if stop is None else gst
        uT = ust.enter_context(nc.sbuf_tensor("sb_uT", [128, 8, TT], BF16))

        def norm_rstd(src, junk, ssb, nm):
            ACT(junk, src, AF.Square, [(nm, None)], [('junk', None), (nm + 'ss', 0)], accum_out=ssb[:, 0:1])
            ACT(ssb[:, 1:2], ssb[:, 0:1], AF.Ln, [(nm + 'ss', 0), ('epsb', None)], [(nm + 'ss', 1)], scale=1.0 / D, bias=epsb[:, 0:1])
            ACT(ssb[:, 2:3], ssb[:, 1:2], AF.Exp, [(nm + 'ss', 1)], [(nm + 'ss', 2)], scale=-0.5)

        with ExitStack() as st:
            def sb(name, shape, dt=F32):
                return st.enter_context(nc.sbuf_tensor('sb_' + name, shape, dt))
            xin = [sb("xin%d" % i, [128, D]) for i in range(3)]
            ssb = [sb("ssb%d" % i, [128, 4]) for i in range(3)]
            xn = [sb("xn%d" % i, [128, D], BF16) for i in range(2)]
            junk = sb("junk", [128, D], BF16)
            for ti in range(NT):
                b3 = ti % 3
                src = ctxh.ap()[ti * 128:(ti + 1) * 128, :] if ti < 2 else xh.ap()[(ti - 2) * 128:(ti - 1) * 128, :]
                nm = 'xin%d' % b3
                DMA('sp', xin[b3][:], src, [], [(nm, None)])
                norm_rstd(xin[b3][:], junk[:], ssb[b3], nm)
                xnn = 'xn%d' % (ti % 2)
                TS('dve', xn[ti % 2][:], xin[b3][:], ssb[b3][:, 2:3], None, ALU.mult, None, [(nm, None), (nm + 'ss', 2)], [(xnn, None)])
                pi = 2 if ti < 2 else 0
                for g in range(2):
                    for j in range(4):
                        kc = g * 4 + j
                        PET(PSb(3, g)[:, j * 128:(j + 1) * 128], xn[ti % 2][:, kc * 128:(kc + 1) * 128], idb[:],
                            [(xnn, None), ('idb', None)], [PN(3, g)], inc=(j == 3))
                    for j in range(4):
                        kc = g * 4 + j
                        o = uT[:, kc, ti * 128:(ti + 1) * 128]
                        i_ = PSb(3, g)[:, j * 128:(j + 1) * 128]
                        if j % 2 == 0:
                            TS('dve', o, i_, prm[:, pi, kc:kc + 1], prm[:, pi + 1, kc:kc + 1], ALU.mult, ALU.add,
                               [PN(3, g), ('prm', pi), ('prm', pi + 1)], [('uT', ti)])
                        else:
                            ACT(o, i_, AF.Identity, [PN(3, g), ('prm', pi), ('prm', pi + 1)], [('uT', ti)],
                                scale=prm[:, pi, kc:kc + 1], bias=prm[:, pi + 1, kc:kc + 1])
            S.barrier()
            S.emit()

        if dbg:
            dbg_u = nc.dram_tensor("dbg_u", [128, 8, TT], BF16, kind="ExternalOutput")
            DMA('sp', dbg_u.ap(), uT[:], [('uT', None)], [('dbg_u', None)])

        if stop == 'B':
            dump('d_uT', uT[:], [128, 8, TT], BF16, [('uT', None)])
            return finish_dbg()
        def wslice(c0, n):
            return winh.ap()[:, c0:c0 + n].rearrange("(k p) n -> p k n", p=128)

        with ExitStack() as st:
            def sb(name, shape, dt=F32):
                return st.enter_context(nc.sbuf_tensor('sb_' + name, shape, dt))
            wg = sb("wg", [128, 8, 16], BF16); gb34 = sb("gb34", [128, NT, 16]); G = sb("G", [128, NT, 16])
            SP = [sb("SP%d" % d, [128, NT * 4]) for d in range(2)]
            EE = [sb("EE%d" % d, [128, NT, 4]) for d in range(2)]
            WW = [sb("WW%d" % d, [128, NT, 4]) for d in range(2)]
            EL = [sb("EL%d" % d, [128, NT, 4]) for d in range(2)]
            tmpw = sb("tmpw", [128, NT, 4])
            cst = [sb("cst%d" % d, [128, 272]) for d in range(2)]
            gm = sb("gm", [128, D])
            DMA('pool', wg[:], wslice(O_MG, 16), [], [('wg', None)])
            gb16 = sb("gb16", [128, 16])
            DMA('sp', gb16[:], bc(gbh, 0, 16), [], [('gb16', None)])
            DMA('sp', gm[:], bc(mngh, 0, D), [], [('gm', None)])
            for ti in range(NT):
                bank = 0 if ti < 32 else 1
                col = (ti % 32) * 16
                for kc in range(8):
                    PE(PS(0, bank)[:, col:col + 16], uT[:, kc, ti * 128:(ti + 1) * 128], wg[:, kc, :], kc == 0, kc == 7,
                       [('uT', None), ('wg', None)], [PN(0, bank)], inc=(kc == 7))
            for ti in range(NT):
                bank = 0 if ti < 32 else 1
                col = (ti % 32) * 16
                TTo('dve', G[:, ti, :], PS(0, bank)[:, col:col + 16], gb16[:], ALU.add, [PN(0, bank), ('gb16', None)], [('G', ti)])
            if stop == 'C0a':
                dump('d_G', G[:], [128, NT, 16], F32, [('G', None)])
                return finish_dbg()
            for d in range(2):
                spv = SP[d][:].rearrange("p (t n) -> p t n", n=4)
                ACT(spv, G[:, :, 8 * d + 4:8 * d + 8], AF.Exp, [('G', None)], [('SP', d)], scale=-1.0)
                ACT(SP[d][:], SP[d][:], AF.Ln, [('SP', d), ('ones', None)], [('SP', d)], bias=ones[:, 0:1])
                if stop == 'C0b':
                    continue
                tri = triu if d == 0 else tril
                PE(PS(1, d)[:, 0:136], tri[:], SP[d][:], True, True, [('SP', d), ('triu', None), ('tril', None)], [PN(1, d)])
                PE(PS(1, d)[:, 136:272], ones[:], SP[d][:], True, True, [('SP', d), ('ones', None)], [PN(1, d)])
                if stop == 'C0c':
                    CP('dve', EE[d][:].rearrange("p t n -> p (t n)"), PS(1, d)[:, 0:136], [PN(1, d)], [('EE', d)])
                    continue
                CP('dve', cst[d][:], PS(1, d)[:, 0:272], [PN(1, d)], [('cst', d)])
                cs = cst[d][:, 0:136].rearrange("p (t n) -> p t n", n=4)
                tt_ = cst[d][:, 136:272].rearrange("p (t n) -> p t n", n=4)
                SK = os.environ.get('DBGSKIP', '')
                if '1' not in SK:
                    ACT(EE[d][:], cs, AF.Exp, [('cst', d)], [('EE', d)], scale=-1.0)
                if '2' not in SK:
                    TTo('dve', tmpw[:], G[:, :, 8 * d:8 * d + 4], cs, ALU.add, [('G', None), ('cst', d)], [('tmpw', None)])
                if '3' not in SK:
                    ACT(WW[d][:], tmpw[:], AF.Exp, [('tmpw', None)], [('WW', d)])
                if '4' not in SK:
                    ACT(EL[d][:], tt_, AF.Exp, [('cst', d)], [('EL', d)], scale=-1.0)

            if stop == 'C0b':
                dump('d_SP', SP[0][:], [128, NT * 4], F32, [('SP', 0)])
                return finish_dbg()
            if stop == 'C0c':
                dump('d_EE', EE[0][:], [128, NT, 4], F32, [('EE', 0)])
                return finish_dbg()
            if stop == 'C0':
                dump('d_EE', EE[0][:], [128, NT, 4], F32, [('EE', 0)])
                dump('d_WW', WW[1][:], [128, NT, 4], F32, [('WW', 1)])
                dump('d_EL', EL[0][:], [128, NT, 4], F32, [('EL', 0)])
                dump('d_G', G[:], [128, NT, 16], F32, [('G', None)])
                return finish_dbg()
            wm2 = [sb("wm%d" % i, [128, 8, 768], BF16) for i in range(2)]

            def load_wm(hh):
                w_ = wm2[hh % 2]
                p_ = hh % 2
                DMA('pool', w_[:, :, 0:128], wslice(O_MQ + hh * 128, 128), [], [('wm', (p_, 0))])
                DMA('pool', w_[:, :, 128:256], wslice(O_MK + hh * 128, 128), [], [('wm', (p_, 1))])
                DMA('pool', w_[:, :, 256:512], wslice(O_MV + hh * 256, 256), [], [('wm', (p_, 2))])
                DMA('pool', w_[:, :, 512:768], wslice(O_MO + hh * 256, 256), [], [('wm', (p_, 3))])

            load_wm(0)
            qT = sb("qT", [128, TT], BF16); kT = sb("kT", [128, TT], BF16)
            vt = sb("vt", [128, NT, 257], BF16)
            kt = [sb("kt%d" % d, [128, NT, 128], BF16) for d in range(2)]
            SD = [sb("SD%d" % i, [128, 128], BF16) for i in range(2)]
            Cf = sb("Cf", [128, 257]); Cb = sb("Cb", [128, 257], BF16); tmpC = sb("tmpC", [128, 257])
            sml = [sb("sml%d" % i, [128, 8]) for i in range(2)]
            hst = [sb("hst%d" % i, [128, 256]) for i in range(3)]
            hfl = [sb("hfl%d" % i, [128, 256]) for i in range(3)]
            hs = [sb("hs%d" % i, [128, 256]) for i in range(2)]
            osg = [sb("osg%d" % i, [128, 256]) for i in range(2)]
            ab = [sb("ab%d" % i, [128, 256], BF16) for i in range(2)]
            aTs = [sb("aTs%d" % i, [128, 2, 512], BF16) for i in range(2)]
            junk2 = sb("junk2", [128, 256], BF16)
            S.op('pool', nc.gpsimd.memset, dict(ap=vt[:, :, 256:257], constant=1.0), [], [('vt1', None)])

            for h in range(4):
                wm = wm2[h % 2]
                wp_ = h % 2
                if h + 1 < 4:
                    load_wm(h + 1)
                cnt = 0
                for blk in range(9):
                    c0 = blk * 512
                    n = min(512, TT - c0)
                    for which in range(2):
                        pi_, pb_ = (cnt // 2) % 2, cnt % 2
                        cnt += 1
                        for kc in range(8):
                            PE(PS(pi_, pb_)[:, 0:n], wm[:, kc, which * 128:(which + 1) * 128], uT[:, kc, c0:c0 + n], kc == 0, kc == 7,
                               [('wm', (wp_, which)), ('uT', None)], [PN(pi_, pb_)], inc=(kc == 7))
                        if which == 0:
                            ACT(qT[:, c0:c0 + n], PS(pi_, pb_)[:, 0:n], AF.Copy, [PN(pi_, pb_)], [('qT', blk)], scale=128.0 ** -0.5)
                        else:
                            CP('dve', kT[:, c0:c0 + n], PS(pi_, pb_)[:, 0:n], [PN(pi_, pb_)], [('kT', blk)])
                for ti in range(NT):
                    pb_ = ti % 2
                    for kc in range(8):
                        PE(PS(2, pb_)[:, 0:384], uT[:, kc, ti * 128:(ti + 1) * 128], wm[:, kc, 128:512], kc == 0, kc == 7,
                           [('uT', None), ('wm', (wp_, 1)), ('wm', (wp_, 2))], [PN(2, pb_)], inc=(kc == 7))
                    TS('dve', kt[0][:, ti, :], PS(2, pb_)[:, 0:128], WW[0][:, ti, h:h + 1], None, ALU.mult, None,
                       [PN(2, pb_), ('WW', 0)], [('kt0', ti)])
                    TS('dve', kt[1][:, ti, :], PS(2, pb_)[:, 0:128], WW[1][:, ti, h:h + 1], None, ALU.mult, None,
                       [PN(2, pb_), ('WW', 1)], [('kt1', ti)])
                    CP('act', vt[:, ti, 0:256], PS(2, pb_)[:, 128:384], [PN(2, pb_)], [('vt', ti)])

                for d in range(2):
                    order = list(range(NT)) if d == 0 else [1, 0] + list(range(NT - 1, 1, -1))
                    mask = triu if d == 0 else tril
                    lat = [t_ for t_ in order if t_ >= 2]

                    def emit_sd(ti, li):
                        sl = li % 2
                        cs_ = slice(ti * 128, (ti + 1) * 128)
                        PE(PS(0, sl)[:, 0:128], kT[:, cs_], qT[:, cs_], True, True, [('kT', None), ('qT', None)], [PN(0, sl)])
                        STT('dve', SD[sl][:], PS(0, sl)[:, 0:128], WW[d][:, ti, h:h + 1], mask[:], ALU.mult, ALU.mult,
                            [PN(0, sl), ('WW', d), ('triu', None), ('tril', None)], [('SD', sl)])

                    def emit_oraw(li_):
                        ti_ = lat[li_]
                        b2_ = li_ % 2
                        for kc in range(8):
                            PE(PS(3, 0)[:, 0:256], uT[:, kc, ti_ * 128:(ti_ + 1) * 128], wm[:, kc, 512:768], kc == 0, kc == 7,
                               [('uT', None), ('wm', (wp_, 3))], [PN(3, 0)], inc=(kc == 7))
                        ACT(osg[b2_][:], PS(3, 0)[:, 0:256], AF.Sigmoid, [PN(3, 0)], [('osg', b2_)])
                        TTo('pool', osg[b2_][:], osg[b2_][:], gm[:, h * 256:(h + 1) * 256], ALU.mult, [('osg', b2_), ('gm', None)], [('osg', b2_)])

                    def emit_hfload(li_):
                        c_ = lat[li_] - 2
                        DMA('sp', hfl[li_ % 3][:], hfscr.ap()[h, c_], [('hf', (h, c_))], [('hfl', li_ % 3)])

                    def emit_tr(li_):
                        c_ = lat[li_] - 2
                        b2_ = li_ % 2
                        blk, sub = c_ // 4, c_ % 4
                        ab_ = blk % 2
                        for jj in range(2):
                            PET(PSb(3, 1)[:, jj * 128:(jj + 1) * 128], ab[b2_][:, jj * 128:(jj + 1) * 128], idb[:],
                                [('ab', b2_), ('idb', None)], [PN(3, 1)], inc=(jj == 1))
                        CP('act', aTs[ab_][:, :, sub * 128:(sub + 1) * 128],
                           PSb(3, 1)[:, 0:256].rearrange("p (a b) -> p a b", b=128), [PN(3, 1)], [('aTs', ab_)])
                        if sub == 0:
                            DMA('sp', aTd.ap()[:, 2 * h:2 * h + 2, blk * 512:(blk + 1) * 512], aTs[ab_][:],
                                [('aTs', ab_)], [('aTd', (h, blk))])

                    emit_sd(lat[0], 0)
                    if d == 1:
                        emit_hfload(0)
                        emit_hfload(1)
                        emit_oraw(0)
                    li = 0
                    for j, ti in enumerate(order):
                        latent = ti >= 2
                        last = (j == NT - 1)
                        cs_ = slice(ti * 128, (ti + 1) * 128)
                        if latent and li + 1 < len(lat):
                            emit_sd(lat[li + 1], li + 1)
                        pb_ = j % 2
                        if not last:
                            PE(PS(2, pb_)[:, 0:257], kt[d][:, ti, :], vt[:, ti, :], True, True,
                               [('kt%d' % d, ti), ('vt', ti), ('vt1', None)], [PN(2, pb_)])
                        if latent:
                            sl = li % 2
                            PE(PS(1, sl)[:, 0:257], SD[sl][:], vt[:, ti, :], True, j == 0,
                               [('SD', sl), ('vt', ti), ('vt1', None)], [PN(1, sl)], inc=(j == 0))
                            if j > 0:
                                PE(PS(1, sl)[:, 0:257], qT[:, cs_], Cb[:], False, True, [('qT', None), ('Cb', None)], [PN(1, sl)])
                        if not last:
                            Ecol = EL[d][:, ti, h:h + 1]
                            if j == 0:
                                ACT(Cf[:], PS(2, pb_)[:, 0:257], AF.Copy, [PN(2, pb_), ('EL', d)], [('Cf', None)], scale=Ecol)
                                ACT(Cb[:], PS(2, pb_)[:, 0:257], AF.Copy, [PN(2, pb_), ('EL', d)], [('Cb', None)], scale=Ecol)
                            else:
                                TTo('dve', tmpC[:], PS(2, pb_)[:, 0:257], Cf[:], ALU.add, [PN(2, pb_), ('Cf', None)], [('tmpC', None)])
                                ACT(Cb[:], tmpC[:], AF.Copy, [('tmpC', None), ('EL', d)], [('Cb', None)], scale=Ecol)
                                ACT(Cf[:], tmpC[:], AF.Copy, [('tmpC', None), ('EL', d)], [('Cf', None)], scale=Ecol)
                        if latent:
                            sl = li % 2
                            c = ti - 2
                            num = PS(1, sl)
                            sm = sml[li % 2]
                            smn = 'sml%d' % (li % 2)
                            e_ = EE[d][:, ti, h:h + 1]
                            if d == 1:
                                if li + 1 < len(lat):
                                    emit_oraw(li + 1)
                                if li >= 1:
                                    emit_tr(li - 1)
                                if li + 2 < len(lat):
                                    emit_hfload(li + 2)
                            TS('dve', sm[:, 0:1], num[:, 256:257], e_, None, ALU.mult, None, [PN(1, sl), ('EE', d)], [(smn, 0)])
                            TS('dve', sm[:, 7:8], sm[:, 0:1], -1.0, None, ALU.mult, None, [(smn, 0)], [(smn, 7)])
                            TTo('dve', sm[:, 1:2], sm[:, 0:1], sm[:, 7:8], ALU.max, [(smn, 0), (smn, 7)], [(smn, 1)])
                            TS('dve', sm[:, 1:2], sm[:, 1:2], 1.0, None, ALU.max, None, [(smn, 1)], [(smn, 1)])
                            S.op('dve', nc.vector.reciprocal, dict(out=sm[:, 2:3], in_=sm[:, 1:2]), [(smn, 1)], [(smn, 2)])
                            TS('dve', sm[:, 3:4], sm[:, 2:3], e_, None, ALU.mult, None, [(smn, 2), ('EE', d)], [(smn, 3)])
                            if d == 0:
                                b3 = li % 3
                                ACT(hst[b3][:], num[:, 0:256], AF.Copy, [PN(1, sl), (smn, 3)], [('hst', b3)], scale=sm[:, 3:4])
                                DMA('sp', hfscr.ap()[h, c], hst[b3][:], [('hst', b3)], [('hf', (h, c))])
                            else:
                                b3 = li % 3
                                b2 = li % 2
                                STT('dve', hs[b2][:], num[:, 0:256], sm[:, 3:4], hfl[b3][:], ALU.mult, ALU.add,
                                    [PN(1, sl), (smn, 3), ('hfl', b3)], [('hs', b2)])
                                ACT(junk2[:], hs[b2][:], AF.Square, [('hs', b2)], [('junk2', None), (smn, 4)], accum_out=sm[:, 4:5])
                                ACT(sm[:, 5:6], sm[:, 4:5], AF.Ln, [(smn, 4), ('epsb', None)], [(smn, 5)], scale=1.0 / 256, bias=epsb[:, 0:1])
                                ACT(sm[:, 6:7], sm[:, 5:6], AF.Exp, [(smn, 5)], [(smn, 6)], scale=-0.5)
                                STT('dve', ab[b2][:], hs[b2][:], sm[:, 6:7], osg[b2][:], ALU.mult, ALU.mult,
                                    [('hs', b2), (smn, 6), ('osg', b2)], [('ab', b2)])
                                if li == len(lat) - 1:
                                    emit_tr(li)
                            li += 1
            S.barrier()
            S.emit()

        if stop == 'C1':
            dump('d_aT', aTd.ap(), [128, 8, T], BF16, [('aTd', None)])
            return finish_dbg()
        with ExitStack() as st:
            def sb(name, shape, dt=F32):
                return st.enter_context(nc.sbuf_tensor('sb_' + name, shape, dt))
            cost = sb("cost", [128, T]); sint = sb("sint", [128, T])
            DMA('sp', cost[:], cosh_.ap(), [], [('cost', None)])
            DMA('sp', sint[:], sinh_.ap(), [], [('sint', None)])
            lamb = sb("lamb", [128, 256]); lt = sb("lt", [128, 8])
            gd = sb("gd", [128, D])
            DMA('sp', lamb[:], bc(dlh, 0, 256), [], [('lamb', None)])
            DMA('sp', gd[:], bc(dngh, 0, D), [], [('gd', None)])
            TS('pool', gd[:], gd[:], 1.0 - LAM_INIT, None, ALU.mult, None, [('gd', None)], [('gd', None)])
            lp = sb("lp", [128, 128])
            TTo('dve', lp[:, 0:64], lamb[:, 0:64], lamb[:, 64:128], ALU.mult, [('lamb', None)], [('lp', 0)])
            TTo('dve', lp[:, 64:128], lamb[:, 128:192], lamb[:, 192:256], ALU.mult, [('lamb', None)], [('lp', 1)])
            S.op('dve', nc.vector.reduce_sum, dict(out=lt[:, 0:1], in_=lp[:, 0:64], axis=AX.X), [('lp', 0)], [('lt', 0)])
            S.op('dve', nc.vector.reduce_sum, dict(out=lt[:, 1:2], in_=lp[:, 64:128], axis=AX.X), [('lp', 1)], [('lt', 1)])
            ACT(lt[:, 2:4], lt[:, 0:2], AF.Exp, [('lt', 0), ('lt', 1)], [('lt', 2)])
            TTo('dve', lt[:, 4:5], lt[:, 3:4], lt[:, 2:3], ALU.subtract, [('lt', 2)], [('lt', 4)])
            TS('dve', lt[:, 5:6], lt[:, 4:5], -LAM_INIT, None, ALU.add, None, [('lt', 4)], [('lt', 5)])
            neglam = lt[:, 5:6]

            wd = sb("wd", [128, 5, 1024], BF16)
            qT = sb("dqT", [128, T], BF16); kT = sb("dkT", [128, TT], BF16)
            vd = sb("vd", [128, NT, 129], BF16)
            t1 = [sb("t1_%d" % i, [128, 512]) for i in range(2)]
            t2 = [sb("t2_%d" % i, [128, 512]) for i in range(2)]
            Pex = [sb("Pex%d" % i, [128, 1024], BF16) for i in range(3)]
            es = [sb("es%d" % i, [128, 8]) for i in range(2)]
            Ocp = sb("Ocp", [128, 8 * 129]); es4 = sb("es4", [128, 24]); A4 = sb("A4", [128, 4, 128]); sq4 = sb("sq4", [128, 4, 128])
            bb4 = sb("bb4", [128, 4, 128], BF16)
            pending = []

            def flush(upto):
                keep = []
                for (trig, fn) in pending:
                    if upto is None or trig <= upto:
                        fn()
                    else:
                        keep.append((trig, fn))
                pending[:] = keep
            A1 = [sb("A1_%d" % i, [128, 128]) for i in range(2)]
            bb = [sb("bb%d" % i, [128, 128], BF16) for i in range(2)]
            bst = [sb("bst%d" % i, [128, 512], BF16) for i in range(2)]
            junk3 = sb("junk3", [128, 128], BF16)
            S.op('pool', nc.gpsimd.memset, dict(ap=vd[:, :, 128:129], constant=1.0), [], [('vd1', None)])
            cw = [sb("cw_%d" % i, [128, 8, D], BF16) for i in range(2)]
            ccnt = [0]

            def oreg(m, sub):
                r = m * 4 + sub
                if r < 3:
                    return PS(2, 0)[:, r * 129:(r + 1) * 129], PN(2, 0)
                if r < 6:
                    return PS(2, 1)[:, (r - 3) * 129:(r - 2) * 129], PN(2, 1)
                return PS(3, 0)[:, (r - 6) * 129:(r - 5) * 129], PN(3, 0)

            for h in range(8):
                for pi_, c0 in ((0, O_DQ + h * 128), (2, O_DK + h * 128), (4, O_DV + h * 128)):
                    DMA('pool', wd[:, pi_, :].rearrange("p (k n) -> p k n", n=128), wslice(c0, 128), [], [('wd', pi_)])
                for pi_ in (0, 2):
                    s4 = wd[:, pi_, :].rearrange("p (a two j) -> p a two j", two=2, j=16)
                    d4 = wd[:, pi_ + 1, :].rearrange("p (a two j) -> p a two j", two=2, j=16)
                    CP('pool', d4[:, :, 0, :], s4[:, :, 1, :], [('wd', pi_)], [('wd', pi_ + 1)])
                    CP('pool', d4[:, :, 1, :], s4[:, :, 0, :], [('wd', pi_)], [('wd', pi_ + 1)])
                def emit_vgroup(g):
                    tiles = list(range(g * 4, min(NT, g * 4 + 4)))
                    pb_ = g % 2
                    for ii, ti in enumerate(tiles):
                        for kc in range(8):
                            PE(PS(2, pb_)[:, ii * 128:(ii + 1) * 128], uT[:, kc, ti * 128:(ti + 1) * 128], wd[:, 4, kc * 128:(kc + 1) * 128],
                               kc == 0, kc == 7, [('uT', None), ('wd', 4)], [PN(2, pb_)], inc=(kc == 7 and ii == len(tiles) - 1))
                    nt_ = len(tiles)
                    CP('act', vd[:, tiles[0]:tiles[0] + nt_, 0:128], PS(2, pb_)[:, 0:nt_ * 128].rearrange("p (a b) -> p a b", b=128),
                       [PN(2, pb_)], [('vd', g)])

                vg = [0]
                cnt = 0
                for which in range(2):
                    dst = qT if which == 0 else kT
                    dn = 'dqT' if which == 0 else 'dkT'
                    off = 0 if which == 0 else TC
                    wp = 0 if which == 0 else 2
                    for blk in range(8):
                        c0 = TC + blk * 512
                        pi_ = cnt % 2
                        cnt += 1
                        for ab_ in range(2):
                            for kc in range(8):
                                PE(PS(pi_, ab_), wd[:, wp + ab_, kc * 128:(kc + 1) * 128], uT[:, kc, c0:c0 + 512], kc == 0, kc == 7,
                                   [('wd', wp + ab_), ('uT', None)], [PN(pi_, ab_)], inc=(kc == 7))
                        if vg[0] < 9 and (cnt % 2 == 1 or vg[0] < cnt // 2):
                            emit_vgroup(vg[0])
                            vg[0] += 1
                        tb_ = blk % 2
                        TTo('dve', t1[tb_][:], PS(pi_, 0), cost[:, blk * 512:(blk + 1) * 512], ALU.mult, [PN(pi_, 0), ('cost', None)], [('t1', tb_)])
                        TTo('dve', t2[tb_][:], PS(pi_, 1), sint[:, blk * 512:(blk + 1) * 512], ALU.mult, [PN(pi_, 1), ('sint', None)], [('t2', tb_)])
                        TTo('pool', dst[:, off + blk * 512:off + (blk + 1) * 512], t1[tb_][:], t2[tb_][:], ALU.add,
                            [('t1', tb_), ('t2', tb_)], [(dn, blk)])
                    if which == 0:
                        flush(None)
                for kc in range(8):
                    PE(PS(0, 0)[:, 0:256], wd[:, 2, kc * 128:(kc + 1) * 128], uT[:, kc, 0:TC], kc == 0, kc == 7,
                       [('wd', 2), ('uT', None)], [PN(0, 0)], inc=(kc == 7))
                CP('act', kT[:, 0:TC], PS(0, 0)[:, 0:256], [PN(0, 0)], [('dkT', 'c')])
                while vg[0] < 9:
                    emit_vgroup(vg[0])
                    vg[0] += 1
                for e in range(4 * h, 4 * h + 4):
                    for g in range(3):
                        cb_ = ccnt[0] % 2
                        ccnt[0] += 1
                        if g < 2:
                            src = w1h.ap()[e][:, g * D:(g + 1) * D].rearrange("(k p) n -> p k n", p=128)
                            dst = w1bd.ap()[e * 128:(e + 1) * 128, :].rearrange("p (k n) -> p k n", n=2 * D)[:, :, g * D:(g + 1) * D]
                            dn = ('w1bd', (e, g))
                        else:
                            src = w2h.ap()[e].rearrange("(k p) n -> p k n", p=128)
                            dst = w2bd.ap()[e * 128:(e + 1) * 128, :].rearrange("p (k n) -> p k n", n=D)
                            dn = ('w2bd', e)
                        DMA('pool', cw[cb_][:], src, [], [('cw', cb_)])
                        DMA('sp', dst, cw[cb_][:], [('cw', cb_)], [dn])
                for qb in range(8):
                    q0 = qb * 512

                    def emit_qk(kc, n):
                        s_ = n % 2
                        for m in range(2):
                            PE(PS(s_, m), kT[64 * m:64 * m + 64, kc * 128:(kc + 1) * 128], qT[64 * m:64 * m + 64, q0:q0 + 512], True, True,
                               [('dkT', None), ('dqT', None)], [PN(s_, m)], inc=(m == 1))
                        ACT(Pex[n % 3][:], PSUM[s_][:], AF.Exp, [PN(s_, 0), PN(s_, 1)], [('Pex', n % 3)], scale=0.125)

                    emit_qk(0, 0)
                    for kc in range(NT):
                        if kc + 1 < NT:
                            emit_qk(kc + 1, kc + 1)
                        pe_ = Pex[kc % 3]
                        for m in range(2):
                            for sub in range(4):
                                oap, on = oreg(m, sub)
                                r_ = m * 4 + sub
                                PE(oap, pe_[:, m * 512 + sub * 128:m * 512 + (sub + 1) * 128], vd[:, kc, :],
                                   kc == 0 and r_ in (0, 3, 6), kc == NT - 1 and r_ in (2, 5, 7),
                                   [('Pex', kc % 3), ('vd', None), ('vd1', None)], [on], inc=(m == 1 and sub == 3))
                        flush(kc)
                    CP('dve', Ocp[:, 0:387], PS(2, 0)[:, 0:387], [PN(2, 0)], [('Ocp', 0)])
                    CP('dve', Ocp[:, 387:774], PS(2, 1)[:, 0:387], [PN(2, 1)], [('Ocp', 1)])
                    CP('dve', Ocp[:, 774:1032], PS(3, 0)[:, 0:258], [PN(3, 0)], [('Ocp', 2)])

                    def st2(h=h):
                        for sub in range(4):
                            o0 = Ocp[:, sub * 129:(sub + 1) * 129]
                            o1 = Ocp[:, (4 + sub) * 129:(5 + sub) * 129]
                            S.op('dve', nc.vector.reciprocal, dict(out=es4[:, sub:sub + 1], in_=o0[:, 128:129]), [('Ocp', None)], [('es4', (0, sub))])
                            S.op('dve', nc.vector.reciprocal, dict(out=es4[:, 4 + sub:5 + sub], in_=o1[:, 128:129]), [('Ocp', None)], [('es4', (1, sub))])
                            TS('dve', es4[:, 8 + sub:9 + sub], es4[:, 4 + sub:5 + sub], neglam, None, ALU.mult, None, [('es4', (1, sub)), ('lt', 5)], [('es4', (2, sub))])
                            TS('dve', A4[:, sub, :], o0[:, 0:128], es4[:, sub:sub + 1], None, ALU.mult, None, [('Ocp', None), ('es4', (0, sub))], [('A4', sub)])
                            STT('dve', A4[:, sub, :], o1[:, 0:128], es4[:, 8 + sub:9 + sub], A4[:, sub, :], ALU.mult, ALU.add,
                                [('Ocp', None), ('es4', (2, sub)), ('A4', sub)], [('A4', sub)])
                            TTo('pool', sq4[:, sub, :], A4[:, sub, :], A4[:, sub, :], ALU.mult, [('A4', sub)], [('sq4', sub)])
                        S.op('dve', nc.vector.reduce_sum, dict(out=es4[:, 12:16], in_=sq4[:], axis=AX.X), [('sq4', None)], [('es4', 3)])

                    def st3():
                        ACT(es4[:, 16:20], es4[:, 12:16], AF.Ln, [('es4', 3), ('epsb', None)], [('es4', 4)], scale=1.0 / 128, bias=epsb[:, 0:1])
                        ACT(es4[:, 20:24], es4[:, 16:20], AF.Exp, [('es4', 4)], [('es4', 5)], scale=-0.5)

                    def st4(h=h):
                        for sub in range(4):
                            STT('dve', bb4[:, sub, :], A4[:, sub, :], es4[:, 20 + sub:21 + sub], gd[:, h * 128:(h + 1) * 128], ALU.mult, ALU.mult,
                                [('A4', sub), ('es4', 5), ('gd', None)], [('bb4', sub)])

                    def st5():
                        for sub in range(4):
                            PET(PSb(3, 1)[:, sub * 128:(sub + 1) * 128], bb4[:, sub, :], idb[:], [('bb4', sub), ('idb', None)], [PN(3, 1)], inc=(sub == 3))

                    def st6(h=h, qb=qb, q0=q0):
                        sb_ = qb % 2
                        CP('dve', bst[sb_][:], PSb(3, 1)[:, 0:512], [PN(3, 1)], [('bst', sb_)])
                        DMA('sp', bTd.ap()[:, h, q0:q0 + 512], bst[sb_][:], [('bst', sb_)], [('bTd', (h, qb))])

                    pending.extend([(0, st2), (4, st3), (5, st4), (8, st5), (9, st6)])

            flush(None)
            S.barrier()
            S.emit()
        with ExitStack() as st:
            def sb(name, shape, dt=F32):
                return st.enter_context(nc.sbuf_tensor('sb_' + name, shape, dt))
            wgc = [sb("wgc%d" % i, [128, 8, 128], BF16) for i in range(2)]
            sgs = [sb("sgs%d" % i, [128, T], BF16) for i in range(2)]
            for cc in range(16):
                b_ = cc % 2
                DMA('pool', wgc[b_][:], wslice(O_GA + cc * 128, 128), [], [('wgc', b_)])
                for blk in range(8):
                    pi_, pb_ = (blk // 2) % 2, blk % 2
                    for kc in range(8):
                        PE(PS(pi_, pb_), wgc[b_][:, kc, :], uT[:, kc, TC + blk * 512:TC + (blk + 1) * 512], kc == 0, kc == 7,
                           [('wgc', b_), ('uT', None)], [PN(pi_, pb_)], inc=(kc == 7))
                    ACT(sgs[b_][:, blk * 512:(blk + 1) * 512], PS(pi_, pb_), AF.Sigmoid, [PN(pi_, pb_)], [('sgs', b_)])
                DMA('sp', sgd.ap()[:, cc, :], sgs[b_][:], [('sgs', b_)], [('sgd', cc)])
            S.barrier()
            S.emit()
        if stop is None:
            ust.close()

        if stop == 'C2':
            dump('d_bT', bTd.ap(), [128, 8, T], BF16, [('bTd', None)])
            dump('d_sg', sgd.ap(), [128, 16, T], BF16, [('sgd', None)])
            return finish_dbg()
        L = sbg("L", [128, 32, NE])
        with ExitStack() as st:
            G1 = st.enter_context(nc.sbuf_tensor("sb_G1", [128, D], F32))
            DMA('sp', G1[:], bc(modscr, 16 * 128, D), [('modscr', None)], [('G1', None)])
            def sb(name, shape, dt=F32):
                return st.enter_context(nc.sbuf_tensor('sb_' + name, shape, dt))
            wa = sb("wa", [128, 8, D], BF16); wb = sb("wb", [128, 8, D], BF16); wo = sb("wo", [128, 8, D], BF16)
            DMA('pool', wa[:], wah.ap().rearrange("(k p) n -> p k n", p=128), [], [('wa', None)])
            DMA('pool', wb[:], wbh.ap().rearrange("(k p) n -> p k n", p=128), [], [('wb', None)])
            DMA('pool', wo[:], woh.ap().rearrange("(k p) n -> p k n", p=128), [], [('wo', None)])
            rw = sb("rw", [128, 8, NE]); rbb = sb("rbb", [128, NE])
            DMA('sp', rw[:], rwh.ap().rearrange("(k p) n -> p k n", p=128), [], [('rw', None)])
            DMA('sp', rbb[:], bc(rbh, 0, NE), [], [('rbb', None)])
            aTb = [sb("aTb%d" % i, [128, 8, 512], BF16) for i in range(1)] * 2
            bTb = [sb("bTb%d" % i, [128, 8, 512], BF16) for i in range(1)] * 2
            sAb = [sb("sAb%d" % i, [128, 8, 512], BF16) for i in range(1)] * 2
            sBb = [sb("sBb%d" % i, [128, 8, 512], BF16) for i in range(1)] * 2
            yT = [sb("yT%d" % i, [128, 8, 512], BF16) for i in range(1)] * 2
            t1 = [sb("m1_%d" % i, [128, 512]) for i in range(2)]
            t2 = [sb("m2_%d" % i, [128, 512]) for i in range(2)]
            xin = [sb("xi%d" % i, [128, D]) for i in range(2)]
            x1t = [sb("x1t%d" % i, [128, D]) for i in range(2)]
            xn2 = [sb("xn2_%d" % i, [128, D]) for i in range(2)]
            ssb = [sb("ss2_%d" % i, [128, 4]) for i in range(2)]
            u2f = [sb("u2f%d" % i, [128, 8, 128]) for i in range(2)]
            junk = sb("junk4", [128, D], BF16)
            MUL2t = sb("MUL2t", [128, D]); ADD2t = sb("ADD2t", [128, D]); N2Gt = sb("N2Gt", [128, D])
            u2tmp = sb("u2tmp", [128, D]); u2tb = [sb("u2tb%d" % i, [128, D], BF16) for i in range(2)]
            DMA('sp', ADD2t[:], bc(modscr, 3072, D), [('modscr', None)], [('ADD2t', None)])
            DMA('sp', MUL2t[:], bc(modscr, 4096, D), [('modscr', None)], [('MUL2t', None)])
            DMA('sp', N2Gt[:], bc(vecsh, 56 * 128, D), [], [('N2Gt', None)])
            TS('pool', MUL2t[:], MUL2t[:], 1.0, None, ALU.add, None, [('MUL2t', None)], [('MUL2t', None)])
            TTo('pool', MUL2t[:], MUL2t[:], N2Gt[:], ALU.mult, [('MUL2t', None), ('N2Gt', None)], [('MUL2t', None)])
            for tb in range(8):
                b_ = 0
                tsl = slice(tb * 512, (tb + 1) * 512)
                DMA('sp', aTb[b_][:], aTd.ap()[:, :, tsl], [('aTd', None)], [('aTb', b_)])
                DMA('sp', bTb[b_][:], bTd.ap()[:, :, tsl], [('bTd', None)], [('bTb', b_)])
                DMA('sp', sAb[b_][:], sgd.ap()[:, 0:8, tsl], [('sgd', None)], [('sAb', b_)])
                DMA('sp', sBb[b_][:], sgd.ap()[:, 8:16, tsl], [('sgd', None)], [('sBb', b_)])
                for cc in range(8):
                    pi_ = cc % 2
                    for (pb_, w_, w_n, src, srcn) in ((0, wa, 'wa', aTb, 'aTb'), (1, wb, 'wb', bTb, 'bTb')):
                        for kc in range(8):
                            PE(PS(pi_, pb_), w_[:, kc, cc * 128:(cc + 1) * 128], src[b_][:, kc, :], kc == 0, kc == 7,
                               [(w_n, None), (srcn, b_)], [PN(pi_, pb_)], inc=(kc == 7))
                    TTo('dve', t1[pi_][:], PS(pi_, 0), sAb[b_][:, cc, :], ALU.mult, [PN(pi_, 0), ('sAb', b_)], [('m1', pi_)])
                    TTo('dve', t2[pi_][:], PS(pi_, 1), sBb[b_][:, cc, :], ALU.mult, [PN(pi_, 1), ('sBb', b_)], [('m2', pi_)])
                    TTo('pool', yT[b_][:, cc, :], t1[pi_][:], t2[pi_][:], ALU.add, [('m1', pi_), ('m2', pi_)], [('yT', (b_, cc))])
                for sub in range(4):
                    tt = tb * 4 + sub
                    xb_ = tt % 2
                    DMA('sp', xin[xb_][:], xh.ap()[tt * 128:(tt + 1) * 128, :], [], [('xi', xb_)])
                    for half in range(2):
                        for cc in range(8):
                            PE(PS(2, half), yT[b_][:, cc, sub * 128:(sub + 1) * 128], wo[:, cc, half * 512:(half + 1) * 512], cc == 0, cc == 7,
                               [('yT', (b_, cc)), ('wo', None)], [PN(2, half)], inc=(cc == 7))
                        hs_ = slice(half * 512, (half + 1) * 512)
                        TTo('dve', x1t[xb_][:, hs_], PS(2, half), G1[:, hs_], ALU.mult, [PN(2, half), ('G1', None)], [('x1t', (xb_, half))])
                        TTo('pool', x1t[xb_][:, hs_], x1t[xb_][:, hs_], xin[xb_][:, hs_], ALU.add,
                            [('x1t', (xb_, half)), ('xi', xb_)], [('x1t', (xb_, half))])
                    DMA('sp', x1d.ap()[tt * 128:(tt + 1) * 128, :], x1t[xb_][:], [('x1t', (xb_, 0)), ('x1t', (xb_, 1))], [('x1d', tt)])
                    sn = 'ss2_%d' % xb_
                    ACT(junk[:], x1t[xb_][:], AF.Square, [('x1t', (xb_, 0)), ('x1t', (xb_, 1))], [('junk4', None), (sn, 0)], accum_out=ssb[xb_][:, 0:1])
                    ACT(ssb[xb_][:, 1:2], ssb[xb_][:, 0:1], AF.Ln, [(sn, 0), ('epsb', None)], [(sn, 1)], scale=1.0 / D, bias=epsb[:, 0:1])
                    ACT(ssb[xb_][:, 2:3], ssb[xb_][:, 1:2], AF.Exp, [(sn, 1)], [(sn, 2)], scale=-0.5)
                    TS('dve', xn2[xb_][:], x1t[xb_][:], ssb[xb_][:, 2:3], None, ALU.mult, None,
                       [('x1t', (xb_, 0)), ('x1t', (xb_, 1)), (sn, 2)], [('xn2', xb_)])
                    for g in range(2):
                        for j in range(4):
                            kc = g * 4 + j
                            PET(PS(3, g)[:, j * 128:(j + 1) * 128], xn2[xb_][:, kc * 128:(kc + 1) * 128], idf[:],
                                [('xn2', xb_), ('idf', None)], [PN(3, g)], inc=(j == 3))
                        for j in range(4):
                            kc = g * 4 + j
                            o = u2f[xb_][:, kc, :]
                            i_ = PS(3, g)[:, j * 128:(j + 1) * 128]
                            if j % 2 == 0:
                                TS('dve', o, i_, prm[:, 4, kc:kc + 1], prm[:, 5, kc:kc + 1], ALU.mult, ALU.add,
                                   [PN(3, g), ('prm', 4), ('prm', 5)], [('u2f', (xb_, kc))])
                            else:
                                ACT(o, i_, AF.Identity, [PN(3, g), ('prm', 4), ('prm', 5)], [('u2f', (xb_, kc))],
                                    scale=prm[:, 4, kc:kc + 1], bias=prm[:, 5, kc:kc + 1])
                    TTo('pool', u2tmp[:], xn2[xb_][:], MUL2t[:], ALU.mult, [('xn2', xb_), ('MUL2t', None)], [('u2tmp', None)])
                    TTo('pool', u2tb[xb_][:], u2tmp[:], ADD2t[:], ALU.add, [('u2tmp', None), ('ADD2t', None)], [('u2tb', xb_)])
                    DMA('sp', u2tokd.ap()[tt * 128:(tt + 1) * 128, :], u2tb[xb_][:], [('u2tb', xb_)], [('u2tokd', tt)])
                    for kc in range(8):
                        PE(PS(3, 1)[:, 0:NE], u2f[xb_][:, kc, :], rw[:, kc, :], kc == 0, kc == 7,
                           [('u2f', (xb_, kc)), ('rw', None)], [PN(3, 1)], inc=(kc == 7))
                    TTo('dve', L[:, tt, :], PS(3, 1)[:, 0:NE], rbb[:], ALU.add, [PN(3, 1), ('rbb', None)], [('L', tt)])
            S.barrier()
            S.emit()

        if stop == 'D':
            dump('d_x1', x1d.ap(), [T, D], F32, [('x1d', None)])
            dump('d_L', L[:], [128, 32, NE], F32, [('L', None)])
            return finish_dbg()
        IOA = bass.IndirectOffsetOnAxis
        with ExitStack() as st:
            def sb(name, shape, dt=F32):
                return st.enter_context(nc.sbuf_tensor('sb_' + name, shape, dt))
            G2 = sb("G2", [128, D])
            DMA('sp', G2[:], bc(modscr, 40 * 128, D), [('modscr', None)], [('G2', None)])
            stri = sb("stri", [128, 128]); thrB = sb("thrB", [128, NBLK]); pidx = sb("pidx", [128, 1])
            DMA('sp', stri[:], strih.ap(), [], [('stri', None)])
            DMA('sp', thrB[:], thrBh.ap(), [], [('thrB', None)])
            DMA('sp', pidx[:], pidxh.ap(), [], [('pidx', None)])
            gate = sb("gate", [128, 32, NE]); MK = sb("MK", [128, 32, NE]); POS = sb("POS", [128, 32, NE])
            m8 = [sb("m8_%d" % i, [128, 16]) for i in range(2)]
            ex = [sb("ex%d" % i, [128, NE]) for i in range(2)]
            b1in = sb("b1in", [128, 4, 128]); b1T = sb("b1T", [128, 512]); b2s = sb("b2s", [NE, D]); fg = sb("fg", [128, D])
            DMA('sp', b1in[:], b1h.ap().rearrange("(a p) n -> p a n", p=128), [], [('b1in', None)])
            DMA('sp', b2s[:], b2h.ap(), [], [('b2s', None)])
            DMA('sp', fg[:], bc(fngh, 0, D), [], [('fg', None)])
            for a_ in range(4):
                PET(PS(0, 0)[:, a_ * 128:(a_ + 1) * 128], b1in[:, a_, :], idf[:], [('b1in', None), ('idf', None)], [PN(0, 0)], inc=(a_ == 3))
            CP('dve', b1T[:], PS(0, 0), [PN(0, 0)], [('b1T', None)])
            b1v = b1T[:].rearrange("p (e j) -> p e j", j=16)
            TS('dve', b1v[:, :, 8:16], b1v[:, :, 8:16], 1.0, None, ALU.add, None, [('b1T', None)], [('b1T', None)])
            DMA('sp', b1Td.ap().rearrange("(e p) j -> p e j", p=128), b1T[:].rearrange("p (e j) -> p e j", j=16), [('b1T', None)], [('b1Td', None)])
            base = sb("base", [128, NE])
            S.op('dve', nc.vector.memset, dict(ap=base[:], constant=0.0), [], [('base', None)])
            for tt in range(32):
                b_ = tt % 2
                mn = 'm8_%d' % b_
                S.op('dve', nc.vector.max, dict(out=m8[b_][:, 0:8], in_=L[:, tt, :]), [('L', tt)], [(mn, 0)])
                TS('dve', MK[:, tt, :], L[:, tt, :], m8[b_][:, 3:4], None, ALU.is_ge, None, [('L', tt), (mn, 0)], [('MK', tt)])
                TS('dve', m8[b_][:, 8:9], m8[b_][:, 0:1], -1.0, None, ALU.mult, None, [(mn, 0)], [(mn, 1)])
                ACT(ex[b_][:], L[:, tt, :], AF.Exp, [('L', tt), (mn, 1)], [('ex', b_)], bias=m8[b_][:, 8:9])
                TTo('dve', ex[b_][:], ex[b_][:], MK[:, tt, :], ALU.mult, [('ex', b_), ('MK', tt)], [('ex', b_)])
                S.op('dve', nc.vector.reduce_sum, dict(out=m8[b_][:, 9:10], in_=ex[b_][:], axis=AX.X), [('ex', b_)], [(mn, 2)])
                S.op('dve', nc.vector.reciprocal, dict(out=m8[b_][:, 10:11], in_=m8[b_][:, 9:10]), [(mn, 2)], [(mn, 3)])
                TS('dve', gate[:, tt, :], ex[b_][:], m8[b_][:, 10:11], None, ALU.mult, None, [('ex', b_), (mn, 3)], [('gate', tt)])
                PE(PS(0, b_)[:, 0:NE], stri[:], MK[:, tt, :], True, True, [('stri', None), ('MK', tt)], [PN(0, b_)])
                PE(PS(0, b_)[:, NE:2 * NE], ones[:], MK[:, tt, :], True, True, [('ones', None), ('MK', tt)], [PN(0, b_)])
                TTo('dve', POS[:, tt, :], PS(0, b_)[:, 0:NE], base[:], ALU.add, [PN(0, b_), ('base', None)], [('POS', tt)])
                TTo('dve', base[:], base[:], PS(0, b_)[:, NE:2 * NE], ALU.add, [PN(0, b_), ('base', None)], [('base', None)])
            nb = sb("nb", [128, NE]); cA = sb("cA", [128, NE]); cB = sb("cB", [128, NE]); pst = sb("pst", [128, NE])
            S.op('dve', nc.vector.memset, dict(ap=nb[:], constant=0.0), [], [('nb', None)])
            for k in range(T // BLK):
                STT('dve', nb[:], base[:], float(BLK) * k, nb[:], ALU.is_gt, ALU.add, [('base', None), ('nb', None)], [('nb', None)])
            TS('dve', nb[:], nb[:], float(BLK), None, ALU.mult, None, [('nb', None)], [('nb', None)])
            CP('dve', cA[:], nb[:], [('nb', None)], [('cA', None)])
            cur, oth, cn, on = cA, cB, 'cA', 'cB'
            for sh in (1, 2, 4, 8, 16):
                CP('dve', oth[:, 0:sh], cur[:, 0:sh], [(cn, None)], [(on, 0)])
                TTo('dve', oth[:, sh:NE], cur[:, sh:NE], cur[:, 0:NE - sh], ALU.add, [(cn, None)], [(on, 1)])
                cur, oth, cn, on = oth, cur, on, cn
            pend, pendn = cur, cn
            TTo('dve', pst[:], pend[:], nb[:], ALU.subtract, [(pendn, None), ('nb', None)], [('pst', None)])
            D4 = sb("D4", [128, 32, 4], mybir.dt.int32); g4 = sb("g4", [128, 32, 4])
            key = [sb("key%d" % i, [128, NE]) for i in range(2)]
            oh = [sb("oh%d" % i, [128, NE]) for i in range(2)]
            k8 = [sb("k8_%d" % i, [128, 8]) for i in range(2)]
            for tt in range(32):
                b_ = tt % 2
                kn, k8n = 'key%d' % b_, 'k8_%d' % b_
                TTo('dve', POS[:, tt, :], POS[:, tt, :], pst[:], ALU.add, [('POS', tt), ('pst', None)], [('POS', tt)])
                STT('dve', key[b_][:], POS[:, tt, :], 1.0, MK[:, tt, :], ALU.add, ALU.mult, [('POS', tt), ('MK', tt)], [(kn, None)])
                S.op('dve', nc.vector.max, dict(out=k8[b_][:], in_=key[b_][:]), [(kn, None)], [(k8n, None)])
                TS('dve', D4[:, tt, :], k8[b_][:, 0:4], -1.0, None, ALU.add, None, [(k8n, None)], [('D4', tt)])
                for j in range(4):
                    on_ = 'oh%d' % (j % 2)
                    TS('dve', oh[j % 2][:], key[b_][:], k8[b_][:, j:j + 1], None, ALU.is_equal, None, [(kn, None), (k8n, None)], [(on_, None)])
                    TTo('dve', oh[j % 2][:], oh[j % 2][:], gate[:, tt, :], ALU.mult, [(on_, None), ('gate', tt)], [(on_, None)])
                    S.op('dve', nc.vector.reduce_sum, dict(out=g4[:, tt, j:j + 1], in_=oh[j % 2][:], axis=AX.X), [(on_, None)], [('g4', (tt, j))])
            be = sb("be", [128, NBLK]); OFFS = sb("OFFS", [128, NBLK], mybir.dt.int32)
            S.op('dve', nc.vector.memset, dict(ap=be[:], constant=0.0), [], [('be', None)])
            for e in range(NE):
                STT('dve', be[:], thrB[:], pend[:, e:e + 1], be[:], ALU.is_ge, ALU.add, [('thrB', None), (pendn, None), ('be', None)], [('be', None)])
            TS('dve', be[:], be[:], float(NE - 1), 128.0, ALU.min, ALU.mult, [('be', None)], [('be', None)])
            TS('dve', OFFS[:], be[:], pidx[:, 0:1], None, ALU.add, None, [('be', None), ('pidx', None)], [('OFFS', None)])
            ut = [sb("ut%d" % i, [128, D], BF16) for i in range(2)]
            for tt in range(32):
                b_ = tt % 2
                DMA('sp', ut[b_][:], u2tokd.ap()[tt * 128:(tt + 1) * 128, :], [('u2tokd', tt)], [('ut', b_)])
                for j in range(4):
                    S.dma('pool', dict(out=Xg.ap(), out_offset=IOA(ap=D4[:, tt, j:j + 1], axis=0), in_=ut[b_][:], in_offset=None),
                          [('ut', b_), ('D4', tt)], [('Xg', (tt, j))], method=nc.gpsimd.indirect_dma_start)

            with ExitStack() as st2:
                def sb2(name, shape, dt=F32):
                    return st2.enter_context(nc.sbuf_tensor('sb_' + name, shape, dt))
                w1s = [sb2("w1s%d" % i, [128, 8 * 2 * D], BF16) for i in range(2)]
                w2s = [sb2("w2s%d" % i, [128, 8 * D], BF16) for i in range(2)]
                b1g = [sb2("b1g%d" % i, [128, 16]) for i in range(2)]
                xg = [sb2("xg%d" % i, [128, D], BF16) for i in range(4)]
                xT = [sb2("xT%d" % i, [128, 8, BLK], BF16) for i in range(2)]
                actT = [sb2("actT%d" % i, [128, 8, BLK], BF16) for i in range(2)]
                hg = [sb2("hg%d" % i, [128, BLK]) for i in range(2)]
                hl = [sb2("hl%d" % i, [128, BLK]) for i in range(2)]
                yst = [sb2("yst%d" % i, [128, D]) for i in range(2)]

                def load_w(b):
                    wb_ = b % 2
                    off = IOA(ap=OFFS[:, b:b + 1], axis=0)
                    for (dst, dn, src, sn) in ((w1s, 'w1s', w1bd, 'w1bd'), (w2s, 'w2s', w2bd, 'w2bd'), (b1g, 'b1g', b1Td, 'b1Td')):
                        S.dma('pool', dict(out=dst[wb_][:], out_offset=None, in_=src.ap(), in_offset=off),
                              [('OFFS', None), (sn, None)], [(dn, wb_)], method=nc.gpsimd.indirect_dma_start)

                def load_x(b):
                    for sub in range(2):
                        t_ = 2 * b + sub
                        xi_ = 2 * (b % 2) + sub
                        DMA('sp', xg[xi_][:], Xg.ap()[t_ * 128:(t_ + 1) * 128, :], [('Xg', None)], [('xg', xi_)])

                load_w(0)
                ycnt = 0
                for b in range(NBLK):
                    wb_ = b % 2
                    if b + 1 < NBLK:
                        load_w(b + 1)
                    if b == 0:
                        load_x(0)
                    if b + 1 < NBLK:
                        load_x(b + 1)
                    for sub in range(2):
                        t_ = 2 * b + sub
                        xi_ = 2 * (b % 2) + sub
                        for kc in range(8):
                            PET(PSb(3, 0)[:, kc * 128:(kc + 1) * 128], xg[xi_][:, kc * 128:(kc + 1) * 128], idb[:], [('xg', xi_), ('idb', None)], [PN(3, 0)], inc=(kc == 7))
                        CP('act', xT[wb_][:, :, sub * 128:(sub + 1) * 128], PSb(3, 0).rearrange("p (k n) -> p k n", n=128), [PN(3, 0)], [('xT', (wb_, sub))])
                    for cp in range(8):
                        pi_ = cp % 2
                        for gl in range(2):
                            for kc in range(8):
                                c0 = kc * 2 * D + gl * D + cp * 128
                                PE(PS(pi_, gl)[:, 0:BLK], w1s[wb_][:, c0:c0 + 128], xT[wb_][:, kc, :], kc == 0, kc == 7,
                                   [('w1s', wb_), ('xT', (wb_, 0)), ('xT', (wb_, 1))], [PN(pi_, gl)], inc=(kc == 7))
                        bg = b1g[wb_][:, cp:cp + 1]
                        bl = b1g[wb_][:, 8 + cp:9 + cp]
                        TS('dve', hg[pi_][:], PS(pi_, 0)[:, 0:BLK], bg, 7.0, ALU.add, ALU.min, [PN(pi_, 0), ('b1g', wb_)], [('hg', pi_)])
                        ACT(hg[pi_][:], hg[pi_][:], AF.Silu, [('hg', pi_)], [('hg', pi_)], scale=1.702)
                        TS('dve', hl[pi_][:], PS(pi_, 1)[:, 0:BLK], bl, 8.0, ALU.add, ALU.min, [PN(pi_, 1), ('b1g', wb_)], [('hl', pi_)])
                        STT('dve', actT[wb_][:, cp, :], hl[pi_][:], -6.0, hg[pi_][:], ALU.max, ALU.mult, [('hg', pi_), ('hl', pi_)], [('actT', (wb_, cp))])
                    for sub in range(2):
                        t_ = 2 * b + sub
                        ys_ = ycnt % 2
                        ycnt += 1
                        for half in range(2):
                            for cp in range(8):
                                c0 = cp * D + half * 512
                                PE(PS(2, half), actT[wb_][:, cp, sub * 128:(sub + 1) * 128], w2s[wb_][:, c0:c0 + 512], cp == 0, cp == 7,
                                   [('actT', (wb_, cp)), ('w2s', wb_)], [PN(2, half)], inc=(cp == 7))
                            hs_ = slice(half * 512, (half + 1) * 512)
                            if half == 0:
                                ACT(yst[ys_][:, hs_], PS(2, half), AF.Copy, [PN(2, half)], [('yst', (ys_, half))], scale=1.0 / 1.702)
                            else:
                                TS('dve', yst[ys_][:, hs_], PS(2, half), 1.0 / 1.702, None, ALU.mult, None, [PN(2, half)], [('yst', (ys_, half))])
                        DMA('sp', Yg.ap()[t_ * 128:(t_ + 1) * 128, :], yst[ys_][:], [('yst', (ys_, 0)), ('yst', (ys_, 1))], [('Yg', t_)])


                S.barrier()
                S.emit()

            yg = [[sb("yg%d_%d" % (i, j), [128, D]) for j in range(4)] for i in range(2)]
            Yacc = [sb("Yacc%d" % i, [128, D]) for i in range(2)]; gTt = [sb("gTt%d" % i, [NE, 128]) for i in range(2)]
            x1l = [sb("x1l%d" % i, [128, D]) for i in range(2)]; ot = [sb("ot%d" % i, [128, D]) for i in range(2)]
            ssf = [sb("ssf%d" % i, [128, 4]) for i in range(2)]
            for tt in range(32):
                b_ = tt % 2
                ya, x1, o_, sf, gt = Yacc[b_], x1l[b_], ot[b_], ssf[b_], gTt[b_]
                yn, xn_, on_, sn, gn = ('Yacc', b_), ('x1l', b_), ('ot', b_), 'ssf%d' % b_, ('gTt', b_)
                for j in range(4):
                    S.dma('pool', dict(out=yg[b_][j][:], out_offset=None, in_=Yg.ap(), in_offset=IOA(ap=D4[:, tt, j:j + 1], axis=0)),
                          [('D4', tt), ('Yg', None)], [('yg', (b_, j))], method=nc.gpsimd.indirect_dma_start)
                DMA('sp', x1[:], x1d.ap()[tt * 128:(tt + 1) * 128, :], [('x1d', tt)], [xn_])
                TS('dve', ya[:], yg[b_][0][:], g4[:, tt, 0:1], None, ALU.mult, None, [('yg', (b_, 0)), ('g4', (tt, 0))], [yn])
                for j in range(1, 4):
                    STT('dve', ya[:], yg[b_][j][:], g4[:, tt, j:j + 1], ya[:], ALU.mult, ALU.add,
                        [('yg', (b_, j)), ('g4', (tt, j)), yn], [yn])
                PET(PS(3, b_)[0:NE, 0:128], gate[:, tt, :], idf[:], [('gate', tt), ('idf', None)], [PN(3, b_)])
                CP('act', gt[:], PS(3, b_)[0:NE, 0:128], [PN(3, b_)], [gn])
                for half in range(2):
                    hs_ = slice(half * 512, (half + 1) * 512)
                    PE(PS(b_, half), gt[:], b2s[:, hs_], True, True, [gn, ('b2s', None)], [PN(b_, half)])
                    TTo('dve', ya[:, hs_], ya[:, hs_], PS(b_, half), ALU.add, [PN(b_, half), yn], [yn])
                TTo('dve', ya[:], ya[:], G2[:], ALU.mult, [yn, ('G2', None)], [yn])
                TTo('dve', x1[:], x1[:], ya[:], ALU.add, [xn_, yn], [xn_])
                ACT(o_[:], x1[:], AF.Square, [xn_], [on_, (sn, 0)], accum_out=sf[:, 0:1])
                ACT(sf[:, 1:2], sf[:, 0:1], AF.Ln, [(sn, 0), ('epsb', None)], [(sn, 1)], scale=1.0 / D, bias=epsb[:, 0:1])
                ACT(sf[:, 2:3], sf[:, 1:2], AF.Exp, [(sn, 1)], [(sn, 2)], scale=-0.5)
                STT('dve', o_[:], x1[:], sf[:, 2:3], fg[:], ALU.mult, ALU.mult, [xn_, (sn, 2), ('fg', None)], [on_])
                DMA('sp', yh.ap()[tt * 128:(tt + 1) * 128, :], o_[:], [on_], [('y', tt)])
            S.barrier()
            S.emit()
        print("ninstr", S.ninstr)
    return nc


def _consts():
    idf = np.eye(128, dtype=np.float32)
    s = np.arange(128)
    triu = (s[:, None] <= s[None, :]).astype(np.float32)
    tril = (s[:, None] >= s[None, :]).astype(np.float32)
    ones = np.ones((128, 128), np.float32)
    t = np.arange(T)
    row = (t // 64).astype(np.float64)
    col = (t % 64).astype(np.float64)
    inv = 10000.0 ** (-np.arange(16, dtype=np.float64) / 16.0)
    inv32 = inv.astype(np.float32).astype(np.float64)
    cost = np.zeros((128, T), np.float32)
    sint = np.zeros((128, T), np.float32)
    for p in range(128):
        d = p % 64
        pos = row if d < 32 else col
        j = d % 16
        ang = (pos.astype(np.float32) * np.float32(inv32[j])).astype(np.float32)
        first = (d % 32) < 16
        cost[p] = np.cos(ang)
        sint[p] = -np.sin(ang) if first else np.sin(ang)
    stri = (s[:, None] < s[None, :]).astype(np.float32)
    thrB = np.tile((float(BLK) * np.arange(NBLK, dtype=np.float32))[None, :], (128, 1))
    pidx = np.arange(128, dtype=np.float32).reshape(128, 1)
    return dict(idf=idf, idb=idf.astype(ml_dtypes.bfloat16), triu=triu, tril=tril, ones=ones, cost=cost, sint=sint,
                stri=stri, thrB=np.ascontiguousarray(thrB), pidx=pidx)


def _in_maps(x, c, ctx, c_ctx, ada_w, ada_b, norm1_g, norm2_g, w_in, mlstm_gate_b, mlstm_norm_g,
             diff_lambda, diff_norm_g, w_branch_a, w_branch_b, w_out, router_w, router_b,
             exp_w1, exp_b1, exp_w2, exp_b2, final_norm_g):
    f = lambda a: np.ascontiguousarray(np.asarray(a, dtype=np.float32))
    cons = _consts()
    vecs = np.concatenate([f(ada_b)[0].reshape(48, 128), f(norm1_g)[0].reshape(8, 128), f(norm2_g)[0].reshape(8, 128)], axis=0)
    shared = dict(
        ada_w=f(ada_w)[0], vecs=f(vecs), b1r=f(exp_b1)[0].reshape(512, 128), w_in=f(w_in)[0], gate_b=f(mlstm_gate_b)[0],
        mng=f(mlstm_norm_g)[0].reshape(D), dng=f(diff_norm_g)[0].reshape(D), fng=f(final_norm_g), dlam=f(diff_lambda)[0].reshape(256),
        w_a=f(w_branch_a)[0], w_b=f(w_branch_b)[0], w_o=f(w_out)[0], rw=f(router_w)[0], rb=f(router_b)[0],
        w1=f(exp_w1)[0], w2=f(exp_w2)[0], b2=f(exp_b2)[0], **cons)
    maps = []
    xf, cf, ctxf, ccf = f(x), f(c), f(ctx), f(c_ctx)
    for b in range(8):
        m = dict(shared)
        m["x"] = xf[b]
        m["ctx"] = ctxf[b]
        m["cvec"] = np.ascontiguousarray(np.stack([cf[b], ccf], axis=1))
        maps.append(m)
    return maps


def kernel(**inputs):
    maps = _in_maps(**inputs)
    nc = build_nc()
    res = run_bass_kernel_spmd(nc, maps, core_ids=list(range(8)))
    return np.stack([np.asarray(r["y"], dtype=np.float32) for r in res.results], axis=0)
```

```python
import math
import os
from contextlib import ExitStack

import ml_dtypes
import numpy as np

import concourse.bass as bass
import concourse.mybir as mybir
from concourse.bass_utils import run_bass_kernel_spmd

F32 = mybir.dt.float32
BF16 = mybir.dt.bfloat16
ALU = mybir.AluOpType
AF = mybir.ActivationFunctionType
AX = mybir.AxisListType

D = 1024
T = 4096
TC = 256
TT = T + TC
NT = TT // 128
NE = 32
BLK = 256
NBLK = 96
NBUF = NBLK * BLK
EPS = 1e-6
LAM_INIT = 0.8 - 0.6 * math.exp(0.0)
O_MQ, O_MK, O_MV, O_MG, O_MO, O_DQ, O_DK, O_DV, O_GA, O_GB = 0, 512, 1024, 2048, 2064, 3088, 4112, 5136, 6160, 7184
INC = 8208

ENG_ATTR = {'pe': 'tensor', 'act': 'scalar', 'dve': 'vector', 'pool': 'gpsimd', 'sp': 'sync'}
NDMA_SEMS = 8
NPOOL_SEMS = 4


class Sched:
    def __init__(self, nc, stack):
        self.nc = nc
        self.prog = {e: [] for e in ENG_ATTR}
        self.sem = {}
        for e in ENG_ATTR:
            self.sem[e] = stack.enter_context(nc.semaphore('s_' + e))
        self.dq = {}
        for q in ('sp', 'pool'):
            for i in range(NDMA_SEMS):
                k = ('dma', q, i)
                self.sem[k] = stack.enter_context(nc.semaphore('d_%s%d' % (q, i)))
            self.dq[q] = 0
        self.count = {k: 0 for k in self.sem}
        self.seen = {e: {} for e in ENG_ATTR}
        self.state = {}
        self.ninstr = 0

    @staticmethod
    def _ov(a, b):
        return a is None or b is None or a == b

    def _collect(self, eng, reads, writes, is_dma):
        need = {}

        def add(ev, kind):
            if ev is None:
                return
            k, v = ev
            if (not is_dma) and k == eng:
                if kind == 'rar' or (eng == 'pe' and kind != 'raw'):
                    return
            if need.get(k, 0) < v:
                need[k] = v

        for (n, s) in reads:
            for slot, st in self.state.get(n, {}).items():
                if self._ov(slot, s):
                    add(st[0], 'raw')
                    if n.startswith('PS'):
                        for r in st[1]:
                            add(r, 'rar')
        for (n, s) in writes:
            for slot, st in self.state.get(n, {}).items():
                if self._ov(slot, s):
                    add(st[0], 'waw')
                    for r in st[1]:
                        add(r, 'war')
        return need

    def _emit_waits(self, eng, need):
        seen = self.seen[eng]
        for k, v in need.items():
            if seen.get(k, 0) >= v:
                continue
            seen[k] = v
            self.prog[eng].append(('wait', self.sem[k], v))

    def _mark(self, ev, reads, writes):
        for (n, s) in writes:
            d = self.state.setdefault(n, {})
            if s is None:
                d.clear()
                d[None] = [ev, []]
            else:
                d[s] = [ev, []]
        for (n, s) in reads:
            d = self.state.setdefault(n, {})
            st = d.setdefault(s, [None, []])
            st[1] = [r for r in st[1] if r[0] != ev[0]] + [ev]

    def op(self, eng, method, kw, reads=(), writes=(), inc=True):
        need = self._collect(eng, reads, writes, False)
        self._emit_waits(eng, need)
        self.ninstr += 1
        if inc:
            self.count[eng] += 1
            ev = (eng, self.count[eng])
            self.prog[eng].append(('ins', method, kw, self.sem[eng], 1))
        else:
            ev = (eng, self.count[eng] + 1)
            self.prog[eng].append(('ins', method, kw, None, 0))
        self._mark(ev, reads, writes)

    def dma(self, q, kw, reads=(), writes=(), method=None):
        nsem = NDMA_SEMS if q != 'pool' else NPOOL_SEMS
        i = self.dq[q] % nsem
        self.dq[q] += 1
        k = ('dma', q, i)
        need = self._collect(q, reads, writes, True)
        if self.count[k] > 0:
            need[k] = max(need.get(k, 0), self.count[k])
        self._emit_waits(q, need)
        self.ninstr += 1
        self.count[k] += 16
        ev = (k, self.count[k])
        if method is None:
            method = getattr(self.nc, ENG_ATTR[q]).dma_start
        self.prog[q].append(('ins', method, kw, self.sem[k], 16))
        self._mark(ev, reads, writes)

    def barrier(self):
        for e in ENG_ATTR:
            need = {k: v for k, v in self.count.items() if v > 0 and k != e}
            self._emit_waits(e, need)

    def emit(self):
        nc = self.nc
        if os.environ.get('DBGPROG'):
            names = {id(v): k for k, v in self.sem.items()}
            for e in ENG_ATTR:
                print('ENGINE', e)
                c = 0
                for it in self.prog[e][-int(os.environ['DBGPROG']):]:
                    if it[0] == 'wait':
                        print('   wait', names[id(it[1])], it[2])
                    else:
                        print('   ins', getattr(it[1], '__name__', it[1]), 'inc' if it[3] is not None else '-', [k for k in it[2] if k in ('func',)] and it[2].get('func'))
        with nc.Block() as block:
            for e, attr in ENG_ATTR.items():
                prog = self.prog[e]

                def body(engine, prog=prog):
                    for it in prog:
                        if it[0] == 'wait':
                            engine.wait_ge(it[1], it[2])
                        else:
                            ins = it[1](**it[2])
                            if it[3] is not None:
                                ins.then_inc(it[3], it[4])
                getattr(block, attr)(body)
        self.prog = {e: [] for e in ENG_ATTR}


def build_nc(dbg=False, stop=None):
    nc = bass.Bass("TRN2", target_bir_lowering=False)

    def din(name, shape, dt=F32):
        return nc.dram_tensor(name, shape, dt, kind="ExternalInput")

    def dscr(name, shape, dt=F32):
        return nc.dram_tensor(name, shape, dt, kind="Internal")

    xh = din("x", [T, D]); ctxh = din("ctx", [TC, D]); cvh = din("cvec", [D, 2])
    adawh = din("ada_w", [D, 6 * D]); vecsh = din("vecs", [64, 128]); b1h = din("b1r", [512, 128])
    winh = din("w_in", [D, INC]); gbh = din("gate_b", [16]); mngh = din("mng", [D]); dngh = din("dng", [D])
    fngh = din("fng", [D]); dlh = din("dlam", [256]); wah = din("w_a", [D, D]); wbh = din("w_b", [D, D])
    woh = din("w_o", [D, D]); rwh = din("rw", [D, NE]); rbh = din("rb", [NE])
    w1h = din("w1", [NE, D, 2 * D]); w2h = din("w2", [NE, D, D]); b2h = din("b2", [NE, D])
    idfh = din("idf", [128, 128]); idbh = din("idb", [128, 128], BF16)
    triuh = din("triu", [128, 128]); trilh = din("tril", [128, 128]); onesh = din("ones", [128, 128])
    cosh_ = din("cost", [128, T]); sinh_ = din("sint", [128, T])
    strih = din("stri", [128, 128]); thrBh = din("thrB", [128, NBLK]); pidxh = din("pidx", [128, 1])
    yh = nc.dram_tensor("y", [T, D], F32, kind="ExternalOutput")

    modscr = dscr("modscr", [48 * 128]); hfscr = dscr("hfscr", [4, 32, 128, 256])
    aTd = dscr("aTd", [128, 8, T], BF16); bTd = dscr("bTd", [128, 8, T], BF16)
    sgd = dscr("sgd", [128, 16, T], BF16); x1d = dscr("x1d", [T, D]); u2d = dscr("u2d", [128, 8, T], BF16)
    w1bd = dscr("w1bd", [NE * 128, 8 * 2 * D], BF16); w2bd = dscr("w2bd", [NE * 128, 8 * D], BF16)
    b1Td = dscr("b1Td", [NE * 128, 16]); u2tokd = dscr("u2tokd", [T, D], BF16)
    Xg = dscr("Xg", [NBUF, D], BF16); Yg = dscr("Yg", [NBUF, D])

    def bc(h, off, n, parts=128):
        return bass.AP(h, off, [[0, parts], [1, n]])

    with ExitStack() as gst:
        S = Sched(nc, gst)

        def sbg(name, shape, dt=F32):
            return gst.enter_context(nc.sbuf_tensor('sb_' + name, shape, dt))

        PSUM = [gst.enter_context(nc.psum_tensor("PS%d" % i, [128, 1024], F32)) for i in range(4)]

        def PS(i, b):
            return PSUM[i][:, b * 512:(b + 1) * 512]

        def PSb(i, b):
            return PSUM[i][:].bitcast(BF16)[:, b * 1024:(b + 1) * 1024]

        def PN(i, b):
            return ('PS%d' % i, b)

        def PE(out, lhsT, rhs, start, stop, R, W, inc=True):
            S.op('pe', nc.tensor.matmul, dict(out=out, lhsT=lhsT, rhs=rhs, start=start, stop=stop), R, W, inc)

        def PET(out, in_, ident, R, W, inc=True):
            S.op('pe', nc.tensor.transpose, dict(out=out, in_=in_, identity=ident), R, W, inc)

        def ACT(out, in_, func, R, W, **kw):
            S.op('act', nc.scalar.activation, dict(out=out, in_=in_, func=func, **kw), R, W)

        def TS(eng, out, in0, s1, s2, op0, op1, R, W):
            m = nc.vector.tensor_scalar if eng == 'dve' else nc.gpsimd.tensor_scalar
            kw = dict(out=out, in0=in0, scalar1=s1, scalar2=s2, op0=op0)
            if op1 is not None:
                kw['op1'] = op1
            S.op(eng, m, kw, R, W)

        def TTo(eng, out, in0, in1, op, R, W):
            m = nc.vector.tensor_tensor if eng == 'dve' else nc.gpsimd.tensor_tensor
            S.op(eng, m, dict(out=out, in0=in0, in1=in1, op=op), R, W)

        def STT(eng, out, in0, scalar, in1, op0, op1, R, W):
            m = nc.vector.scalar_tensor_tensor if eng == 'dve' else nc.gpsimd.scalar_tensor_tensor
            S.op(eng, m, dict(out=out, in0=in0, scalar=scalar, in1=in1, op0=op0, op1=op1), R, W)

        def CP(eng, out, in_, R, W):
            if eng == 'act':
                ACT(out, in_, AF.Copy, R, W)
            else:
                m = nc.vector.tensor_copy if eng == 'dve' else nc.gpsimd.tensor_copy
                S.op(eng, m, dict(out=out, in_=in_), R, W)

        def DMA(q, out, in_, R, W):
            S.dma(q, dict(out=out, in_=in_), R, W)


        def dump(name, src, shape, dt, reads):
            h_ = nc.dram_tensor(name, shape, dt, kind="ExternalOutput")
            DMA('sp', h_.ap(), src, reads, [(name, None)])

        def finish_dbg():
            S.barrier()
            S.emit()
            return nc
        idf = sbg("idf", [128, 128]); idb = sbg("idb", [128, 128], BF16)
        triu = sbg("triu", [128, 128]); tril = sbg("tril", [128, 128]); ones = sbg("ones", [128, 128])
        epsb = sbg("epsb", [128, 1])
        prm = sbg("prm", [128, 6, 8])
        DMA('sp', idf[:], idfh.ap(), [], [('idf', None)])
        DMA('sp', idb[:], idbh.ap(), [], [('idb', None)])
        DMA('sp', triu[:], triuh.ap(), [], [('triu', None)])
        DMA('sp', tril[:], trilh.ap(), [], [('tril', None)])
        DMA('sp', ones[:], onesh.ap(), [], [('ones', None)])
        S.op('dve', nc.vector.memset, dict(ap=epsb[:], constant=EPS), [], [('epsb', None)])

        with ExitStack() as st:
            def sb(name, shape, dt=F32):
                return st.enter_context(nc.sbuf_tensor('sb_' + name, shape, dt))
            cv = sb("cv", [128, 8, 2]); sc = sb("sc", [128, 8, 2]); vin = sb("vin", [64, 128]); vT = sb("vT", [128, 64])
            adw = [sb("adw%d" % i, [128, 8, 1024]) for i in range(2)]
            modT = sb("modT", [128, 48, 2]); mrow = sb("mrow", [48, 128]); tmpa = sb("tmpa", [128, 8])
            DMA('sp', cv[:], cvh.ap().rearrange("(k p) n -> p k n", p=128), [], [('cv', None)])
            DMA('sp', vin[:], vecsh.ap(), [], [('vin', None)])
            ACT(sc[:], cv[:], AF.Silu, [('cv', None)], [('sc', None)])
            PET(PS(0, 0)[:, 0:64], vin[:], idf[0:64, 0:64], [('vin', None), ('idf', None)], [PN(0, 0)])
            CP('dve', vT[:], PS(0, 0)[:, 0:64], [PN(0, 0)], [('vT', None)])
            for jb in range(6):
                DMA('sp', adw[jb % 2][:], adawh.ap()[:, jb * 1024:(jb + 1) * 1024].rearrange("(k p) n -> p k n", p=128),
                    [], [('adw', jb % 2)])
                for jj in range(8):
                    j = jb * 8 + jj
                    for kc in range(8):
                        PE(PS(0, 1)[:, 2 * j:2 * j + 2], adw[jb % 2][:, kc, jj * 128:(jj + 1) * 128], sc[:, kc, :],
                           kc == 0, kc == 7, [('adw', jb % 2), ('sc', None)], [PN(0, 1)], inc=(kc == 7))
            pm = PS(0, 1)[:, 0:96].rearrange("p (j n) -> p j n", n=2)
            for n_ in range(2):
                TTo('dve', modT[:, :, n_], pm[:, :, n_], vT[:, 0:48], ALU.add, [PN(0, 1), ('vT', None)], [('modT', n_)])
            for (pi, n_, gcol, sccol, shcol) in ((0, 0, 48, 8, 0), (2, 1, 48, 8, 0), (4, 0, 56, 32, 24)):
                TS('dve', tmpa[:], modT[:, sccol:sccol + 8, n_], 1.0, None, ALU.add, None, [('modT', n_)], [('tmpa', None)])
                TTo('dve', prm[:, pi, :], tmpa[:], vT[:, gcol:gcol + 8], ALU.mult, [('tmpa', None), ('vT', None)], [('prm', pi)])
                CP('dve', prm[:, pi + 1, :], modT[:, shcol:shcol + 8, n_], [('modT', n_)], [('prm', pi + 1)])
            PET(PS(0, 0)[0:48, 0:128], modT[:, :, 0], idf[:], [('modT', 0), ('idf', None)], [PN(0, 0)])
            CP('dve', mrow[:], PS(0, 0)[0:48, 0:128], [PN(0, 0)], [('mrow', None)])
            DMA('sp', modscr.ap().rearrange("(j p) -> j p", p=128), mrow[:], [('mrow', None)], [('modscr', None)])
            S.barrier()
            S.emit()

        if stop == 'A':
            dump('d_prm', prm[:], [128, 6, 8], F32, [('prm', None)])
            return finish_dbg()
        ust = ExitStack() if stop is None else gst
        uT = ust.enter_context(nc.sbuf_tensor("sb_uT", [128, 8, TT], BF16))

        def norm_rstd(src, junk, ssb, nm):
            ACT(junk, src, AF.Square, [(nm, None)], [('junk', None), (nm + 'ss', 0)], accum_out=ssb[:, 0:1])
            ACT(ssb[:, 1:2], ssb[:, 0:1], AF.Ln, [(nm + 'ss', 0), ('epsb', None)], [(nm + 'ss', 1)], scale=1.0 / D, bias=epsb[:, 0:1])
            ACT(ssb[:, 2:3], ssb[:, 1:2], AF.Exp, [(nm + 'ss', 1)], [(nm + 'ss', 2)], scale=-0.5)

        with ExitStack() as st:
            def sb(name, shape, dt=F32):
                return st.enter_context(nc.sbuf_tensor('sb_' + name, shape, dt))
            xin = [sb("xin%d" % i, [128, D]) for i in range(3)]
            ssb = [sb("ssb%d" % i, [128, 4]) for i in range(3)]
            xn = [sb("xn%d" % i, [128, D], BF16) for i in range(2)]
            junk = sb("junk", [128, D], BF16)
            for ti in range(NT):
                b3 = ti % 3
                src = ctxh.ap()[ti * 128:(ti + 1) * 128, :] if ti < 2 else xh.ap()[(ti - 2) * 128:(ti - 1) * 128, :]
                nm = 'xin%d' % b3
                DMA('sp', xin[b3][:], src, [], [(nm, None)])
                norm_rstd(xin[b3][:], junk[:], ssb[b3], nm)
                xnn = 'xn%d' % (ti % 2)
                TS('dve', xn[ti % 2][:], xin[b3][:], ssb[b3][:, 2:3], None, ALU.mult, None, [(nm, None), (nm + 'ss', 2)], [(xnn, None)])
                pi = 2 if ti < 2 else 0
                for g in range(2):
                    for j in range(4):
                        kc = g * 4 + j
                        PET(PSb(3, g)[:, j * 128:(j + 1) * 128], xn[ti % 2][:, kc * 128:(kc + 1) * 128], idb[:],
                            [(xnn, None), ('idb', None)], [PN(3, g)], inc=(j == 3))
                    for j in range(4):
                        kc = g * 4 + j
                        o = uT[:, kc, ti * 128:(ti + 1) * 128]
                        i_ = PSb(3, g)[:, j * 128:(j + 1) * 128]
                        if j % 2 == 0:
                            TS('dve', o, i_, prm[:, pi, kc:kc + 1], prm[:, pi + 1, kc:kc + 1], ALU.mult, ALU.add,
                               [PN(3, g), ('prm', pi), ('prm', pi + 1)], [('uT', ti)])
                        else:
                            ACT(o, i_, AF.Identity, [PN(3, g), ('prm', pi), ('prm', pi + 1)], [('uT', ti)],
                                scale=prm[:, pi, kc:kc + 1], bias=prm[:, pi + 1, kc:kc + 1])
            S.barrier()
            S.emit()

        if dbg:
            dbg_u = nc.dram_tensor("dbg_u", [128, 8, TT], BF16, kind="ExternalOutput")
            DMA('sp', dbg_u.ap(), uT[:], [('uT', None)], [('dbg_u', None)])

        if stop == 'B':
            dump('d_uT', uT[:], [128, 8, TT], BF16, [('uT', None)])
            return finish_dbg()
        def wslice(c0, n):
            return winh.ap()[:, c0:c0 + n].rearrange("(k p) n -> p k n", p=128)

        with ExitStack() as st:
            def sb(name, shape, dt=F32):
                return st.enter_context(nc.sbuf_tensor('sb_' + name, shape, dt))
            wg = sb("wg", [128, 8, 16], BF16); gb34 = sb("gb34", [128, NT, 16]); G = sb("G", [128, NT, 16])
            SP = [sb("SP%d" % d, [128, NT * 4]) for d in range(2)]
            EE = [sb("EE%d" % d, [128, NT, 4]) for d in range(2)]
            WW = [sb("WW%d" % d, [128, NT, 4]) for d in range(2)]
            EL = [sb("EL%d" % d, [128, NT, 4]) for d in range(2)]
            tmpw = sb("tmpw", [128, NT, 4])
            cst = [sb("cst%d" % d, [128, 272]) for d in range(2)]
            gm = sb("gm", [128, D])
            DMA('pool', wg[:], wslice(O_MG, 16), [], [('wg', None)])
            gb16 = sb("gb16", [128, 16])
            DMA('sp', gb16[:], bc(gbh, 0, 16), [], [('gb16', None)])
            DMA('sp', gm[:], bc(mngh, 0, D), [], [('gm', None)])
            for ti in range(NT):
                bank = 0 if ti < 32 else 1
                col = (ti % 32) * 16
                for kc in range(8):
                    PE(PS(0, bank)[:, col:col + 16], uT[:, kc, ti * 128:(ti + 1) * 128], wg[:, kc, :], kc == 0, kc == 7,
                       [('uT', None), ('wg', None)], [PN(0, bank)], inc=(kc == 7))
            for ti in range(NT):
                bank = 0 if ti < 32 else 1
                col = (ti % 32) * 16
                TTo('dve', G[:, ti, :], PS(0, bank)[:, col:col + 16], gb16[:], ALU.add, [PN(0, bank), ('gb16', None)], [('G', ti)])
            if stop == 'C0a':
                dump('d_G', G[:], [128, NT, 16], F32, [('G', None)])
                return finish_dbg()
            for d in range(2):
                spv = SP[d][:].rearrange("p (t n) -> p t n", n=4)
                ACT(spv, G[:, :, 8 * d + 4:8 * d + 8], AF.Exp, [('G', None)], [('SP', d)], scale=-1.0)
                ACT(SP[d][:], SP[d][:], AF.Ln, [('SP', d), ('ones', None)], [('SP', d)], bias=ones[:, 0:1])
                if stop == 'C0b':
                    continue
                tri = triu if d == 0 else tril
                PE(PS(1, d)[:, 0:136], tri[:], SP[d][:], True, True, [('SP', d), ('triu', None), ('tril', None)], [PN(1, d)])
                PE(PS(1, d)[:, 136:272], ones[:], SP[d][:], True, True, [('SP', d), ('ones', None)], [PN(1, d)])
                if stop == 'C0c':
                    CP('dve', EE[d][:].rearrange("p t n -> p (t n)"), PS(1, d)[:, 0:136], [PN(1, d)], [('EE', d)])
                    continue
                CP('dve', cst[d][:], PS(1, d)[:, 0:272], [PN(1, d)], [('cst', d)])
                cs = cst[d][:, 0:136].rearrange("p (t n) -> p t n", n=4)
                tt_ = cst[d][:, 136:272].rearrange("p (t n) -> p t n", n=4)
                SK = os.environ.get('DBGSKIP', '')
                if '1' not in SK:
                    ACT(EE[d][:], cs, AF.Exp, [('cst', d)], [('EE', d)], scale=-1.0)
                if '2' not in SK:
                    TTo('dve', tmpw[:], G[:, :, 8 * d:8 * d + 4], cs, ALU.add, [('G', None), ('cst', d)], [('tmpw', None)])
                if '3' not in SK:
                    ACT(WW[d][:], tmpw[:], AF.Exp, [('tmpw', None)], [('WW', d)])
                if '4' not in SK:
                    ACT(EL[d][:], tt_, AF.Exp, [('cst', d)], [('EL', d)], scale=-1.0)

            if stop == 'C0b':
                dump('d_SP', SP[0][:], [128, NT * 4], F32, [('SP', 0)])
                return finish_dbg()
            if stop == 'C0c':
                dump('d_EE', EE[0][:], [128, NT, 4], F32, [('EE', 0)])
                return finish_dbg()
            if stop == 'C0':
                dump('d_EE', EE[0][:], [128, NT, 4], F32, [('EE', 0)])
                dump('d_WW', WW[1][:], [128, NT, 4], F32, [('WW', 1)])
                dump('d_EL', EL[0][:], [128, NT, 4], F32, [('EL', 0)])
                dump('d_G', G[:], [128, NT, 16], F32, [('G', None)])
                return finish_dbg()
            wm = sb("wm", [128, 8, 768], BF16)
            qT = sb("qT", [128, TT], BF16); kT = sb("kT", [128, TT], BF16)
            vt = sb("vt", [128, NT, 257], BF16)
            kt = [sb("kt%d" % d, [128, NT, 128], BF16) for d in range(2)]
            SD = [sb("SD%d" % i, [128, 128], BF16) for i in range(2)]
            Cf = sb("Cf", [128, 257]); Cb = sb("Cb", [128, 257], BF16); tmpC = sb("tmpC", [128, 257])
            sml = [sb("sml%d" % i, [128, 8]) for i in range(2)]
            hst = [sb("hst%d" % i, [128, 256]) for i in range(3)]
            hfl = [sb("hfl%d" % i, [128, 256]) for i in range(3)]
            hs = [sb("hs%d" % i, [128, 256]) for i in range(2)]
            osg = [sb("osg%d" % i, [128, 256]) for i in range(2)]
            ab = [sb("ab%d" % i, [128, 256], BF16) for i in range(2)]
            aTs = [sb("aTs%d" % i, [128, 2, 512], BF16) for i in range(2)]
            junk2 = sb("junk2", [128, 256], BF16)
            S.op('pool', nc.gpsimd.memset, dict(ap=vt[:, :, 256:257], constant=1.0), [], [('vt1', None)])

            for h in range(4):
                DMA('pool', wm[:, :, 0:128], wslice(O_MQ + h * 128, 128), [], [('wm', 0)])
                DMA('pool', wm[:, :, 128:256], wslice(O_MK + h * 128, 128), [], [('wm', 1)])
                DMA('pool', wm[:, :, 256:512], wslice(O_MV + h * 256, 256), [], [('wm', 2)])
                DMA('pool', wm[:, :, 512:768], wslice(O_MO + h * 256, 256), [], [('wm', 3)])
                cnt = 0
                for blk in range(9):
                    c0 = blk * 512
                    n = min(512, TT - c0)
                    for which in range(2):
                        pi_, pb_ = (cnt // 2) % 2, cnt % 2
                        cnt += 1
                        for kc in range(8):
                            PE(PS(pi_, pb_)[:, 0:n], wm[:, kc, which * 128:(which + 1) * 128], uT[:, kc, c0:c0 + n], kc == 0, kc == 7,
                               [('wm', which), ('uT', None)], [PN(pi_, pb_)], inc=(kc == 7))
                        if which == 0:
                            ACT(qT[:, c0:c0 + n], PS(pi_, pb_)[:, 0:n], AF.Copy, [PN(pi_, pb_)], [('qT', blk)], scale=128.0 ** -0.5)
                        else:
                            CP('dve', kT[:, c0:c0 + n], PS(pi_, pb_)[:, 0:n], [PN(pi_, pb_)], [('kT', blk)])
                for ti in range(NT):
                    pb_ = ti % 2
                    for kc in range(8):
                        PE(PS(2, pb_)[:, 0:384], uT[:, kc, ti * 128:(ti + 1) * 128], wm[:, kc, 128:512], kc == 0, kc == 7,
                           [('uT', None), ('wm', 1), ('wm', 2)], [PN(2, pb_)], inc=(kc == 7))
                    TS('dve', kt[0][:, ti, :], PS(2, pb_)[:, 0:128], WW[0][:, ti, h:h + 1], None, ALU.mult, None,
                       [PN(2, pb_), ('WW', 0)], [('kt0', ti)])
                    TS('dve', kt[1][:, ti, :], PS(2, pb_)[:, 0:128], WW[1][:, ti, h:h + 1], None, ALU.mult, None,
                       [PN(2, pb_), ('WW', 1)], [('kt1', ti)])
                    CP('act', vt[:, ti, 0:256], PS(2, pb_)[:, 128:384], [PN(2, pb_)], [('vt', ti)])

                for d in range(2):
                    order = list(range(NT)) if d == 0 else [1, 0] + list(range(NT - 1, 1, -1))
                    mask = triu if d == 0 else tril
                    lat = [t_ for t_ in order if t_ >= 2]

                    def emit_sd(ti, li):
                        sl = li % 2
                        cs_ = slice(ti * 128, (ti + 1) * 128)
                        PE(PS(0, sl)[:, 0:128], kT[:, cs_], qT[:, cs_], True, True, [('kT', None), ('qT', None)], [PN(0, sl)])
                        STT('dve', SD[sl][:], PS(0, sl)[:, 0:128], WW[d][:, ti, h:h + 1], mask[:], ALU.mult, ALU.mult,
                            [PN(0, sl), ('WW', d), ('triu', None), ('tril', None)], [('SD', sl)])

                    def emit_oraw(li_):
                        ti_ = lat[li_]
                        b2_ = li_ % 2
                        for kc in range(8):
                            PE(PS(3, 0)[:, 0:256], uT[:, kc, ti_ * 128:(ti_ + 1) * 128], wm[:, kc, 512:768], kc == 0, kc == 7,
                               [('uT', None), ('wm', 3)], [PN(3, 0)], inc=(kc == 7))
                        ACT(osg[b2_][:], PS(3, 0)[:, 0:256], AF.Sigmoid, [PN(3, 0)], [('osg', b2_)])
                        TTo('pool', osg[b2_][:], osg[b2_][:], gm[:, h * 256:(h + 1) * 256], ALU.mult, [('osg', b2_), ('gm', None)], [('osg', b2_)])

                    def emit_hfload(li_):
                        c_ = lat[li_] - 2
                        DMA('sp', hfl[li_ % 3][:], hfscr.ap()[h, c_], [('hf', (h, c_))], [('hfl', li_ % 3)])

                    def emit_tr(li_):
                        c_ = lat[li_] - 2
                        b2_ = li_ % 2
                        blk, sub = c_ // 4, c_ % 4
                        ab_ = blk % 2
                        for jj in range(2):
                            PET(PSb(3, 1)[:, jj * 128:(jj + 1) * 128], ab[b2_][:, jj * 128:(jj + 1) * 128], idb[:],
                                [('ab', b2_), ('idb', None)], [PN(3, 1)], inc=(jj == 1))
                        CP('act', aTs[ab_][:, :, sub * 128:(sub + 1) * 128],
                           PSb(3, 1)[:, 0:256].rearrange("p (a b) -> p a b", b=128), [PN(3, 1)], [('aTs', ab_)])
                        if sub == 0:
                            DMA('sp', aTd.ap()[:, 2 * h:2 * h + 2, blk * 512:(blk + 1) * 512], aTs[ab_][:],
                                [('aTs', ab_)], [('aTd', (h, blk))])

                    emit_sd(lat[0], 0)
                    if d == 1:
                        emit_hfload(0)
                        emit_hfload(1)
                        emit_oraw(0)
                    li = 0
                    for j, ti in enumerate(order):
                        latent = ti >= 2
                        last = (j == NT - 1)
                        cs_ = slice(ti * 128, (ti + 1) * 128)
                        if latent and li + 1 < len(lat):
                            emit_sd(lat[li + 1], li + 1)
                        pb_ = j % 2
                        if not last:
                            PE(PS(2, pb_)[:, 0:257], kt[d][:, ti, :], vt[:, ti, :], True, True,
                               [('kt%d' % d, ti), ('vt', ti), ('vt1', None)], [PN(2, pb_)])
                        if latent:
                            sl = li % 2
                            PE(PS(1, sl)[:, 0:257], SD[sl][:], vt[:, ti, :], True, j == 0,
                               [('SD', sl), ('vt', ti), ('vt1', None)], [PN(1, sl)], inc=(j == 0))
                            if j > 0:
                                PE(PS(1, sl)[:, 0:257], qT[:, cs_], Cb[:], False, True, [('qT', None), ('Cb', None)], [PN(1, sl)])
                        if not last:
                            Ecol = EL[d][:, ti, h:h + 1]
                            if j == 0:
                                ACT(Cf[:], PS(2, pb_)[:, 0:257], AF.Copy, [PN(2, pb_), ('EL', d)], [('Cf', None)], scale=Ecol)
                                ACT(Cb[:], PS(2, pb_)[:, 0:257], AF.Copy, [PN(2, pb_), ('EL', d)], [('Cb', None)], scale=Ecol)
                            else:
                                TTo('dve', tmpC[:], PS(2, pb_)[:, 0:257], Cf[:], ALU.add, [PN(2, pb_), ('Cf', None)], [('tmpC', None)])
                                ACT(Cb[:], tmpC[:], AF.Copy, [('tmpC', None), ('EL', d)], [('Cb', None)], scale=Ecol)
                                ACT(Cf[:], tmpC[:], AF.Copy, [('tmpC', None), ('EL', d)], [('Cf', None)], scale=Ecol)
                        if latent:
                            sl = li % 2
                            c = ti - 2
                            num = PS(1, sl)
                            sm = sml[li % 2]
                            smn = 'sml%d' % (li % 2)
                            e_ = EE[d][:, ti, h:h + 1]
                            if d == 1:
                                if li + 1 < len(lat):
                                    emit_oraw(li + 1)
                                if li >= 1:
                                    emit_tr(li - 1)
                                if li + 2 < len(lat):
                                    emit_hfload(li + 2)
                            TS('dve', sm[:, 0:1], num[:, 256:257], e_, None, ALU.mult, None, [PN(1, sl), ('EE', d)], [(smn, 0)])
                            TS('dve', sm[:, 7:8], sm[:, 0:1], -1.0, None, ALU.mult, None, [(smn, 0)], [(smn, 7)])
                            TTo('dve', sm[:, 1:2], sm[:, 0:1], sm[:, 7:8], ALU.max, [(smn, 0), (smn, 7)], [(smn, 1)])
                            TS('dve', sm[:, 1:2], sm[:, 1:2], 1.0, None, ALU.max, None, [(smn, 1)], [(smn, 1)])
                            S.op('dve', nc.vector.reciprocal, dict(out=sm[:, 2:3], in_=sm[:, 1:2]), [(smn, 1)], [(smn, 2)])
                            TS('dve', sm[:, 3:4], sm[:, 2:3], e_, None, ALU.mult, None, [(smn, 2), ('EE', d)], [(smn, 3)])
                            if d == 0:
                                b3 = li % 3
                                ACT(hst[b3][:], num[:, 0:256], AF.Copy, [PN(1, sl), (smn, 3)], [('hst', b3)], scale=sm[:, 3:4])
                                DMA('sp', hfscr.ap()[h, c], hst[b3][:], [('hst', b3)], [('hf', (h, c))])
                            else:
                                b3 = li % 3
                                b2 = li % 2
                                STT('dve', hs[b2][:], num[:, 0:256], sm[:, 3:4], hfl[b3][:], ALU.mult, ALU.add,
                                    [PN(1, sl), (smn, 3), ('hfl', b3)], [('hs', b2)])
                                ACT(junk2[:], hs[b2][:], AF.Square, [('hs', b2)], [('junk2', None), (smn, 4)], accum_out=sm[:, 4:5])
                                ACT(sm[:, 5:6], sm[:, 4:5], AF.Ln, [(smn, 4), ('epsb', None)], [(smn, 5)], scale=1.0 / 256, bias=epsb[:, 0:1])
                                ACT(sm[:, 6:7], sm[:, 5:6], AF.Exp, [(smn, 5)], [(smn, 6)], scale=-0.5)
                                STT('dve', ab[b2][:], hs[b2][:], sm[:, 6:7], osg[b2][:], ALU.mult, ALU.mult,
                                    [('hs', b2), (smn, 6), ('osg', b2)], [('ab', b2)])
                                if li == len(lat) - 1:
                                    emit_tr(li)
                            li += 1
            S.barrier()
            S.emit()

        if stop == 'C1':
            dump('d_aT', aTd.ap(), [128, 8, T], BF16, [('aTd', None)])
            return finish_dbg()
        with ExitStack() as st:
            def sb(name, shape, dt=F32):
                return st.enter_context(nc.sbuf_tensor('sb_' + name, shape, dt))
            cost = sb("cost", [128, T]); sint = sb("sint", [128, T])
            DMA('sp', cost[:], cosh_.ap(), [], [('cost', None)])
            DMA('sp', sint[:], sinh_.ap(), [], [('sint', None)])
            lamb = sb("lamb", [128, 256]); lt = sb("lt", [128, 8])
            gd = sb("gd", [128, D])
            DMA('sp', lamb[:], bc(dlh, 0, 256), [], [('lamb', None)])
            DMA('sp', gd[:], bc(dngh, 0, D), [], [('gd', None)])
            TS('pool', gd[:], gd[:], 1.0 - LAM_INIT, None, ALU.mult, None, [('gd', None)], [('gd', None)])
            lp = sb("lp", [128, 128])
            TTo('dve', lp[:, 0:64], lamb[:, 0:64], lamb[:, 64:128], ALU.mult, [('lamb', None)], [('lp', 0)])
            TTo('dve', lp[:, 64:128], lamb[:, 128:192], lamb[:, 192:256], ALU.mult, [('lamb', None)], [('lp', 1)])
            S.op('dve', nc.vector.reduce_sum, dict(out=lt[:, 0:1], in_=lp[:, 0:64], axis=AX.X), [('lp', 0)], [('lt', 0)])
            S.op('dve', nc.vector.reduce_sum, dict(out=lt[:, 1:2], in_=lp[:, 64:128], axis=AX.X), [('lp', 1)], [('lt', 1)])
            ACT(lt[:, 2:4], lt[:, 0:2], AF.Exp, [('lt', 0), ('lt', 1)], [('lt', 2)])
            TTo('dve', lt[:, 4:5], lt[:, 3:4], lt[:, 2:3], ALU.subtract, [('lt', 2)], [('lt', 4)])
            TS('dve', lt[:, 5:6], lt[:, 4:5], -LAM_INIT, None, ALU.add, None, [('lt', 4)], [('lt', 5)])
            neglam = lt[:, 5:6]

            wd = sb("wd", [128, 5, 1024], BF16)
            qT = sb("dqT", [128, T], BF16); kT = sb("dkT", [128, TT], BF16)
            vd = sb("vd", [128, NT, 129], BF16)
            t1 = [sb("t1_%d" % i, [128, 512]) for i in range(2)]
            t2 = [sb("t2_%d" % i, [128, 512]) for i in range(2)]
            Pex = [sb("Pex%d" % i, [128, 1024], BF16) for i in range(3)]
            es = [sb("es%d" % i, [128, 8]) for i in range(2)]
            Ocp = sb("Ocp", [128, 8 * 129]); es4 = sb("es4", [128, 24]); A4 = sb("A4", [128, 4, 128]); sq4 = sb("sq4", [128, 4, 128])
            bb4 = sb("bb4", [128, 4, 128], BF16)
            pending = []

            def flush(upto):
                keep = []
                for (trig, fn) in pending:
                    if upto is None or trig <= upto:
                        fn()
                    else:
                        keep.append((trig, fn))
                pending[:] = keep
            A1 = [sb("A1_%d" % i, [128, 128]) for i in range(2)]
            bb = [sb("bb%d" % i, [128, 128], BF16) for i in range(2)]
            bst = [sb("bst%d" % i, [128, 512], BF16) for i in range(2)]
            junk3 = sb("junk3", [128, 128], BF16)
            S.op('pool', nc.gpsimd.memset, dict(ap=vd[:, :, 128:129], constant=1.0), [], [('vd1', None)])
            cw = [sb("cw_%d" % i, [128, 8, D], BF16) for i in range(2)]
            ccnt = [0]

            def oreg(m, sub):
                r = m * 4 + sub
                if r < 3:
                    return PS(2, 0)[:, r * 129:(r + 1) * 129], PN(2, 0)
                if r < 6:
                    return PS(2, 1)[:, (r - 3) * 129:(r - 2) * 129], PN(2, 1)
                return PS(3, 0)[:, (r - 6) * 129:(r - 5) * 129], PN(3, 0)

            for h in range(8):
                for pi_, c0 in ((0, O_DQ + h * 128), (2, O_DK + h * 128), (4, O_DV + h * 128)):
                    DMA('pool', wd[:, pi_, :].rearrange("p (k n) -> p k n", n=128), wslice(c0, 128), [], [('wd', pi_)])
                for pi_ in (0, 2):
                    s4 = wd[:, pi_, :].rearrange("p (a two j) -> p a two j", two=2, j=16)
                    d4 = wd[:, pi_ + 1, :].rearrange("p (a two j) -> p a two j", two=2, j=16)
                    CP('pool', d4[:, :, 0, :], s4[:, :, 1, :], [('wd', pi_)], [('wd', pi_ + 1)])
                    CP('pool', d4[:, :, 1, :], s4[:, :, 0, :], [('wd', pi_)], [('wd', pi_ + 1)])
                def emit_vgroup(g):
                    tiles = list(range(g * 4, min(NT, g * 4 + 4)))
                    pb_ = g % 2
                    for ii, ti in enumerate(tiles):
                        for kc in range(8):
                            PE(PS(2, pb_)[:, ii * 128:(ii + 1) * 128], uT[:, kc, ti * 128:(ti + 1) * 128], wd[:, 4, kc * 128:(kc + 1) * 128],
                               kc == 0, kc == 7, [('uT', None), ('wd', 4)], [PN(2, pb_)], inc=(kc == 7 and ii == len(tiles) - 1))
                    nt_ = len(tiles)
                    CP('act', vd[:, tiles[0]:tiles[0] + nt_, 0:128], PS(2, pb_)[:, 0:nt_ * 128].rearrange("p (a b) -> p a b", b=128),
                       [PN(2, pb_)], [('vd', g)])

                vg = [0]
                cnt = 0
                for which in range(2):
                    dst = qT if which == 0 else kT
                    dn = 'dqT' if which == 0 else 'dkT'
                    off = 0 if which == 0 else TC
                    wp = 0 if which == 0 else 2
                    for blk in range(8):
                        c0 = TC + blk * 512
                        pi_ = cnt % 2
                        cnt += 1
                        for ab_ in range(2):
                            for kc in range(8):
                                PE(PS(pi_, ab_), wd[:, wp + ab_, kc * 128:(kc + 1) * 128], uT[:, kc, c0:c0 + 512], kc == 0, kc == 7,
                                   [('wd', wp + ab_), ('uT', None)], [PN(pi_, ab_)], inc=(kc == 7))
                        if vg[0] < 9 and (cnt % 2 == 1 or vg[0] < cnt // 2):
                            emit_vgroup(vg[0])
                            vg[0] += 1
                        tb_ = blk % 2
                        TTo('dve', t1[tb_][:], PS(pi_, 0), cost[:, blk * 512:(blk + 1) * 512], ALU.mult, [PN(pi_, 0), ('cost', None)], [('t1', tb_)])
                        TTo('dve', t2[tb_][:], PS(pi_, 1), sint[:, blk * 512:(blk + 1) * 512], ALU.mult, [PN(pi_, 1), ('sint', None)], [('t2', tb_)])
                        TTo('pool', dst[:, off + blk * 512:off + (blk + 1) * 512], t1[tb_][:], t2[tb_][:], ALU.add,
                            [('t1', tb_), ('t2', tb_)], [(dn, blk)])
                    if which == 0:
                        flush(None)
                for kc in range(8):
                    PE(PS(0, 0)[:, 0:256], wd[:, 2, kc * 128:(kc + 1) * 128], uT[:, kc, 0:TC], kc == 0, kc == 7,
                       [('wd', 2), ('uT', None)], [PN(0, 0)], inc=(kc == 7))
                CP('act', kT[:, 0:TC], PS(0, 0)[:, 0:256], [PN(0, 0)], [('dkT', 'c')])
                while vg[0] < 9:
                    emit_vgroup(vg[0])
                    vg[0] += 1
                for e in range(4 * h, 4 * h + 4):
                    for g in range(3):
                        cb_ = ccnt[0] % 2
                        ccnt[0] += 1
                        if g < 2:
                            src = w1h.ap()[e][:, g * D:(g + 1) * D].rearrange("(k p) n -> p k n", p=128)
                            dst = w1bd.ap()[e * 128:(e + 1) * 128, :].rearrange("p (k n) -> p k n", n=2 * D)[:, :, g * D:(g + 1) * D]
                            dn = ('w1bd', (e, g))
                        else:
                            src = w2h.ap()[e].rearrange("(k p) n -> p k n", p=128)
                            dst = w2bd.ap()[e * 128:(e + 1) * 128, :].rearrange("p (k n) -> p k n", n=D)
                            dn = ('w2bd', e)
                        DMA('pool', cw[cb_][:], src, [], [('cw', cb_)])
                        DMA('sp', dst, cw[cb_][:], [('cw', cb_)], [dn])
                for qb in range(8):
                    q0 = qb * 512

                    def emit_qk(kc, n):
                        s_ = n % 2
                        for m in range(2):
                            PE(PS(s_, m), kT[64 * m:64 * m + 64, kc * 128:(kc + 1) * 128], qT[64 * m:64 * m + 64, q0:q0 + 512], True, True,
                               [('dkT', None), ('dqT', None)], [PN(s_, m)], inc=(m == 1))
                        ACT(Pex[n % 3][:], PSUM[s_][:], AF.Exp, [PN(s_, 0), PN(s_, 1)], [('Pex', n % 3)], scale=0.125)

                    emit_qk(0, 0)
                    for kc in range(NT):
                        if kc + 1 < NT:
                            emit_qk(kc + 1, kc + 1)
                        pe_ = Pex[kc % 3]
                        for m in range(2):
                            for sub in range(4):
                                oap, on = oreg(m, sub)
                                r_ = m * 4 + sub
                                PE(oap, pe_[:, m * 512 + sub * 128:m * 512 + (sub + 1) * 128], vd[:, kc, :],
                                   kc == 0 and r_ in (0, 3, 6), kc == NT - 1 and r_ in (2, 5, 7),
                                   [('Pex', kc % 3), ('vd', None), ('vd1', None)], [on], inc=(m == 1 and sub == 3))
                        flush(kc)
                    CP('dve', Ocp[:, 0:387], PS(2, 0)[:, 0:387], [PN(2, 0)], [('Ocp', 0)])
                    CP('dve', Ocp[:, 387:774], PS(2, 1)[:, 0:387], [PN(2, 1)], [('Ocp', 1)])
                    CP('dve', Ocp[:, 774:1032], PS(3, 0)[:, 0:258], [PN(3, 0)], [('Ocp', 2)])

                    def st2(h=h):
                        for sub in range(4):
                            o0 = Ocp[:, sub * 129:(sub + 1) * 129]
                            o1 = Ocp[:, (4 + sub) * 129:(5 + sub) * 129]
                            S.op('dve', nc.vector.reciprocal, dict(out=es4[:, sub:sub + 1], in_=o0[:, 128:129]), [('Ocp', None)], [('es4', (0, sub))])
                            S.op('dve', nc.vector.reciprocal, dict(out=es4[:, 4 + sub:5 + sub], in_=o1[:, 128:129]), [('Ocp', None)], [('es4', (1, sub))])
                            TS('dve', es4[:, 8 + sub:9 + sub], es4[:, 4 + sub:5 + sub], neglam, None, ALU.mult, None, [('es4', (1, sub)), ('lt', 5)], [('es4', (2, sub))])
                            TS('dve', A4[:, sub, :], o0[:, 0:128], es4[:, sub:sub + 1], None, ALU.mult, None, [('Ocp', None), ('es4', (0, sub))], [('A4', sub)])
                            STT('dve', A4[:, sub, :], o1[:, 0:128], es4[:, 8 + sub:9 + sub], A4[:, sub, :], ALU.mult, ALU.add,
                                [('Ocp', None), ('es4', (2, sub)), ('A4', sub)], [('A4', sub)])
                            TTo('pool', sq4[:, sub, :], A4[:, sub, :], A4[:, sub, :], ALU.mult, [('A4', sub)], [('sq4', sub)])
                        S.op('dve', nc.vector.reduce_sum, dict(out=es4[:, 12:16], in_=sq4[:], axis=AX.X), [('sq4', None)], [('es4', 3)])

                    def st3():
                        ACT(es4[:, 16:20], es4[:, 12:16], AF.Ln, [('es4', 3), ('epsb', None)], [('es4', 4)], scale=1.0 / 128, bias=epsb[:, 0:1])
                        ACT(es4[:, 20:24], es4[:, 16:20], AF.Exp, [('es4', 4)], [('es4', 5)], scale=-0.5)

                    def st4(h=h):
                        for sub in range(4):
                            STT('dve', bb4[:, sub, :], A4[:, sub, :], es4[:, 20 + sub:21 + sub], gd[:, h * 128:(h + 1) * 128], ALU.mult, ALU.mult,
                                [('A4', sub), ('es4', 5), ('gd', None)], [('bb4', sub)])

                    def st5():
                        for sub in range(4):
                            PET(PSb(3, 1)[:, sub * 128:(sub + 1) * 128], bb4[:, sub, :], idb[:], [('bb4', sub), ('idb', None)], [PN(3, 1)], inc=(sub == 3))

                    def st6(h=h, qb=qb, q0=q0):
                        sb_ = qb % 2
                        CP('dve', bst[sb_][:], PSb(3, 1)[:, 0:512], [PN(3, 1)], [('bst', sb_)])
                        DMA('sp', bTd.ap()[:, h, q0:q0 + 512], bst[sb_][:], [('bst', sb_)], [('bTd', (h, qb))])

                    pending.extend([(0, st2), (4, st3), (5, st4), (8, st5), (9, st6)])

            flush(None)
            S.barrier()
            S.emit()
        with ExitStack() as st:
            def sb(name, shape, dt=F32):
                return st.enter_context(nc.sbuf_tensor('sb_' + name, shape, dt))
            wgc = [sb("wgc%d" % i, [128, 8, 128], BF16) for i in range(2)]
            sgs = [sb("sgs%d" % i, [128, T], BF16) for i in range(2)]
            for cc in range(16):
                b_ = cc % 2
                DMA('pool', wgc[b_][:], wslice(O_GA + cc * 128, 128), [], [('wgc', b_)])
                for blk in range(8):
                    pi_, pb_ = (blk // 2) % 2, blk % 2
                    for kc in range(8):
                        PE(PS(pi_, pb_), wgc[b_][:, kc, :], uT[:, kc, TC + blk * 512:TC + (blk + 1) * 512], kc == 0, kc == 7,
                           [('wgc', b_), ('uT', None)], [PN(pi_, pb_)], inc=(kc == 7))
                    ACT(sgs[b_][:, blk * 512:(blk + 1) * 512], PS(pi_, pb_), AF.Sigmoid, [PN(pi_, pb_)], [('sgs', b_)])
                DMA('sp', sgd.ap()[:, cc, :], sgs[b_][:], [('sgs', b_)], [('sgd', cc)])
            S.barrier()
            S.emit()
        if stop is None:
            ust.close()

        if stop == 'C2':
            dump('d_bT', bTd.ap(), [128, 8, T], BF16, [('bTd', None)])
            dump('d_sg', sgd.ap(), [128, 16, T], BF16, [('sgd', None)])
            return finish_dbg()
        L = sbg("L", [128, 32, NE])
        with ExitStack() as st:
            G1 = st.enter_context(nc.sbuf_tensor("sb_G1", [128, D], F32))
            DMA('sp', G1[:], bc(modscr, 16 * 128, D), [('modscr', None)], [('G1', None)])
            def sb(name, shape, dt=F32):
                return st.enter_context(nc.sbuf_tensor('sb_' + name, shape, dt))
            wa = sb("wa", [128, 8, D], BF16); wb = sb("wb", [128, 8, D], BF16); wo = sb("wo", [128, 8, D], BF16)
            DMA('pool', wa[:], wah.ap().rearrange("(k p) n -> p k n", p=128), [], [('wa', None)])
            DMA('pool', wb[:], wbh.ap().rearrange("(k p) n -> p k n", p=128), [], [('wb', None)])
            DMA('pool', wo[:], woh.ap().rearrange("(k p) n -> p k n", p=128), [], [('wo', None)])
            rw = sb("rw", [128, 8, NE]); rbb = sb("rbb", [128, NE])
            DMA('sp', rw[:], rwh.ap().rearrange("(k p) n -> p k n", p=128), [], [('rw', None)])
            DMA('sp', rbb[:], bc(rbh, 0, NE), [], [('rbb', None)])
            aTb = [sb("aTb%d" % i, [128, 8, 512], BF16) for i in range(1)] * 2
            bTb = [sb("bTb%d" % i, [128, 8, 512], BF16) for i in range(1)] * 2
            sAb = [sb("sAb%d" % i, [128, 8, 512], BF16) for i in range(1)] * 2
            sBb = [sb("sBb%d" % i, [128, 8, 512], BF16) for i in range(1)] * 2
            yT = [sb("yT%d" % i, [128, 8, 512], BF16) for i in range(1)] * 2
            t1 = [sb("m1_%d" % i, [128, 512]) for i in range(2)]
            t2 = [sb("m2_%d" % i, [128, 512]) for i in range(2)]
            xin = [sb("xi%d" % i, [128, D]) for i in range(2)]
            x1t = [sb("x1t%d" % i, [128, D]) for i in range(2)]
            xn2 = [sb("xn2_%d" % i, [128, D]) for i in range(2)]
            ssb = [sb("ss2_%d" % i, [128, 4]) for i in range(2)]
            u2f = [sb("u2f%d" % i, [128, 8, 128]) for i in range(2)]
            junk = sb("junk4", [128, D], BF16)
            MUL2t = sb("MUL2t", [128, D]); ADD2t = sb("ADD2t", [128, D]); N2Gt = sb("N2Gt", [128, D])
            u2tmp = sb("u2tmp", [128, D]); u2tb = [sb("u2tb%d" % i, [128, D], BF16) for i in range(2)]
            DMA('sp', ADD2t[:], bc(modscr, 3072, D), [('modscr', None)], [('ADD2t', None)])
            DMA('sp', MUL2t[:], bc(modscr, 4096, D), [('modscr', None)], [('MUL2t', None)])
            DMA('sp', N2Gt[:], bc(vecsh, 56 * 128, D), [], [('N2Gt', None)])
            TS('pool', MUL2t[:], MUL2t[:], 1.0, None, ALU.add, None, [('MUL2t', None)], [('MUL2t', None)])
            TTo('pool', MUL2t[:], MUL2t[:], N2Gt[:], ALU.mult, [('MUL2t', None), ('N2Gt', None)], [('MUL2t', None)])
            for tb in range(8):
                b_ = 0
                tsl = slice(tb * 512, (tb + 1) * 512)
                DMA('sp', aTb[b_][:], aTd.ap()[:, :, tsl], [('aTd', None)], [('aTb', b_)])
                DMA('sp', bTb[b_][:], bTd.ap()[:, :, tsl], [('bTd', None)], [('bTb', b_)])
                DMA('sp', sAb[b_][:], sgd.ap()[:, 0:8, tsl], [('sgd', None)], [('sAb', b_)])
                DMA('sp', sBb[b_][:], sgd.ap()[:, 8:16, tsl], [('sgd', None)], [('sBb', b_)])
                for cc in range(8):
                    pi_ = cc % 2
                    for (pb_, w_, w_n, src, srcn) in ((0, wa, 'wa', aTb, 'aTb'), (1, wb, 'wb', bTb, 'bTb')):
                        for kc in range(8):
                            PE(PS(pi_, pb_), w_[:, kc, cc * 128:(cc + 1) * 128], src[b_][:, kc, :], kc == 0, kc == 7,
                               [(w_n, None), (srcn, b_)], [PN(pi_, pb_)], inc=(kc == 7))
                    TTo('dve', t1[pi_][:], PS(pi_, 0), sAb[b_][:, cc, :], ALU.mult, [PN(pi_, 0), ('sAb', b_)], [('m1', pi_)])
                    TTo('dve', t2[pi_][:], PS(pi_, 1), sBb[b_][:, cc, :], ALU.mult, [PN(pi_, 1), ('sBb', b_)], [('m2', pi_)])
                    TTo('pool', yT[b_][:, cc, :], t1[pi_][:], t2[pi_][:], ALU.add, [('m1', pi_), ('m2', pi_)], [('yT', (b_, cc))])
                for sub in range(4):
                    tt = tb * 4 + sub
                    xb_ = tt % 2
                    DMA('sp', xin[xb_][:], xh.ap()[tt * 128:(tt + 1) * 128, :], [], [('xi', xb_)])
                    for half in range(2):
                        for cc in range(8):
                            PE(PS(2, half), yT[b_][:, cc, sub * 128:(sub + 1) * 128], wo[:, cc, half * 512:(half + 1) * 512], cc == 0, cc == 7,
                               [('yT', (b_, cc)), ('wo', None)], [PN(2, half)], inc=(cc == 7))
                        hs_ = slice(half * 512, (half + 1) * 512)
                        TTo('dve', x1t[xb_][:, hs_], PS(2, half), G1[:, hs_], ALU.mult, [PN(2, half), ('G1', None)], [('x1t', (xb_, half))])
                        TTo('pool', x1t[xb_][:, hs_], x1t[xb_][:, hs_], xin[xb_][:, hs_], ALU.add,
                            [('x1t', (xb_, half)), ('xi', xb_)], [('x1t', (xb_, half))])
                    DMA('sp', x1d.ap()[tt * 128:(tt + 1) * 128, :], x1t[xb_][:], [('x1t', (xb_, 0)), ('x1t', (xb_, 1))], [('x1d', tt)])
                    sn = 'ss2_%d' % xb_
                    ACT(junk[:], x1t[xb_][:], AF.Square, [('x1t', (xb_, 0)), ('x1t', (xb_, 1))], [('junk4', None), (sn, 0)], accum_out=ssb[xb_][:, 0:1])
                    ACT(ssb[xb_][:, 1:2], ssb[xb_][:, 0:1], AF.Ln, [(sn, 0), ('epsb', None)], [(sn, 1)], scale=1.0 / D, bias=epsb[:, 0:1])
                    ACT(ssb[xb_][:, 2:3], ssb[xb_][:, 1:2], AF.Exp, [(sn, 1)], [(sn, 2)], scale=-0.5)
                    TS('dve', xn2[xb_][:], x1t[xb_][:], ssb[xb_][:, 2:3], None, ALU.mult, None,
                       [('x1t', (xb_, 0)), ('x1t', (xb_, 1)), (sn, 2)], [('xn2', xb_)])
                    for g in range(2):
                        for j in range(4):
                            kc = g * 4 + j
                            PET(PS(3, g)[:, j * 128:(j + 1) * 128], xn2[xb_][:, kc * 128:(kc + 1) * 128], idf[:],
                                [('xn2', xb_), ('idf', None)], [PN(3, g)], inc=(j == 3))
                        for j in range(4):
                            kc = g * 4 + j
                            o = u2f[xb_][:, kc, :]
                            i_ = PS(3, g)[:, j * 128:(j + 1) * 128]
                            if j % 2 == 0:
                                TS('dve', o, i_, prm[:, 4, kc:kc + 1], prm[:, 5, kc:kc + 1], ALU.mult, ALU.add,
                                   [PN(3, g), ('prm', 4), ('prm', 5)], [('u2f', (xb_, kc))])
                            else:
                                ACT(o, i_, AF.Identity, [PN(3, g), ('prm', 4), ('prm', 5)], [('u2f', (xb_, kc))],
                                    scale=prm[:, 4, kc:kc + 1], bias=prm[:, 5, kc:kc + 1])
                    TTo('dve', u2tmp[:], xn2[xb_][:], MUL2t[:], ALU.mult, [('xn2', xb_), ('MUL2t', None)], [('u2tmp', None)])
                    TTo('pool', u2tb[xb_][:], u2tmp[:], ADD2t[:], ALU.add, [('u2tmp', None), ('ADD2t', None)], [('u2tb', xb_)])
                    DMA('sp', u2tokd.ap()[tt * 128:(tt + 1) * 128, :], u2tb[xb_][:], [('u2tb', xb_)], [('u2tokd', tt)])
                    for kc in range(8):
                        PE(PS(3, 1)[:, 0:NE], u2f[xb_][:, kc, :], rw[:, kc, :], kc == 0, kc == 7,
                           [('u2f', (xb_, kc)), ('rw', None)], [PN(3, 1)], inc=(kc == 7))
                    TTo('dve', L[:, tt, :], PS(3, 1)[:, 0:NE], rbb[:], ALU.add, [PN(3, 1), ('rbb', None)], [('L', tt)])
            S.barrier()
            S.emit()

        if stop == 'D':
            dump('d_x1', x1d.ap(), [T, D], F32, [('x1d', None)])
            dump('d_L', L[:], [128, 32, NE], F32, [('L', None)])
            return finish_dbg()
        IOA = bass.IndirectOffsetOnAxis
        with ExitStack() as st:
            def sb(name, shape, dt=F32):
                return st.enter_context(nc.sbuf_tensor('sb_' + name, shape, dt))
            G2 = sb("G2", [128, D])
            DMA('sp', G2[:], bc(modscr, 40 * 128, D), [('modscr', None)], [('G2', None)])
            stri = sb("stri", [128, 128]); thrB = sb("thrB", [128, NBLK]); pidx = sb("pidx", [128, 1])
            DMA('sp', stri[:], strih.ap(), [], [('stri', None)])
            DMA('sp', thrB[:], thrBh.ap(), [], [('thrB', None)])
            DMA('sp', pidx[:], pidxh.ap(), [], [('pidx', None)])
            gate = sb("gate", [128, 32, NE]); MK = sb("MK", [128, 32, NE]); POS = sb("POS", [128, 32, NE])
            m8 = [sb("m8_%d" % i, [128, 16]) for i in range(2)]
            ex = [sb("ex%d" % i, [128, NE]) for i in range(2)]
            b1in = sb("b1in", [128, 4, 128]); b1T = sb("b1T", [128, 512]); b2s = sb("b2s", [NE, D]); fg = sb("fg", [128, D])
            DMA('sp', b1in[:], b1h.ap().rearrange("(a p) n -> p a n", p=128), [], [('b1in', None)])
            DMA('sp', b2s[:], b2h.ap(), [], [('b2s', None)])
            DMA('sp', fg[:], bc(fngh, 0, D), [], [('fg', None)])
            for a_ in range(4):
                PET(PS(0, 0)[:, a_ * 128:(a_ + 1) * 128], b1in[:, a_, :], idf[:], [('b1in', None), ('idf', None)], [PN(0, 0)], inc=(a_ == 3))
            CP('dve', b1T[:], PS(0, 0), [PN(0, 0)], [('b1T', None)])
            b1v = b1T[:].rearrange("p (e j) -> p e j", j=16)
            TS('dve', b1v[:, :, 8:16], b1v[:, :, 8:16], 1.0, None, ALU.add, None, [('b1T', None)], [('b1T', None)])
            DMA('sp', b1Td.ap().rearrange("(e p) j -> p e j", p=128), b1T[:].rearrange("p (e j) -> p e j", j=16), [('b1T', None)], [('b1Td', None)])
            base = sb("base", [128, NE])
            S.op('dve', nc.vector.memset, dict(ap=base[:], constant=0.0), [], [('base', None)])
            for tt in range(32):
                b_ = tt % 2
                mn = 'm8_%d' % b_
                S.op('dve', nc.vector.max, dict(out=m8[b_][:, 0:8], in_=L[:, tt, :]), [('L', tt)], [(mn, 0)])
                TS('dve', MK[:, tt, :], L[:, tt, :], m8[b_][:, 3:4], None, ALU.is_ge, None, [('L', tt), (mn, 0)], [('MK', tt)])
                TS('dve', m8[b_][:, 8:9], m8[b_][:, 0:1], -1.0, None, ALU.mult, None, [(mn, 0)], [(mn, 1)])
                ACT(ex[b_][:], L[:, tt, :], AF.Exp, [('L', tt), (mn, 1)], [('ex', b_)], bias=m8[b_][:, 8:9])
                TTo('dve', ex[b_][:], ex[b_][:], MK[:, tt, :], ALU.mult, [('ex', b_), ('MK', tt)], [('ex', b_)])
                S.op('dve', nc.vector.reduce_sum, dict(out=m8[b_][:, 9:10], in_=ex[b_][:], axis=AX.X), [('ex', b_)], [(mn, 2)])
                S.op('dve', nc.vector.reciprocal, dict(out=m8[b_][:, 10:11], in_=m8[b_][:, 9:10]), [(mn, 2)], [(mn, 3)])
                TS('dve', gate[:, tt, :], ex[b_][:], m8[b_][:, 10:11], None, ALU.mult, None, [('ex', b_), (mn, 3)], [('gate', tt)])
                PE(PS(0, b_)[:, 0:NE], stri[:], MK[:, tt, :], True, True, [('stri', None), ('MK', tt)], [PN(0, b_)])
                PE(PS(0, b_)[:, NE:2 * NE], ones[:], MK[:, tt, :], True, True, [('ones', None), ('MK', tt)], [PN(0, b_)])
                TTo('dve', POS[:, tt, :], PS(0, b_)[:, 0:NE], base[:], ALU.add, [PN(0, b_), ('base', None)], [('POS', tt)])
                TTo('dve', base[:], base[:], PS(0, b_)[:, NE:2 * NE], ALU.add, [PN(0, b_), ('base', None)], [('base', None)])
            nb = sb("nb", [128, NE]); cA = sb("cA", [128, NE]); cB = sb("cB", [128, NE]); pst = sb("pst", [128, NE])
            S.op('dve', nc.vector.memset, dict(ap=nb[:], constant=0.0), [], [('nb', None)])
            for k in range(T // BLK):
                STT('dve', nb[:], base[:], float(BLK) * k, nb[:], ALU.is_gt, ALU.add, [('base', None), ('nb', None)], [('nb', None)])
            TS('dve', nb[:], nb[:], float(BLK), None, ALU.mult, None, [('nb', None)], [('nb', None)])
            CP('dve', cA[:], nb[:], [('nb', None)], [('cA', None)])
            cur, oth, cn, on = cA, cB, 'cA', 'cB'
            for sh in (1, 2, 4, 8, 16):
                CP('dve', oth[:, 0:sh], cur[:, 0:sh], [(cn, None)], [(on, 0)])
                TTo('dve', oth[:, sh:NE], cur[:, sh:NE], cur[:, 0:NE - sh], ALU.add, [(cn, None)], [(on, 1)])
                cur, oth, cn, on = oth, cur, on, cn
            pend, pendn = cur, cn
            TTo('dve', pst[:], pend[:], nb[:], ALU.subtract, [(pendn, None), ('nb', None)], [('pst', None)])
            D4 = sb("D4", [128, 32, 4], mybir.dt.int32); g4 = sb("g4", [128, 32, 4])
            key = [sb("key%d" % i, [128, NE]) for i in range(2)]
            oh = [sb("oh%d" % i, [128, NE]) for i in range(2)]
            k8 = [sb("k8_%d" % i, [128, 8]) for i in range(2)]
            for tt in range(32):
                b_ = tt % 2
                kn, k8n = 'key%d' % b_, 'k8_%d' % b_
                TTo('dve', POS[:, tt, :], POS[:, tt, :], pst[:], ALU.add, [('POS', tt), ('pst', None)], [('POS', tt)])
                STT('dve', key[b_][:], POS[:, tt, :], 1.0, MK[:, tt, :], ALU.add, ALU.mult, [('POS', tt), ('MK', tt)], [(kn, None)])
                S.op('dve', nc.vector.max, dict(out=k8[b_][:], in_=key[b_][:]), [(kn, None)], [(k8n, None)])
                TS('dve', D4[:, tt, :], k8[b_][:, 0:4], -1.0, None, ALU.add, None, [(k8n, None)], [('D4', tt)])
                for j in range(4):
                    on_ = 'oh%d' % (j % 2)
                    TS('dve', oh[j % 2][:], key[b_][:], k8[b_][:, j:j + 1], None, ALU.is_equal, None, [(kn, None), (k8n, None)], [(on_, None)])
                    TTo('dve', oh[j % 2][:], oh[j % 2][:], gate[:, tt, :], ALU.mult, [(on_, None), ('gate', tt)], [(on_, None)])
                    S.op('dve', nc.vector.reduce_sum, dict(out=g4[:, tt, j:j + 1], in_=oh[j % 2][:], axis=AX.X), [(on_, None)], [('g4', (tt, j))])
            be = sb("be", [128, NBLK]); OFFS = sb("OFFS", [128, NBLK], mybir.dt.int32)
            S.op('dve', nc.vector.memset, dict(ap=be[:], constant=0.0), [], [('be', None)])
            for e in range(NE):
                STT('dve', be[:], thrB[:], pend[:, e:e + 1], be[:], ALU.is_ge, ALU.add, [('thrB', None), (pendn, None), ('be', None)], [('be', None)])
            TS('dve', be[:], be[:], float(NE - 1), 128.0, ALU.min, ALU.mult, [('be', None)], [('be', None)])
            TS('dve', OFFS[:], be[:], pidx[:, 0:1], None, ALU.add, None, [('be', None), ('pidx', None)], [('OFFS', None)])
            ut = [sb("ut%d" % i, [128, D], BF16) for i in range(2)]
            for tt in range(32):
                b_ = tt % 2
                DMA('sp', ut[b_][:], u2tokd.ap()[tt * 128:(tt + 1) * 128, :], [('u2tokd', tt)], [('ut', b_)])
                for j in range(4):
                    S.dma('pool', dict(out=Xg.ap(), out_offset=IOA(ap=D4[:, tt, j:j + 1], axis=0), in_=ut[b_][:], in_offset=None),
                          [('ut', b_), ('D4', tt)], [('Xg', (tt, j))], method=nc.gpsimd.indirect_dma_start)

            with ExitStack() as st2:
                def sb2(name, shape, dt=F32):
                    return st2.enter_context(nc.sbuf_tensor('sb_' + name, shape, dt))
                w1s = [sb2("w1s%d" % i, [128, 8 * 2 * D], BF16) for i in range(2)]
                w2s = [sb2("w2s%d" % i, [128, 8 * D], BF16) for i in range(2)]
                b1g = [sb2("b1g%d" % i, [128, 16]) for i in range(2)]
                xg = [sb2("xg%d" % i, [128, D], BF16) for i in range(4)]
                xT = [sb2("xT%d" % i, [128, 8, BLK], BF16) for i in range(2)]
                actT = [sb2("actT%d" % i, [128, 8, BLK], BF16) for i in range(2)]
                hg = [sb2("hg%d" % i, [128, BLK]) for i in range(2)]
                hl = [sb2("hl%d" % i, [128, BLK]) for i in range(2)]
                yst = [sb2("yst%d" % i, [128, D]) for i in range(2)]

                def load_w(b):
                    wb_ = b % 2
                    off = IOA(ap=OFFS[:, b:b + 1], axis=0)
                    for (dst, dn, src, sn) in ((w1s, 'w1s', w1bd, 'w1bd'), (w2s, 'w2s', w2bd, 'w2bd'), (b1g, 'b1g', b1Td, 'b1Td')):
                        S.dma('pool', dict(out=dst[wb_][:], out_offset=None, in_=src.ap(), in_offset=off),
                              [('OFFS', None), (sn, None)], [(dn, wb_)], method=nc.gpsimd.indirect_dma_start)

                def load_x(b):
                    for sub in range(2):
                        t_ = 2 * b + sub
                        xi_ = 2 * (b % 2) + sub
                        DMA('sp', xg[xi_][:], Xg.ap()[t_ * 128:(t_ + 1) * 128, :], [('Xg', None)], [('xg', xi_)])

                load_w(0)
                ycnt = 0
                for b in range(NBLK):
                    wb_ = b % 2
                    if b + 1 < NBLK:
                        load_w(b + 1)
                    if b == 0:
                        load_x(0)
                    if b + 1 < NBLK:
                        load_x(b + 1)
                    for sub in range(2):
                        t_ = 2 * b + sub
                        xi_ = 2 * (b % 2) + sub
                        for kc in range(8):
                            PET(PSb(3, 0)[:, kc * 128:(kc + 1) * 128], xg[xi_][:, kc * 128:(kc + 1) * 128], idb[:], [('xg', xi_), ('idb', None)], [PN(3, 0)], inc=(kc == 7))
                        CP('act', xT[wb_][:, :, sub * 128:(sub + 1) * 128], PSb(3, 0).rearrange("p (k n) -> p k n", n=128), [PN(3, 0)], [('xT', (wb_, sub))])
                    for cp in range(8):
                        pi_ = cp % 2
                        for gl in range(2):
                            for kc in range(8):
                                c0 = kc * 2 * D + gl * D + cp * 128
                                PE(PS(pi_, gl)[:, 0:BLK], w1s[wb_][:, c0:c0 + 128], xT[wb_][:, kc, :], kc == 0, kc == 7,
                                   [('w1s', wb_), ('xT', (wb_, 0)), ('xT', (wb_, 1))], [PN(pi_, gl)], inc=(kc == 7))
                        bg = b1g[wb_][:, cp:cp + 1]
                        bl = b1g[wb_][:, 8 + cp:9 + cp]
                        TS('dve', hg[pi_][:], PS(pi_, 0)[:, 0:BLK], bg, 7.0, ALU.add, ALU.min, [PN(pi_, 0), ('b1g', wb_)], [('hg', pi_)])
                        ACT(hg[pi_][:], hg[pi_][:], AF.Silu, [('hg', pi_)], [('hg', pi_)], scale=1.702)
                        TS('dve', hl[pi_][:], PS(pi_, 1)[:, 0:BLK], bl, 8.0, ALU.add, ALU.min, [PN(pi_, 1), ('b1g', wb_)], [('hl', pi_)])
                        STT('dve', actT[wb_][:, cp, :], hl[pi_][:], -6.0, hg[pi_][:], ALU.max, ALU.mult, [('hg', pi_), ('hl', pi_)], [('actT', (wb_, cp))])
                    for sub in range(2):
                        t_ = 2 * b + sub
                        ys_ = ycnt % 2
                        ycnt += 1
                        for half in range(2):
                            for cp in range(8):
                                c0 = cp * D + half * 512
                                PE(PS(2, half), actT[wb_][:, cp, sub * 128:(sub + 1) * 128], w2s[wb_][:, c0:c0 + 512], cp == 0, cp == 7,
                                   [('actT', (wb_, cp)), ('w2s', wb_)], [PN(2, half)], inc=(cp == 7))
                            hs_ = slice(half * 512, (half + 1) * 512)
                            if half == 0:
                                ACT(yst[ys_][:, hs_], PS(2, half), AF.Copy, [PN(2, half)], [('yst', (ys_, half))], scale=1.0 / 1.702)
                            else:
                                TS('dve', yst[ys_][:, hs_], PS(2, half), 1.0 / 1.702, None, ALU.mult, None, [PN(2, half)], [('yst', (ys_, half))])
                        DMA('sp', Yg.ap()[t_ * 128:(t_ + 1) * 128, :], yst[ys_][:], [('yst', (ys_, 0)), ('yst', (ys_, 1))], [('Yg', t_)])


                S.barrier()
                S.emit()

            yg = [[sb("yg%d_%d" % (i, j), [128, D]) for j in range(4)] for i in range(2)]
            Yacc = [sb("Yacc%d" % i, [128, D]) for i in range(2)]; gTt = [sb("gTt%d" % i, [NE, 128]) for i in range(2)]
            x1l = [sb("x1l%d" % i, [128, D]) for i in range(2)]; ot = [sb("ot%d" % i, [128, D]) for i in range(2)]
            ssf = [sb("ssf%d" % i, [128, 4]) for i in range(2)]
            for tt in range(32):
                b_ = tt % 2
                ya, x1, o_, sf, gt = Yacc[b_], x1l[b_], ot[b_], ssf[b_], gTt[b_]
                yn, xn_, on_, sn, gn = ('Yacc', b_), ('x1l', b_), ('ot', b_), 'ssf%d' % b_, ('gTt', b_)
                for j in range(4):
                    S.dma('pool', dict(out=yg[b_][j][:], out_offset=None, in_=Yg.ap(), in_offset=IOA(ap=D4[:, tt, j:j + 1], axis=0)),
                          [('D4', tt), ('Yg', None)], [('yg', (b_, j))], method=nc.gpsimd.indirect_dma_start)
                DMA('sp', x1[:], x1d.ap()[tt * 128:(tt + 1) * 128, :], [('x1d', tt)], [xn_])
                TS('dve', ya[:], yg[b_][0][:], g4[:, tt, 0:1], None, ALU.mult, None, [('yg', (b_, 0)), ('g4', (tt, 0))], [yn])
                for j in range(1, 4):
                    STT('dve', ya[:], yg[b_][j][:], g4[:, tt, j:j + 1], ya[:], ALU.mult, ALU.add,
                        [('yg', (b_, j)), ('g4', (tt, j)), yn], [yn])
                PET(PS(3, b_)[0:NE, 0:128], gate[:, tt, :], idf[:], [('gate', tt), ('idf', None)], [PN(3, b_)])
                CP('act', gt[:], PS(3, b_)[0:NE, 0:128], [PN(3, b_)], [gn])
                for half in range(2):
                    hs_ = slice(half * 512, (half + 1) * 512)
                    PE(PS(b_, half), gt[:], b2s[:, hs_], True, True, [gn, ('b2s', None)], [PN(b_, half)])
                    TTo('dve', ya[:, hs_], ya[:, hs_], PS(b_, half), ALU.add, [PN(b_, half), yn], [yn])
                TTo('dve', ya[:], ya[:], G2[:], ALU.mult, [yn, ('G2', None)], [yn])
                TTo('dve', x1[:], x1[:], ya[:], ALU.add, [xn_, yn], [xn_])
                ACT(o_[:], x1[:], AF.Square, [xn_], [on_, (sn, 0)], accum_out=sf[:, 0:1])
                ACT(sf[:, 1:2], sf[:, 0:1], AF.Ln, [(sn, 0), ('epsb', None)], [(sn, 1)], scale=1.0 / D, bias=epsb[:, 0:1])
                ACT(sf[:, 2:3], sf[:, 1:2], AF.Exp, [(sn, 1)], [(sn, 2)], scale=-0.5)
                STT('dve', o_[:], x1[:], sf[:, 2:3], fg[:], ALU.mult, ALU.mult, [xn_, (sn, 2), ('fg', None)], [on_])
                DMA('sp', yh.ap()[tt * 128:(tt + 1) * 128, :], o_[:], [on_], [('y', tt)])
            S.barrier()
            S.emit()
        print("ninstr", S.ninstr)
    return nc


def _consts():
    idf = np.eye(128, dtype=np.float32)
    s = np.arange(128)
    triu = (s[:, None] <= s[None, :]).astype(np.float32)
    tril = (s[:, None] >= s[None, :]).astype(np.float32)
    ones = np.ones((128, 128), np.float32)
    t = np.arange(T)
    row = (t // 64).astype(np.float64)
    col = (t % 64).astype(np.float64)
    inv = 10000.0 ** (-np.arange(16, dtype=np.float64) / 16.0)
    inv32 = inv.astype(np.float32).astype(np.float64)
    cost = np.zeros((128, T), np.float32)
    sint = np.zeros((128, T), np.float32)
    for p in range(128):
        d = p % 64
        pos = row if d < 32 else col
        j = d % 16
        ang = (pos.astype(np.float32) * np.float32(inv32[j])).astype(np.float32)
        first = (d % 32) < 16
        cost[p] = np.cos(ang)
        sint[p] = -np.sin(ang) if first else np.sin(ang)
    stri = (s[:, None] < s[None, :]).astype(np.float32)
    thrB = np.tile((float(BLK) * np.arange(NBLK, dtype=np.float32))[None, :], (128, 1))
    pidx = np.arange(128, dtype=np.float32).reshape(128, 1)
    return dict(idf=idf, idb=idf.astype(ml_dtypes.bfloat16), triu=triu, tril=tril, ones=ones, cost=cost, sint=sint,
                stri=stri, thrB=np.ascontiguousarray(thrB), pidx=pidx)


def _in_maps(x, c, ctx, c_ctx, ada_w, ada_b, norm1_g, norm2_g, w_in, mlstm_gate_b, mlstm_norm_g,
             diff_lambda, diff_norm_g, w_branch_a, w_branch_b, w_out, router_w, router_b,
             exp_w1, exp_b1, exp_w2, exp_b2, final_norm_g):
    f = lambda a: np.ascontiguousarray(np.asarray(a, dtype=np.float32))
    cons = _consts()
    vecs = np.concatenate([f(ada_b)[0].reshape(48, 128), f(norm1_g)[0].reshape(8, 128), f(norm2_g)[0].reshape(8, 128)], axis=0)
    shared = dict(
        ada_w=f(ada_w)[0], vecs=f(vecs), b1r=f(exp_b1)[0].reshape(512, 128), w_in=f(w_in)[0], gate_b=f(mlstm_gate_b)[0],
        mng=f(mlstm_norm_g)[0].reshape(D), dng=f(diff_norm_g)[0].reshape(D), fng=f(final_norm_g), dlam=f(diff_lambda)[0].reshape(256),
        w_a=f(w_branch_a)[0], w_b=f(w_branch_b)[0], w_o=f(w_out)[0], rw=f(router_w)[0], rb=f(router_b)[0],
        w1=f(exp_w1)[0], w2=f(exp_w2)[0], b2=f(exp_b2)[0], **cons)
    maps = []
    xf, cf, ctxf, ccf = f(x), f(c), f(ctx), f(c_ctx)
    for b in range(8):
        m = dict(shared)
        m["x"] = xf[b]
        m["ctx"] = ctxf[b]
        m["cvec"] = np.ascontiguousarray(np.stack([cf[b], ccf], axis=1))
        maps.append(m)
    return maps


def kernel(**inputs):
    maps = _in_maps(**inputs)
    nc = build_nc()
    res = run_bass_kernel_spmd(nc, maps, core_ids=list(range(8)))
    return np.stack([np.asarray(r["y"], dtype=np.float32) for r in res.results], axis=0)
```
